# Optimizing a Trainium2 kernel written in Bass

```python
import math
import jax
import jax.numpy as jnp
from jax import lax
import numpy as np

D_MODEL = 1024
BATCH = 4
SEQ = 8192
DEPTH = 1
DEC_BATCH = 32
DEC_SEQ = 8
PAST_LEN = 16384
PAGE_SIZE = 128

F32 = jnp.float32

GDN_HEADS = 4
GDN_DK = 128
GDN_DV = 128
GDN_QK = GDN_HEADS * GDN_DK
GDN_V = GDN_HEADS * GDN_DV
GDN_CONV_DIM = 2 * GDN_QK + GDN_V
GDN_CONV = 4
GDN_CHUNK = 64

NSA_HEADS = 8
NSA_KV_HEADS = 2
NSA_GROUP = NSA_HEADS // NSA_KV_HEADS
NSA_HD = 64
NSA_Q = NSA_HEADS * NSA_HD
NSA_KV = NSA_KV_HEADS * NSA_HD
CMP_STRIDE = 16
CMP_LEN = 2 * CMP_STRIDE
SEL_BLOCK = 64
SEL_TOP = 16
WINDOW = 512
Q_BLOCK = 128
FORCE_SCORE = 1.0e4

N_GROUPS = 4
EXPERTS_PER_GROUP = 8
N_EXPERTS = N_GROUPS * EXPERTS_PER_GROUP
TOP_K_IN_GROUP = 2
D_EXPERT = 256

RMS_EPS = 1e-6

PROJ_SIZES = (GDN_CONV_DIM, GDN_V, GDN_HEADS, GDN_HEADS, NSA_Q, 6 * NSA_KV, 3 * NSA_HEADS, D_MODEL, D_MODEL)
PROJ_DIM = sum(PROJ_SIZES)
PROJ_SPLITS = tuple(sum(PROJ_SIZES[:i + 1]) for i in range(len(PROJ_SIZES) - 1))

kernel_name = 'hybrid_gdn_nsa_hmoe_step'


def rmsnorm(x, g):
    xf = x.astype(F32)
    y = xf * lax.rsqrt(jnp.mean(xf * xf, axis=-1, keepdims=True) + RMS_EPS)
    return (y * g.astype(F32)).astype(x.dtype)


def l2norm(x):
    return x * lax.rsqrt(jnp.sum(x * x, axis=-1, keepdims=True) + RMS_EPS)


def masked_softmax(s, mask):
    s = jnp.where(mask, s, -jnp.inf)
    m = jnp.max(s, axis=-1, keepdims=True)
    m = jnp.where(jnp.isfinite(m), m, 0.0)
    e = jnp.where(mask, jnp.exp(s - m), 0.0)
    return e / jnp.maximum(jnp.sum(e, axis=-1, keepdims=True), 1e-30)


def alibi_slopes():
    h = jnp.arange(1, NSA_HEADS + 1, dtype=F32)
    return jnp.exp2(-8.0 * h / NSA_HEADS).reshape(NSA_KV_HEADS, NSA_GROUP)


def project(x, norm_g, w_in):
    xn = rmsnorm(x, norm_g)
    return jnp.split(jnp.einsum('bld,de->ble', xn, w_in), PROJ_SPLITS, axis=-1)


def causal_conv(u, buf, w):
    L = u.shape[1]
    full = jnp.concatenate([buf.astype(u.dtype), u], axis=1)
    out = w[0] * full[:, 0:L]
    for i in range(1, GDN_CONV):
        out = out + w[i] * full[:, i:i + L]
    return jax.nn.silu(out), full[:, L:]


def gated_delta_chunked(q, k, v, g, beta, s0, chunk):
    B, L, H, DK = q.shape
    DV = v.shape[-1]
    n = L // chunk

    def to_c(t):
        return t.astype(F32).reshape(B, n, chunk, H, t.shape[-1]).transpose(0, 3, 1, 2, 4)

    qc, kc, vc = to_c(q), to_c(k), to_c(v)
    gc = g.astype(F32).reshape(B, n, chunk, H).transpose(0, 3, 1, 2)
    bc = beta.astype(F32).reshape(B, n, chunk, H).transpose(0, 3, 1, 2)
    gcum = jnp.cumsum(gc, axis=-1)
    tri_incl = jnp.tril(jnp.ones((chunk, chunk), bool))
    tri_strict = jnp.tril(jnp.ones((chunk, chunk), bool), -1)
    decay = jnp.exp(jnp.where(tri_incl, gcum[..., :, None] - gcum[..., None, :], -jnp.inf))
    kb = kc * bc[..., None]
    a_strict = jnp.where(tri_strict, jnp.einsum('bhncd,bhnsd->bhncs', kb, kc) * decay, 0.0)
    rhs = jnp.concatenate([vc * bc[..., None], kb * jnp.exp(gcum)[..., None]], axis=-1)
    sol = lax.linalg.triangular_solve(a_strict, rhs, left_side=True, lower=True, unit_diagonal=True)
    u_coef, w_coef = sol[..., :DV], sol[..., DV:]
    qk = jnp.einsum('bhncd,bhnsd->bhncs', qc, kc) * decay
    q_dec = qc * jnp.exp(gcum)[..., None]
    k_tail = kc * jnp.exp(gcum[..., -1:] - gcum)[..., None]
    g_last = jnp.exp(gcum[..., -1])
    xs = tuple(jnp.moveaxis(t, 2, 0) for t in (u_coef, w_coef, qk, q_dec, k_tail, g_last))

    def step(S, inp):
        u_c, w_c, qk_c, qd_c, kt_c, gl_c = inp
        u = u_c - jnp.einsum('bhck,bhkv->bhcv', w_c, S)
        o = jnp.einsum('bhck,bhkv->bhcv', qd_c, S) + jnp.einsum('bhcs,bhsv->bhcv', qk_c, u)
        S = S * gl_c[..., None, None] + jnp.einsum('bhck,bhcv->bhkv', kt_c, u)
        return S, o

    S, o = lax.scan(step, s0.astype(F32), xs)
    return o.transpose(1, 0, 3, 2, 4).reshape(B, L, H, DV), S


def gdn_branch(qkv_pre, z, b_raw, a_raw, conv_buf, s0, lp):
    B, L, _ = qkv_pre.shape
    qkv, conv_new = causal_conv(qkv_pre, conv_buf, lp['gdn_conv_w'])
    qkv = qkv.astype(F32)
    q = l2norm(qkv[..., :GDN_QK].reshape(B, L, GDN_HEADS, GDN_DK)) * (GDN_DK ** -0.5)
    k = l2norm(qkv[..., GDN_QK:2 * GDN_QK].reshape(B, L, GDN_HEADS, GDN_DK))
    v = qkv[..., 2 * GDN_QK:].reshape(B, L, GDN_HEADS, GDN_DV)
    beta = jax.nn.sigmoid(b_raw.astype(F32))
    g = -jnp.exp(lp['gdn_a_log'].astype(F32)) * jax.nn.softplus(a_raw.astype(F32) + lp['gdn_dt_bias'].astype(F32))
    o, s_new = gated_delta_chunked(q, k, v, g, beta, s0, math.gcd(L, GDN_CHUNK))
    o = rmsnorm(o, lp['gdn_norm_g']) * jax.nn.silu(z.astype(F32).reshape(B, L, GDN_HEADS, GDN_DV))
    return o.reshape(B, L, GDN_V), s_new, conv_new


def compress(k, pos_w, w_c):
    B, T = k.shape[:2]
    n_ch = T // CMP_STRIDE
    ch = k[:, :n_ch * CMP_STRIDE].reshape(B, n_ch, CMP_STRIDE, NSA_KV_HEADS, NSA_HD)
    pw = pos_w.astype(F32)
    head = jnp.einsum('bmpgd,p->bmgd', ch, pw[:CMP_STRIDE])
    tail = jnp.einsum('bmpgd,p->bmgd', ch, pw[CMP_STRIDE:])
    blocks = head[:, :-1] + tail[:, 1:]
    return jnp.einsum('bcgd,de->bcge', blocks, w_c.astype(F32))


def sel_overlap(n_cmp, n_sel):
    i = jnp.arange(n_cmp)[:, None]
    j = jnp.arange(n_sel)[None, :]
    lo = jnp.maximum(i * CMP_STRIDE, j * SEL_BLOCK)
    hi = jnp.minimum(i * CMP_STRIDE + CMP_LEN, (j + 1) * SEL_BLOCK)
    return jnp.maximum(hi - lo, 0).astype(F32) / CMP_LEN


def nsa_sources(full4, lp):
    f = full4.astype(F32)
    B, T = f.shape[:2]
    kc = compress(f[:, :, 0], lp['cmp_pos_wk'], lp['cmp_wk'])
    vc = compress(f[:, :, 1], lp['cmp_pos_wv'], lp['cmp_wv'])
    ns = -(-T // SEL_BLOCK)
    sel = jnp.pad(f[:, :, 2:4], ((0, 0), (0, ns * SEL_BLOCK - T), (0, 0), (0, 0), (0, 0)))
    sel = sel.reshape(B, ns, SEL_BLOCK, 2, NSA_KV_HEADS, NSA_HD).transpose(3, 0, 4, 1, 2, 5)
    return kc, vc, sel[0], sel[1]


def cmp_attend(q, qpos, kc, vc, slopes):
    n_cmp = kc.shape[1]
    blk_end = jnp.arange(n_cmp) * CMP_STRIDE + CMP_LEN - 1
    dist = qpos[:, None] - blk_end[None, :]
    s = jnp.einsum('bqghd,bcgd->bghqc', q, kc) - slopes[:, :, None, None] * dist.astype(F32)
    p = masked_softmax(s, dist >= 0)
    return jnp.einsum('bghqc,bcgd->bqghd', p, vc), p


def sel_attend(q, qpos, p_cmp, kb, vb, slopes):
    B, G, NS = kb.shape[:3]
    Q = q.shape[1]
    imp = jnp.einsum('bghqc,cs->bgqs', p_cmp, sel_overlap(p_cmp.shape[-1], NS))
    blk = jnp.arange(NS)[None, :]
    cur = (qpos // SEL_BLOCK)[:, None]
    forced = (blk == 0) | (blk == cur) | (blk == cur - 1)
    score = jnp.where(forced, FORCE_SCORE, imp)
    score = jnp.where(blk <= cur, score, -jnp.inf)
    top_v, top_i = lax.top_k(score, min(SEL_TOP, NS))
    ok = jnp.isfinite(top_v)
    take = jax.vmap(jax.vmap(lambda blocks, idx: blocks[idx]))
    kg = take(kb, top_i)
    vg = take(vb, top_i)
    n = top_i.shape[-1]
    kpos = top_i[..., None] * SEL_BLOCK + jnp.arange(SEL_BLOCK)
    dist = qpos[:, None, None] - kpos
    mask = (dist >= 0) & ok[..., None]
    s = jnp.einsum('bqghd,bgqnkd->bghqnk', q, kg) - slopes[None, :, :, None, None, None] * dist[:, :, None].astype(F32)
    p = masked_softmax(s.reshape(B, G, NSA_GROUP, Q, n * SEL_BLOCK), mask.reshape(B, G, 1, Q, n * SEL_BLOCK))
    return jnp.einsum('bghqm,bgqmd->bqghd', p, vg.reshape(B, G, Q, n * SEL_BLOCK, NSA_HD))


def win_attend(q, qpos, kw, vw, kwpos, slopes):
    dist = qpos[:, None] - kwpos[None, :]
    mask = (dist >= 0) & (dist <= WINDOW) & (kwpos >= 0)[None, :]
    s = jnp.einsum('bqghd,bkgd->bghqk', q, kw) - slopes[:, :, None, None] * dist.astype(F32)
    return jnp.einsum('bghqk,bkgd->bqghd', masked_softmax(s, mask), vw)


def nsa_core(q, gates, qpos, kc, vc, kb, vb, kw, vw, kwpos):
    slopes = alibi_slopes()
    o_c, p_c = cmp_attend(q, qpos, kc, vc, slopes)
    o_s = sel_attend(q, qpos, p_c, kb, vb, slopes)
    o_w = win_attend(q, qpos, kw, vw, kwpos, slopes)
    return gates[..., 0:1] * o_c + gates[..., 1:2] * o_s + gates[..., 2:3] * o_w


def nsa_prompt(q, gates, kv6, lp, win_buf):
    B, L = q.shape[:2]
    kc, vc, kb, vb = nsa_sources(kv6[:, :, :4], lp)
    kvw_raw = jnp.pad(kv6[:, :, 4:6], ((0, 0), (WINDOW, 0), (0, 0), (0, 0), (0, 0)))
    kvw = kvw_raw.astype(F32)
    n_blk = L // Q_BLOCK
    qb = jnp.swapaxes(q.reshape(B, n_blk, Q_BLOCK, NSA_KV_HEADS, NSA_GROUP, NSA_HD), 0, 1)
    gb = jnp.swapaxes(gates.reshape(B, n_blk, Q_BLOCK, NSA_KV_HEADS, NSA_GROUP, 3), 0, 1)

    def one_block(args):
        q_i, g_i, i = args
        start = i * Q_BLOCK
        qpos = start + jnp.arange(Q_BLOCK)
        kw = lax.dynamic_slice_in_dim(kvw, start, WINDOW + Q_BLOCK, axis=1)
        kwpos = start - WINDOW + jnp.arange(WINDOW + Q_BLOCK)
        return nsa_core(q_i, g_i, qpos, kc, vc, kb, vb, kw[:, :, 0], kw[:, :, 1], kwpos)

    o = lax.map(one_block, (qb, gb, jnp.arange(n_blk)))
    o = jnp.swapaxes(o, 0, 1).reshape(B, L, NSA_Q)
    return o, kvw_raw[:, kvw_raw.shape[1] - win_buf:]


def nsa_sample(q, gates, kv6, cache_kv_l, page_table, cache_win_l, lp):
    B, L = q.shape[:2]
    past_len = page_table.shape[1] * cache_kv_l.shape[1]
    past = cache_kv_l[page_table].reshape(B, past_len, 4, NSA_KV_HEADS, NSA_HD)
    full4 = jnp.concatenate([past, kv6[:, :, :4].astype(past.dtype)], axis=1)
    kc, vc, kb, vb = nsa_sources(full4, lp)
    win_buf = cache_win_l.shape[1]
    kvw = jnp.concatenate([cache_win_l, kv6[:, :, 4:6].astype(cache_win_l.dtype)], axis=1)
    kwpos = past_len - win_buf + jnp.arange(kvw.shape[1])
    qpos = past_len + jnp.arange(L)
    kvw_f = kvw.astype(F32)
    o = nsa_core(q, gates, qpos, kc, vc, kb, vb, kvw_f[:, :, 0], kvw_f[:, :, 1], kwpos)
    return o.reshape(B, L, NSA_Q), kvw[:, kvw.shape[1] - win_buf:]


def moe(xn, lp):
    N, D = xn.shape
    xf = xn.astype(F32)
    g_logit = xf @ lp['w_grp'].astype(F32) + lp['b_grp'].astype(F32)
    grp_oh = jax.nn.one_hot(jnp.argmax(g_logit, axis=-1), N_GROUPS, dtype=F32)
    p_grp = jnp.sum(jax.nn.softmax(g_logit, axis=-1) * grp_oh, axis=-1, keepdims=True)
    e_logit = (xf @ lp['w_rt'].astype(F32) + lp['b_rt'].astype(F32)).reshape(N, N_GROUPS, EXPERTS_PER_GROUP)
    e_in = jnp.einsum('nge,ng->ne', e_logit, grp_oh)
    top_v, top_i = lax.top_k(e_in, TOP_K_IN_GROUP)
    w = jax.nn.softmax(top_v, axis=-1) * p_grp
    combine = jnp.einsum('nk,nke->ne', w, jax.nn.one_hot(top_i, EXPERTS_PER_GROUP, dtype=F32))
    combine = combine[:, None, :] * grp_oh[:, :, None]
    wg = lp['w_e_gate'].reshape(N_GROUPS, EXPERTS_PER_GROUP, D, D_EXPERT)
    wu = lp['w_e_up'].reshape(N_GROUPS, EXPERTS_PER_GROUP, D, D_EXPERT)
    wd = lp['w_e_down'].reshape(N_GROUPS, EXPERTS_PER_GROUP, D_EXPERT, D)
    out = jnp.zeros((N, D), F32)
    for gi in range(N_GROUPS):
        h = jax.nn.silu(jnp.einsum('nd,edf->nef', xn, wg[gi])) * jnp.einsum('nd,edf->nef', xn, wu[gi])
        out = out + jnp.einsum('nef,efd->nd', h * combine[:, gi, :, None], wd[gi])
    return out


def layer_tail(x, y_a, y_b, gate_a, gate_b, lp):
    y_a = y_a.astype(x.dtype)
    y_b = y_b.astype(x.dtype)
    m = (jax.nn.sigmoid(gate_a) * jnp.einsum('blc,cd->bld', y_a, lp['w_branch_a'])
         + jax.nn.sigmoid(gate_b) * jnp.einsum('blc,cd->bld', y_b, lp['w_branch_b']))
    h = x + jnp.einsum('bld,de->ble', m, lp['w_out']).astype(x.dtype)
    B, L, D = h.shape
    hn = rmsnorm(h, lp['norm2_g']).reshape(B * L, D)
    return h + moe(hn, lp).reshape(B, L, D).astype(h.dtype)


def split_heads(x, lp):
    B, L, _ = x.shape
    qkv_pre, z, b_raw, a_raw, q_n, kv_n, g_n, gate_a, gate_b = project(x, lp['norm1_g'], lp['w_in'])
    q = q_n.astype(F32).reshape(B, L, NSA_KV_HEADS, NSA_GROUP, NSA_HD) * (NSA_HD ** -0.5)
    gates = jax.nn.sigmoid(g_n.astype(F32)).reshape(B, L, NSA_KV_HEADS, NSA_GROUP, 3)
    kv6 = kv_n.reshape(B, L, 6, NSA_KV_HEADS, NSA_HD)
    return (qkv_pre, z, b_raw, a_raw), q, gates, kv6, gate_a, gate_b


def prompt_layer(x, lp, win_buf):
    B = x.shape[0]
    gdn_in, q, gates, kv6, gate_a, gate_b = split_heads(x, lp)
    conv0 = jnp.zeros((B, GDN_CONV - 1, GDN_CONV_DIM), x.dtype)
    s0 = jnp.zeros((B, GDN_HEADS, GDN_DK, GDN_DV), F32)
    y_a, s_new, conv_new = gdn_branch(gdn_in[0], gdn_in[1], gdn_in[2], gdn_in[3], conv0, s0, lp)
    y_b, win_new = nsa_prompt(q, gates, kv6, lp, win_buf)
    y = layer_tail(x, y_a, y_b, gate_a, gate_b, lp)
    return y, kv6[:, :, :4], win_new, s_new, conv_new


def sample_layer(x, cache_kv_l, page_table, cache_win_l, s0, conv_buf, lp):
    gdn_in, q, gates, kv6, gate_a, gate_b = split_heads(x, lp)
    y_a, s_new, conv_new = gdn_branch(gdn_in[0], gdn_in[1], gdn_in[2], gdn_in[3], conv_buf, s0.astype(F32), lp)
    y_b, win_new = nsa_sample(q, gates, kv6, cache_kv_l, page_table, cache_win_l, lp)
    y = layer_tail(x, y_a, y_b, gate_a, gate_b, lp)
    return y, kv6[:, :, :4], win_new, s_new.astype(s0.dtype), conv_new.astype(conv_buf.dtype)


def setup_inputs(seed: int = 0) -> dict:
    key = jax.random.key(seed)
    ks = jax.random.split(key, 32)
    n_pages = PAST_LEN // PAGE_SIZE
    n_used = DEC_BATCH * n_pages
    n_phys = n_used + max(1, n_used // 4)
    win_buf = min(WINDOW, PAST_LEN)

    def nrm(k, shape, scale):
        return scale * jax.random.normal(k, shape, F32)

    page_table = jax.random.permutation(ks[3], n_phys)[:n_used].reshape(DEC_BATCH, n_pages).astype(jnp.int32)
    return {
        'x_prompt': nrm(ks[0], (BATCH, SEQ, D_MODEL), 1.0),
        'x_sample': nrm(ks[1], (DEC_BATCH, DEC_SEQ, D_MODEL), 1.0),
        'cache_kv': nrm(ks[2], (DEPTH, n_phys, PAGE_SIZE, 4, NSA_KV_HEADS, NSA_HD), 1.0),
        'page_table': page_table,
        'cache_win': nrm(ks[4], (DEPTH, DEC_BATCH, win_buf, 2, NSA_KV_HEADS, NSA_HD), 1.0),
        'state_gdn': nrm(ks[5], (DEPTH, DEC_BATCH, GDN_HEADS, GDN_DK, GDN_DV), 0.5),
        'state_conv': nrm(ks[6], (DEPTH, DEC_BATCH, GDN_CONV - 1, GDN_CONV_DIM), 1.0),
        'norm1_g': 1.0 + nrm(ks[7], (DEPTH, D_MODEL), 0.05),
        'w_in': nrm(ks[8], (DEPTH, D_MODEL, PROJ_DIM), D_MODEL ** -0.5),
        'gdn_conv_w': nrm(ks[9], (DEPTH, GDN_CONV, GDN_CONV_DIM), 0.5),
        'gdn_a_log': jnp.log(jax.random.uniform(ks[10], (DEPTH, GDN_HEADS), F32, 1.0, 16.0)),
        'gdn_dt_bias': -2.0 + nrm(ks[11], (DEPTH, GDN_HEADS), 0.5),
        'gdn_norm_g': 1.0 + nrm(ks[12], (DEPTH, GDN_DV), 0.05),
        'cmp_pos_wk': (1.0 + nrm(ks[13], (DEPTH, CMP_LEN), 0.1)) / CMP_LEN,
        'cmp_pos_wv': (1.0 + nrm(ks[14], (DEPTH, CMP_LEN), 0.1)) / CMP_LEN,
        'cmp_wk': nrm(ks[15], (DEPTH, NSA_HD, NSA_HD), 2.0 * NSA_HD ** -0.5),
        'cmp_wv': nrm(ks[16], (DEPTH, NSA_HD, NSA_HD), 2.0 * NSA_HD ** -0.5),
        'w_branch_a': nrm(ks[17], (DEPTH, GDN_V, D_MODEL), GDN_V ** -0.5),
        'w_branch_b': nrm(ks[18], (DEPTH, NSA_Q, D_MODEL), NSA_Q ** -0.5),
        'w_out': nrm(ks[19], (DEPTH, D_MODEL, D_MODEL), D_MODEL ** -0.5),
        'norm2_g': 1.0 + nrm(ks[20], (DEPTH, D_MODEL), 0.05),
        'w_grp': nrm(ks[21], (DEPTH, D_MODEL, N_GROUPS), D_MODEL ** -0.5),
        'b_grp': nrm(ks[22], (DEPTH, N_GROUPS), 0.01),
        'w_rt': nrm(ks[23], (DEPTH, D_MODEL, N_EXPERTS), D_MODEL ** -0.5),
        'b_rt': nrm(ks[24], (DEPTH, N_EXPERTS), 0.01),
        'w_e_gate': nrm(ks[25], (DEPTH, N_EXPERTS, D_MODEL, D_EXPERT), D_MODEL ** -0.5),
        'w_e_up': nrm(ks[26], (DEPTH, N_EXPERTS, D_MODEL, D_EXPERT), D_MODEL ** -0.5),
        'w_e_down': nrm(ks[27], (DEPTH, N_EXPERTS, D_EXPERT, D_MODEL), D_EXPERT ** -0.5),
        'norm_f_g': 1.0 + nrm(ks[28], (D_MODEL,), 0.05),
    }


def reference(x_prompt, x_sample, cache_kv, page_table, cache_win, state_gdn, state_conv,
              norm1_g, w_in, gdn_conv_w, gdn_a_log, gdn_dt_bias, gdn_norm_g,
              cmp_pos_wk, cmp_pos_wv, cmp_wk, cmp_wv,
              w_branch_a, w_branch_b, w_out, norm2_g,
              w_grp, b_grp, w_rt, b_rt, w_e_gate, w_e_up, w_e_down, norm_f_g):
    win_buf = cache_win.shape[2]
    hp, hs = x_prompt, x_sample
    outs = [[] for _ in range(8)]
    for l in range(DEPTH):
        lp = {
            'norm1_g': norm1_g[l], 'w_in': w_in[l],
            'gdn_conv_w': gdn_conv_w[l], 'gdn_a_log': gdn_a_log[l], 'gdn_dt_bias': gdn_dt_bias[l],
            'gdn_norm_g': gdn_norm_g[l],
            'cmp_pos_wk': cmp_pos_wk[l], 'cmp_pos_wv': cmp_pos_wv[l], 'cmp_wk': cmp_wk[l], 'cmp_wv': cmp_wv[l],
            'w_branch_a': w_branch_a[l], 'w_branch_b': w_branch_b[l], 'w_out': w_out[l], 'norm2_g': norm2_g[l],
            'w_grp': w_grp[l], 'b_grp': b_grp[l], 'w_rt': w_rt[l], 'b_rt': b_rt[l],
            'w_e_gate': w_e_gate[l], 'w_e_up': w_e_up[l], 'w_e_down': w_e_down[l],
        }
        hp, kvp, winp, sp, cp = prompt_layer(hp, lp, win_buf)
        hs, kvs, wins, ss, cs = sample_layer(hs, cache_kv[l], page_table, cache_win[l], state_gdn[l], state_conv[l], lp)
        for lst, val in zip(outs, (kvp, kvs, winp, wins, sp, ss, cp, cs)):
            lst.append(val)
    kv_prompt, kv_sample, win_prompt, win_sample, gdn_prompt, gdn_sample, conv_prompt, conv_sample = [jnp.stack(v) for v in outs]
    y_prompt = rmsnorm(hp, norm_f_g)
    y_sample = rmsnorm(hs, norm_f_g)
    return (y_prompt, y_sample, kv_prompt, kv_sample, win_prompt, win_sample, gdn_prompt, gdn_sample, conv_prompt, conv_sample)
```

```python
import numpy as np
from contextlib import ExitStack
import concourse.bass as bass
import concourse.mybir as mybir
from concourse.alu_op_type import AluOpType as ALU
from concourse.bass_utils import run_bass_kernel_spmd

F32 = mybir.dt.float32
BF16 = mybir.dt.bfloat16
I32 = mybir.dt.int32
AF = mybir.ActivationFunctionType
AX = mybir.AxisListType

D = 1024
KC = D // 128
EPS = 1e-6
NCORES = 8
NEXP = 32
DEXP = 256

O_QKV, O_Z, O_B, O_A, O_QN, O_KV, O_GN, O_GA, O_GB = 0, 1536, 2048, 2052, 2056, 2568, 3336, 3360, 4384

N_DMA_SEMS = 40


class Prog:
    def __init__(self, nc, stack):
        self.nc = nc
        self.stack = stack
        self.q = {e: [] for e in ("pe", "dve", "act", "pool", "sp")}
        self.sems = {}
        self.cnt = {}
        self.waited = {}
        self.lastw = {}
        self.readers = {}
        self.dma_rr = 0
        self.dma_rr_g = 0

    def sem(self, name):
        if name not in self.sems:
            self.sems[name] = self.stack.enter_context(self.nc.semaphore(name))
            self.cnt[name] = 0
        return self.sems[name]

    def _auto(self, r, w):
        deps = []
        for k in r:
            if k in self.lastw:
                deps.append(self.lastw[k])
        for k in w:
            if k in self.lastw:
                deps.append(self.lastw[k])
            deps.extend(self.readers.get(k, ()))
        return deps

    def _commit(self, ev, r, w):
        for k in r:
            self.readers.setdefault(k, []).append(ev)
        for k in w:
            self.lastw[k] = ev
            self.readers[k] = []

    def _waits(self, eng, deps):
        best = {}
        for d in deps:
            if d is None:
                continue
            name, val = d
            if eng == "pe" and name == "E_pe":
                continue
            if val > best.get(name, 0):
                best[name] = val
        out = []
        for name, val in best.items():
            if self.waited.get((eng, name), 0) >= val:
                continue
            self.waited[(eng, name)] = val
            out.append((self.sem(name), val))
        return out

    def op(self, eng, fn, r=(), w=(), deps=()):
        waits = self._waits(eng, list(deps) + self._auto(r, w))
        name = "E_" + eng
        s = self.sem(name)
        self.cnt[name] += 1
        val = self.cnt[name]

        def thunk(e):
            for (sm, v) in waits:
                e.wait_ge(sm, v)
            fn(e).then_inc(s, 1)

        self.q[eng].append(thunk)
        ev = (name, val)
        self._commit(ev, r, w)
        return ev

    def dma(self, eng, out, in_, r=(), w=(), deps=(), **kw):
        if eng == "pool":
            chan = f"QG{self.dma_rr_g % 16}"
            self.dma_rr_g += 1
        else:
            chan = f"Q{self.dma_rr % N_DMA_SEMS}"
            self.dma_rr += 1
        s = self.sem(chan)
        prev = (chan, self.cnt[chan]) if self.cnt[chan] > 0 else None
        waits = self._waits(eng, list(deps) + self._auto(r, w) + [prev])
        self.cnt[chan] += 16
        val = self.cnt[chan]

        def thunk(e):
            for (sm, v) in waits:
                e.wait_ge(sm, v)
            e.dma_start(out=out, in_=in_, **kw).then_inc(s, 16)

        self.q[eng].append(thunk)
        ev = (chan, val)
        self._commit(ev, r, w)
        return ev

    def idma(self, out, in_, idx_ap, r=(), w=(), deps=()):
        eng = "pool"
        chan = f"QG{self.dma_rr_g % 16}"
        self.dma_rr_g += 1
        s = self.sem(chan)
        prev = (chan, self.cnt[chan]) if self.cnt[chan] > 0 else None
        waits = self._waits(eng, list(deps) + self._auto(r, w) + [prev])
        self.cnt[chan] += 16
        val = self.cnt[chan]

        def thunk(e):
            for (sm, v) in waits:
                e.wait_ge(sm, v)
            e.indirect_dma_start(out=out, out_offset=None, in_=in_,
                                 in_offset=bass.IndirectOffsetOnAxis(ap=idx_ap, axis=0)).then_inc(s, 16)

        self.q[eng].append(thunk)
        ev = (chan, val)
        self._commit(ev, r, w)
        return ev

    def barrier(self):
        deps = [(n, c) for n, c in self.cnt.items() if c > 0]
        for eng in ("pe", "dve", "act", "pool", "sp"):
            waits = self._waits(eng, [d for d in deps if d[0] != "E_" + eng])

            def thunk(e, waits=waits):
                for (sm, v) in waits:
                    e.wait_ge(sm, v)

            self.q[eng].append(thunk)

    def finish(self, eng="sp"):
        deps = [(n, c) for n, c in self.cnt.items() if n.startswith("Q") and c > 0]
        waits = self._waits(eng, deps)

        def thunk(e):
            for (sm, v) in waits:
                e.wait_ge(sm, v)

        self.q[eng].append(thunk)

    def run(self):
        nc = self.nc
        self.barrier()
        qs = self.q
        self.q = {e: [] for e in ("pe", "dve", "act", "pool", "sp")}
        self._run(nc, qs)

    def _run(self, nc, q):
        self_q = q
        with nc.Block() as block:
            @block.tensor
            def _(e):
                for f in self_q["pe"]:
                    f(e)

            @block.vector
            def _(e):
                for f in self_q["dve"]:
                    f(e)

            @block.scalar
            def _(e):
                for f in self_q["act"]:
                    f(e)

            @block.gpsimd
            def _(e):
                for f in self_q["pool"]:
                    f(e)

            @block.sync
            def _(e):
                for f in self_q["sp"]:
                    f(e)


def build_nc(L=8192, NSEQ=4, TS=8, WINB=512, TPP=8, phases=("proj", "gdn", "nsa", "nsas", "tail"), PAST=16384, NPHYS=5120):
    NT = L // 128
    LH = L // 2
    NTH = LH // 128
    NSTOK = NSEQ * TS
    NP = 768 + 4 + 384
    P_QKV, P_BA, P_KV = 0, 768, 772
    NS = 1536 + 8 + 768
    S_QKV, S_BA, S_KV = 0, 1536, 1544
    NWT = min(WINB, L) // 128
    NTT = NTH + 1

    nc = bass.Bass("TRN2", target_bir_lowering=False)
    dt = nc.dram_tensor
    xp = dt("xp", [L, D], F32, kind="ExternalInput").ap()
    xh = dt("xh", [LH, D], F32, kind="ExternalInput").ap()
    xs = dt("xs", [NSTOK, D], F32, kind="ExternalInput").ap()
    g1 = dt("g1", [128, KC], F32, kind="ExternalInput").ap()
    g2 = dt("g2", [128, KC], F32, kind="ExternalInput").ap()
    gf = dt("gf", [1, D], F32, kind="ExternalInput").ap()
    wp = dt("wp", [D, NP], F32, kind="ExternalInput").ap()
    ws = dt("ws", [D, NS], F32, kind="ExternalInput").ap()
    wgab = dt("wgab", [D, 2048], F32, kind="ExternalInput").ap()
    wa = dt("wa", [512, D], F32, kind="ExternalInput").ap()
    wb = dt("wb", [512, D], F32, kind="ExternalInput").ap()
    wo = dt("wo", [D, D], F32, kind="ExternalInput").ap()
    wr = dt("wr", [D, 36], F32, kind="ExternalInput").ap()
    br = dt("br", [1, 36], F32, kind="ExternalInput").ap()
    weg = dt("weg", [NEXP, D, DEXP], F32, kind="ExternalInput").ap()
    weu = dt("weu", [NEXP, D, DEXP], F32, kind="ExternalInput").ap()
    wed = dt("wed", [NEXP, DEXP, D], F32, kind="ExternalInput").ap()
    cwin = dt("cwin", [NSEQ, WINB, 256], F32, kind="ExternalInput").ap()
    ident_d = dt("ident", [128, 128], F32, kind="ExternalInput").ap()
    selv = dt("selv", [128, 2], F32, kind="ExternalInput").ap()
    wz = dt("wz", [D, 512], F32, kind="ExternalInput").ap()
    cwd = dt("cw", [128, 12, 4], F32, kind="ExternalInput").ap()
    gvec = dt("gvec", [1, 4 + 4 + 128], F32, kind="ExternalInput").ap()
    cmask_d = dt("cmask", [128, 3, 128], F32, kind="ExternalInput").ap()
    NCT = max(1, (8 * NT + 127) // 128)
    NSB = 2 * NT
    wn = dt("wn", [D, 536], F32, kind="ExternalInput").ap()
    qaug_d = dt("qaug", [2, 4, 4, L], F32, kind="ExternalInput").ap()
    kaug_d = dt("kaug", [4, L], F32, kind="ExternalInput").ap()
    caug_d = dt("caug", [4, NCT * 128], F32, kind="ExternalInput").ap()
    ov_d = dt("ovt", [128, NCT, NSB], F32, kind="ExternalInput").ap()
    cmsk_d = dt("cmsk", [128, 17, 128], F32, kind="ExternalInput").ap()
    tab_d = dt("tab", [128, 2, 2 * NSB], F32, kind="ExternalInput").ap()
    tri2_d = dt("tri2", [128, 2, 128], F32, kind="ExternalInput").ap()
    expt_d = dt("expt", [128, NT, 128], F32, kind="ExternalInput").ap()
    pw_d = dt("pwm", [128, 4, 8], F32, kind="ExternalInput").ap()
    wc_d = dt("wcmp", [64, 2, 64], F32, kind="ExternalInput").ap()
    NPG = PAST // 128
    NKT = NPG + 1
    NCTS = (8 * NPG + 127) // 128
    NSBS = 2 * NPG + 1
    ckv = dt("ckv", [NPHYS * 128, 512], F32, kind="ExternalInput").ap()
    ptab = dt("ptab", [NSEQ, NPG], I32, kind="ExternalInput").ap()
    piota = dt("piota", [128, 1], F32, kind="ExternalInput").ap()
    cwin4 = dt("cwin4", [NSEQ, 4, 128, 256], F32, kind="ExternalInput").ap()
    qaugs_d = dt("qaugs", [2, 4, 4, TS], F32, kind="ExternalInput").ap()
    kaugs_d = dt("kaugs", [4, NKT * 128], F32, kind="ExternalInput").ap()
    caugs_d = dt("caugs", [4, NCTS * 128], F32, kind="ExternalInput").ap()
    ovs_d = dt("ovs", [128, NCTS, NSBS], F32, kind="ExternalInput").ap()
    expts_d = dt("expts", [128, 64, 128], F32, kind="ExternalInput").ap()
    sgdn = dt("sgdn", [NSEQ * 4, 128, 128], F32, kind="ExternalInput").ap()
    sconv = dt("sconv", [NSEQ, 3, 1536], F32, kind="ExternalInput").ap()

    kvp = dt("kvp", [L, 4, 64], F32, kind="ExternalOutput").ap()
    winp = dt("winp", [NWT * 128, 2, 64], F32, kind="ExternalOutput").ap()
    convp = dt("convp", [3, 768], F32, kind="ExternalOutput").ap()
    kvs = dt("kvs", [NSTOK, 512], F32, kind="ExternalOutput").ap()
    wins = dt("wins", [NSEQ, WINB, 256], F32, kind="ExternalOutput").ap()
    convs = dt("convs", [NSEQ, 3, 1536], F32, kind="ExternalOutput").ap()
    yp = dt("yp", [LH, D], F32, kind="ExternalOutput").ap()
    ysm = dt("ysm", [NSTOK, D], F32, kind="ExternalOutput").ap()
    gdnp = dt("gdnp", [4, 128, 128], F32, kind="ExternalOutput").ap()
    gdns = dt("gdns", [NSEQ * 4, 128, 128], F32, kind="ExternalOutput").ap()

    qs_scr = dt("qs_scr", [128, 12, L + NSTOK], F32, kind="Internal").ap()
    zs_scr = dt("zs_scr", [L + NSTOK, 512], F32, kind="Internal").ap()
    bg_scr = dt("bg_scr", [L + NSTOK, 8], F32, kind="Internal").ap()
    yaT_scr = dt("yaT_scr", [128, 4, L + NSTOK], BF16, kind="Internal").ap()
    kvtm_scr = dt("kvtm_scr", [L, 768], BF16, kind="Internal").ap()
    kT_scr = dt("kT_scr", [4, 64, L], BF16, kind="Internal").ap()
    qT_scr = dt("qT_scr", [2, 64, 4, L], BF16, kind="Internal").ap()
    gates_scr = dt("gates_scr", [L, 24], F32, kind="Internal").ap()
    ybT_scr = dt("ybT_scr", [128, 4, L + NSTOK], BF16, kind="Internal").ap()
    qTs_scr = dt("qTs_scr", [2, 64, 4, NSTOK], BF16, kind="Internal").ap()
    gates_s_scr = dt("gates_s_scr", [NSTOK, 24], F32, kind="Internal").ap()
    kvs_scr = dt("kvs_scr", [NSTOK, 768], F32, kind="Internal").ap()
    h_scr = dt("h_scr", [NTT, 128, D], F32, kind="Internal").ap()
    hnT_scr = dt("hnT_scr", [128, KC, NTT * 128], BF16, kind="Internal").ap()

    with ExitStack() as st:
        def sbuf(stack, name, shape, dtype):
            return stack.enter_context(nc.sbuf_tensor(name, shape, dtype))

        def psum(stack, name, shape, dtype):
            return stack.enter_context(nc.psum_tensor(name, shape, dtype))

        P = Prog(nc, st)

        ident = sbuf(st, "ident_sb", [128, 128], F32)
        g1T = sbuf(st, "g1T", [128, KC], F32)
        g2T = sbuf(st, "g2T", [128, KC], F32)
        epsc = sbuf(st, "epsc", [128, 1], F32)
        comb = sbuf(st, "comb", [128, NTT, NEXP], F32)
        onec = sbuf(st, "onec", [128, 1], F32)
        P.op("dve", lambda e: e.memset(onec[:], 1.0), w=["onec"])
        P.op("dve", lambda e: e.memset(epsc[:], EPS), w=["epsc"])
        P.dma("sp", ident[:], ident_d, w=["ident"])
        P.dma("sp", g1T[:], g1, w=["g1T"])
        P.dma("sp", g2T[:], g2, w=["g2T"])

        if "gdn" not in phases:
            zt = sbuf(st, "zt", [128, 128], F32)
            P.op("dve", lambda e: e.memset(zt[:], 0.0), w=["zt"])
            for hh in range(4):
                P.dma("sp", gdnp[hh], zt[:], r=["zt"])
            for hh in range(NSEQ * 4):
                P.dma("sp", gdns[hh], zt[:], r=["zt"])

        pA = [psum(st, f"pA{j}", [128, 1024], F32) for j in range(2)]
        pB = [psum(st, f"pB{j}", [128, 512], F32) for j in range(4)]

        def rms_stats(x_ap, npart, junk, ss, rstd, kx, tag):
            P.op("act", lambda e: e.activation(out=junk[:npart, :], in_=x_ap, func=AF.Square,
                                               accum_out=ss[:npart, :]), r=[kx], w=["junk" + tag, "ss" + tag])
            P.op("act", lambda e: e.activation(out=rstd[:npart, :], in_=ss[:npart, :], func=AF.Sqrt,
                                               bias=epsc[:npart, :], scale=1.0 / D),
                 r=["ss" + tag, "epsc"], w=["rstd" + tag])
            P.op("dve", lambda e: e.reciprocal(out=rstd[:npart, :], in_=rstd[:npart, :]),
                 r=["rstd" + tag], w=["rstd" + tag])

        def norm_transpose(x_ap, kx, npart, junk, ss, rstd, xn, pT, kpT, gT, kg, outs, tag):
            rms_stats(x_ap, npart, junk, ss, rstd, kx, tag)
            P.op("dve", lambda e: e.tensor_scalar(out=xn[:npart, :], in0=x_ap, scalar1=rstd[:npart, 0:1], scalar2=1.0,
                                                  op0=ALU.mult, op1=ALU.mult), r=[kx, "rstd" + tag], w=["xn" + tag])
            for kc in range(KC):
                P.op("pe", lambda e, kc=kc: e.transpose(out=pT[:, kc * 128:kc * 128 + npart],
                                                        in_=xn[:npart, kc * 128:(kc + 1) * 128],
                                                        identity=ident[:npart, :npart]),
                     r=["xn" + tag, "ident"], w=[kpT])
            (o0, k0) = outs[0]
            P.op("dve", lambda e: e.tensor_tensor(
                out=o0[:, :, :npart],
                in0=pT[:].rearrange("p (kc t) -> p kc t", kc=KC)[:, :, :npart],
                in1=gT[:].unsqueeze(2).to_broadcast([128, KC, npart]),
                op=ALU.mult), r=[kpT, kg], w=[k0])
            for (o1, k1) in outs[1:]:
                P.op("act", lambda e, o1=o1: e.copy(out=o1[:, :, :npart], in_=o0[:, :, :npart]), r=[k0], w=[k1])

        def phase_proj():
            with ExitStack() as s1:
                wpb = sbuf(s1, "wpb", [128, KC, NP], BF16)
                wsb = sbuf(s1, "wsb", [128, KC, NS], BF16)
                xt = [sbuf(s1, f"xt{j}", [128, D], F32) for j in range(2)]
                junk = sbuf(s1, "junk", [128, D], F32)
                ss = [sbuf(s1, f"ss{j}", [128, 1], F32) for j in range(2)]
                rstd = [sbuf(s1, f"rstd{j}", [128, 1], F32) for j in range(2)]
                xn = [sbuf(s1, f"xn{j}", [128, D], F32) for j in range(2)]
                xnT = [sbuf(s1, f"xnT{j}", [128, KC, 128], BF16) for j in range(2)]
                kvsb = [sbuf(s1, f"kvsb{j}", [128, 384], F32) for j in range(2)]
                qkvtm = sbuf(s1, "qkvtm", [128, 768], F32)
                souts = sbuf(s1, "souts", [NSTOK, NS], F32)
                P.dma("pool", wpb[:], wp.rearrange("(kc p) n -> p kc n", p=128), w=["wpb"])
                P.dma("pool", wsb[:], ws.rearrange("(kc p) n -> p kc n", p=128), w=["wsb"])
                GDN = "gdn" in phases
                if GDN:
                    wzb = sbuf(s1, "wzb", [128, KC, 512], BF16)
                    cw = sbuf(s1, "cw_sb", [128, 12, 4], F32)
                    gv = sbuf(s1, "gv_sb", [128, 136], F32)
                    nA = sbuf(s1, "nA", [128, 4], F32)
                    pre = sbuf(s1, "pre", [128, 12, 131], F32)
                    cv = sbuf(s1, "cv", [128, 12, 128], F32)
                    cv2 = sbuf(s1, "cv2", [128, 12, 128], F32)
                    qs = sbuf(s1, "qs1", [128, 12, 128], F32)
                    zs = sbuf(s1, "zs1", [128, 512], F32)
                    bg = sbuf(s1, "bg1", [128, 8], F32)
                    tsm = sbuf(s1, "tsm1", [128, 8], F32)
                    P.dma("pool", wzb[:], wz.rearrange("(kc p) n -> p kc n", p=128), w=["wzb"])
                    P.dma("sp", cw[:], cwd, w=["cw"])
                    P.dma("sp", gv[:], gvec[0].partition_broadcast(128), w=["gv"])
                    P.op("act", lambda e: e.activation(out=nA[:], in_=gv[:, 0:4], func=AF.Exp), r=["gv"], w=["nA"])
                    P.op("dve", lambda e: e.tensor_scalar(out=nA[:], in0=nA[:], scalar1=-1.0, scalar2=None, op0=ALU.mult),
                         r=["nA"], w=["nA"])

                def gdn_pre(xnT_ap, kxnT, C, tok0, halo_fn):
                    for fc in range(12):
                        dst = pA[1][:, fc * 128:fc * 128 + C] if fc < 8 else pB[2][:, (fc - 8) * 128:(fc - 8) * 128 + C]
                        kd = "pA1" if fc < 8 else "pB2"
                        for kc in range(KC):
                            P.op("pe", lambda e, kc=kc, dst=dst, fc=fc: e.matmul(
                                out=dst, lhsT=wsb[:, kc, S_QKV + fc * 128:S_QKV + (fc + 1) * 128], rhs=xnT_ap[:, kc, :C],
                                start=(kc == 0), stop=(kc == KC - 1)), r=[kxnT, "wsb"], w=[kd])
                    halo_fn()
                    P.op("act", lambda e: e.copy(out=pre[:, 0:8, 3:3 + C],
                                                 in_=pA[1][:].rearrange("p (c t) -> p c t", c=8)[:, :, :C]),
                         r=["pA1"], w=["pre_a"])
                    P.op("act", lambda e: e.copy(out=pre[:, 8:12, 3:3 + C],
                                                 in_=pB[2][:].rearrange("p (c t) -> p c t", c=4)[:, :, :C]),
                         r=["pB2"], w=["pre_b"])
                    pk = ["pre_a", "pre_b", "pre_h"]
                    P.op("pool", lambda e: e.tensor_tensor(out=cv[:, :, :C], in0=pre[:, :, 0:C],
                                                           in1=cw[:, :, 0:1].to_broadcast([128, 12, C]), op=ALU.mult),
                         r=pk + ["cw"], w=["cv"])
                    P.op("pool", lambda e: e.tensor_tensor(out=cv2[:, :, :C], in0=pre[:, :, 1:1 + C],
                                                           in1=cw[:, :, 1:2].to_broadcast([128, 12, C]), op=ALU.mult),
                         r=pk + ["cw"], w=["cv2"])
                    P.op("dve", lambda e: e.tensor_tensor(out=cv[:, :, :C], in0=cv[:, :, :C], in1=cv2[:, :, :C], op=ALU.add),
                         r=["cv", "cv2"], w=["cv"])
                    P.op("pool", lambda e: e.tensor_tensor(out=cv2[:, :, :C], in0=pre[:, :, 2:2 + C],
                                                           in1=cw[:, :, 2:3].to_broadcast([128, 12, C]), op=ALU.mult),
                         r=pk + ["cw"], w=["cv2"])
                    P.op("dve", lambda e: e.tensor_tensor(out=cv[:, :, :C], in0=cv[:, :, :C], in1=cv2[:, :, :C], op=ALU.add),
                         r=["cv", "cv2"], w=["cv"])
                    P.op("pool", lambda e: e.tensor_tensor(out=cv2[:, :, :C], in0=pre[:, :, 3:3 + C],
                                                           in1=cw[:, :, 3:4].to_broadcast([128, 12, C]), op=ALU.mult),
                         r=pk + ["cw"], w=["cv2"])
                    P.op("dve", lambda e: e.tensor_tensor(out=cv[:, :, :C], in0=cv[:, :, :C], in1=cv2[:, :, :C], op=ALU.add),
                         r=["cv", "cv2"], w=["cv"])
                    P.op("act", lambda e: e.activation(out=qs[:, :, :C], in_=cv[:, :, :C], func=AF.Silu), r=["cv"], w=["qs1"])
                    P.dma("sp", qs_scr[:, :, tok0:tok0 + C], qs[:, :, :C], r=["qs1"], w=[f"qs_scr{tok0}"])

                def gdn_zg(xnT_ap, kxnT, n, tok0):
                    for kc in range(KC):
                        P.op("pe", lambda e, kc=kc: e.matmul(out=pB[3][:n, :], lhsT=xnT_ap[:, kc, :n], rhs=wzb[:, kc, :],
                                                             start=(kc == 0), stop=(kc == KC - 1)), r=[kxnT, "wzb"], w=["pB3"])
                    P.op("act", lambda e: e.activation(out=zs[:n, :], in_=pB[3][:n, :], func=AF.Silu), r=["pB3"], w=["zs1"])
                    P.dma("sp", zs_scr[tok0:tok0 + n, :], zs[:n, :], r=["zs1"], w=[f"zs_scr{tok0}"])
                    for kc in range(KC):
                        P.op("pe", lambda e, kc=kc: e.matmul(out=pB[1][:n, 0:8], lhsT=xnT_ap[:, kc, :n],
                                                             rhs=wsb[:, kc, S_BA:S_BA + 8],
                                                             start=(kc == 0), stop=(kc == KC - 1)), r=[kxnT, "wsb"], w=["pB1"])
                    P.op("act", lambda e: e.activation(out=bg[:n, 0:4], in_=pB[1][:n, 0:4], func=AF.Sigmoid),
                         r=["pB1"], w=["bg1"])
                    P.op("dve", lambda e: e.tensor_tensor(out=tsm[:n, 0:4], in0=pB[1][:n, 4:8], in1=gv[:n, 4:8], op=ALU.add),
                         r=["pB1", "gv"], w=["tsm1"])
                    P.op("act", lambda e: e.activation(out=tsm[:n, 0:4], in_=tsm[:n, 0:4], func=AF.Exp), r=["tsm1"], w=["tsm1"])
                    P.op("act", lambda e: e.activation(out=tsm[:n, 0:4], in_=tsm[:n, 0:4], func=AF.Ln, bias=onec[:n, :]),
                         r=["tsm1", "onec"], w=["tsm1"])
                    P.op("dve", lambda e: e.tensor_tensor(out=bg[:n, 4:8], in0=tsm[:n, 0:4], in1=nA[:n, :], op=ALU.mult),
                         r=["tsm1", "nA", "bg1"], w=["bg1"])
                    P.dma("sp", bg_scr[tok0:tok0 + n, :], bg[:n, :], r=["bg1"], w=[f"bg_scr{tok0}"])

                NSA = "nsa" in phases
                if NSA or "nsas" in phases:
                    wnb = sbuf(s1, "wnb", [128, KC, 536], BF16)
                    kvb = sbuf(s1, "kvb", [128, 768], BF16)
                    kTb = sbuf(s1, "kTb", [64, 4, 128], BF16)
                    qTb = sbuf(s1, "qTb", [64, 8, 128], BF16)
                    gts = sbuf(s1, "gts", [128, 24], F32)
                    P.dma("pool", wnb[:], wn.rearrange("(kc p) n -> p kc n", p=128), w=["wnb"])

                def nsa_pre(i, xT, kxT):
                    t0 = i * 128
                    for half in range(2):
                        for kc in range(KC):
                            P.op("pe", lambda e, kc=kc, half=half: e.matmul(
                                out=pA[1][:, half * 512:half * 512 + 384], lhsT=xT[:, kc, :],
                                rhs=wsb[:, kc, S_KV + half * 384:S_KV + (half + 1) * 384],
                                start=(kc == 0), stop=(kc == KC - 1)), r=[kxT, "wsb"], w=["pA1"])
                    for half in range(2):
                        P.op("act", lambda e, half=half: e.copy(out=kvb[:, half * 384:(half + 1) * 384],
                                                                in_=pA[1][:, half * 512:half * 512 + 384]), r=["pA1"], w=["kvb"])
                    P.dma("act", kvtm_scr[t0:t0 + 128, :], kvb[:], r=["kvb"], w=[f"kvtm{i}"])
                    for idx in range(4):
                        g_, j_ = idx // 2, (2, 4)[idx % 2]
                        c0 = S_KV + j_ * 128 + g_ * 64
                        for kc in range(KC):
                            P.op("pe", lambda e, kc=kc, idx=idx, c0=c0: e.matmul(
                                out=pB[2][0:64, idx * 128:(idx + 1) * 128], lhsT=wsb[:, kc, c0:c0 + 64], rhs=xT[:, kc, :],
                                start=(kc == 0), stop=(kc == KC - 1)), r=[kxT, "wsb"], w=["pB2"])
                    P.op("act", lambda e: e.copy(out=kTb[:], in_=pB[2][0:64, :].rearrange("p (a t) -> p a t", a=4)),
                         r=["pB2"], w=["kTb"])
                    P.dma("act", kT_scr[:, :, t0:t0 + 128].rearrange("a p t -> p a t"), kTb[:], r=["kTb"], w=[f"kTs{i}"])
                    for hh in range(8):
                        for kc in range(KC):
                            P.op("pe", lambda e, kc=kc, hh=hh: e.matmul(
                                out=pA[0][0:64, hh * 128:(hh + 1) * 128], lhsT=wnb[:, kc, hh * 64:(hh + 1) * 64], rhs=xT[:, kc, :],
                                start=(kc == 0), stop=(kc == KC - 1)), r=[kxT, "wnb"], w=["pA0"])
                    P.op("act", lambda e: e.activation(out=qTb[:], in_=pA[0][0:64, :].rearrange("p (a t) -> p a t", a=8),
                                                       func=AF.Copy, scale=0.125), r=["pA0"], w=["qTb"])
                    for g_ in range(2):
                        P.dma("act", qT_scr[g_, :, :, t0:t0 + 128], qTb[:, 4 * g_:4 * g_ + 4, :], r=["qTb"], w=[f"qTs{i}"])
                    for kc in range(KC):
                        P.op("pe", lambda e, kc=kc: e.matmul(out=pB[3][:, 0:24], lhsT=xT[:, kc, :], rhs=wnb[:, kc, 512:536],
                                                             start=(kc == 0), stop=(kc == KC - 1)), r=[kxT, "wnb"], w=["pB3"])
                    P.op("act", lambda e: e.activation(out=gts[:], in_=pB[3][:, 0:24], func=AF.Sigmoid), r=["pB3"], w=["gts"])
                    P.dma("act", gates_scr[t0:t0 + 128, :], gts[:], r=["gts"], w=[f"gates{i}"])

                def prompt_tile(i):
                    sl = i % 2
                    t = str(sl)
                    P.dma("sp", xt[sl][:], xp[i * 128:(i + 1) * 128, :], w=["xt" + t])
                    norm_transpose(xt[sl][:], "xt" + t, 128, junk, ss[sl], rstd[sl], xn[sl], pA[sl], f"pA{sl}",
                                   g1T, "g1T", [(xnT[sl], "xnT" + t)], "p1" + t)
                    for kc in range(KC):
                        P.op("pe", lambda e, kc=kc: e.matmul(out=pB[sl][:, 0:384], lhsT=xnT[sl][:, kc, :],
                                                             rhs=wpb[:, kc, P_KV:P_KV + 384],
                                                             start=(kc == 0), stop=(kc == KC - 1)),
                             r=["xnT" + t, "wpb"], w=[f"pB{sl}"])
                    P.op("act", lambda e: e.copy(out=kvsb[sl][:], in_=pB[sl][:, 0:384]), r=[f"pB{sl}"], w=["kvsb" + t])
                    P.dma("act", kvp[i * 128:(i + 1) * 128, :, :],
                          kvsb[sl][:, 0:256].rearrange("p (j d) -> p j d", j=4), r=["kvsb" + t])
                    if GDN:
                        def halo():
                            if i == 0:
                                P.op("pool", lambda e: e.memset(pre[:, :, 0:3], 0.0), w=["pre_h"])
                            else:
                                P.op("pool", lambda e: e.tensor_copy(out=pre[:, :, 0:3], in_=pre[:, :, 128:131]),
                                     r=["pre_a", "pre_b"], w=["pre_h"])
                        gdn_pre(xnT[sl], "xnT" + t, 128, i * 128, halo)
                        gdn_zg(xnT[sl], "xnT" + t, 128, i * 128)
                    if NSA:
                        nsa_pre(i, xnT[sl], "xnT" + t)
                    if i >= NT - NWT:
                        r0 = (i - (NT - NWT)) * 128
                        P.dma("act", winp[r0:r0 + 128, :, :],
                              kvsb[sl][:, 256:384].rearrange("p (j d) -> p j d", j=2), r=["kvsb" + t])
                    if i == NT - 1:
                        for (c0, c1, pm) in ((0, 512, 2), (512, 768, 3)):
                            for kc in range(KC):
                                P.op("pe", lambda e, kc=kc, c0=c0, c1=c1, pm=pm: e.matmul(
                                    out=pB[pm][:, 0:c1 - c0], lhsT=xnT[sl][:, kc, :],
                                    rhs=wpb[:, kc, P_QKV + c0:P_QKV + c1],
                                    start=(kc == 0), stop=(kc == KC - 1)), r=["xnT" + t, "wpb"], w=[f"pB{pm}"])
                            P.op("act", lambda e, c0=c0, c1=c1, pm=pm: e.copy(out=qkvtm[:, c0:c1],
                                                                             in_=pB[pm][:, 0:c1 - c0]),
                                 r=[f"pB{pm}"], w=["qkvtm"])
                        P.dma("act", convp, qkvtm[125:128, :], r=["qkvtm"])

                for i in range(NT):
                    prompt_tile(i)

                sl = NT % 2
                t = str(sl)
                P.dma("sp", xt[sl][:NSTOK, :], xs, w=["xt" + t])
                norm_transpose(xt[sl][:NSTOK, :], "xt" + t, NSTOK, junk, ss[sl], rstd[sl], xn[sl], pA[sl], f"pA{sl}",
                               g1T, "g1T", [(xnT[sl], "xnT" + t)], "p1" + t)
                nchunk = (NS + 511) // 512
                for ci in range(nchunk):
                    c0, c1 = ci * 512, min(NS, ci * 512 + 512)
                    pm = ci % 4
                    for kc in range(KC):
                        P.op("pe", lambda e, kc=kc, c0=c0, c1=c1, pm=pm: e.matmul(
                            out=pB[pm][:NSTOK, 0:c1 - c0], lhsT=xnT[sl][:, kc, :NSTOK], rhs=wsb[:, kc, c0:c1],
                            start=(kc == 0), stop=(kc == KC - 1)), r=["xnT" + t, "wsb"], w=[f"pB{pm}"])
                    P.op("act", lambda e, c0=c0, c1=c1, pm=pm: e.copy(out=souts[:, c0:c1], in_=pB[pm][:NSTOK, 0:c1 - c0]),
                         r=[f"pB{pm}"], w=["souts"])
                if GDN:
                    gdn_zg(xnT[sl], "xnT" + t, NSTOK, L)
                    for b in range(NSEQ):
                        def halo(b=b):
                            for r_ in range(3):
                                P.dma("sp", pre[:, :, r_], sconv[b, r_].rearrange("(c p) -> p c", p=128), w=["pre_h"],
                                      allow_slow_non_contiguous=True)
                        xs_b = xnT[sl][:, :, b * TS:(b + 1) * TS]
                        gdn_pre(xs_b, "xnT" + t, TS, L + b * TS, halo)
                if "nsas" in phases:
                    xT = xnT[sl]
                    for hh in range(8):
                        for kc in range(KC):
                            P.op("pe", lambda e, kc=kc, hh=hh: e.matmul(
                                out=pA[0][0:64, hh * NSTOK:(hh + 1) * NSTOK], lhsT=wnb[:, kc, hh * 64:(hh + 1) * 64], rhs=xT[:, kc, :NSTOK],
                                start=(kc == 0), stop=(kc == KC - 1)), r=["xnT" + t, "wnb"], w=["pA0"])
                    P.op("act", lambda e: e.activation(out=qTb[:, :, :NSTOK], in_=pA[0][0:64, 0:8 * NSTOK].rearrange("p (a t) -> p a t", a=8),
                                                       func=AF.Copy, scale=0.125), r=["pA0"], w=["qTb"])
                    for g_ in range(2):
                        P.dma("act", qTs_scr[g_], qTb[:, 4 * g_:4 * g_ + 4, :NSTOK], r=["qTb"], w=[f"qTss{g_}"])
                    for kc in range(KC):
                        P.op("pe", lambda e, kc=kc: e.matmul(out=pB[3][:NSTOK, 0:24], lhsT=xT[:, kc, :NSTOK], rhs=wnb[:, kc, 512:536],
                                                             start=(kc == 0), stop=(kc == KC - 1)), r=["xnT" + t, "wnb"], w=["pB3"])
                    P.op("act", lambda e: e.activation(out=gts[:NSTOK, :], in_=pB[3][:NSTOK, 0:24], func=AF.Sigmoid), r=["pB3"], w=["gts"])
                    P.dma("act", gates_s_scr, gts[:NSTOK, :], r=["gts"], w=["gatess"])
                    P.dma("act", kvs_scr, souts[:, S_KV:S_KV + 768], r=["souts"], w=["kvs_scr"])
                P.dma("act", kvs, souts[:, S_KV:S_KV + 512], r=["souts"])
                for b in range(NSEQ):
                    P.dma("sp", wins[b, 0:WINB - TS, :], cwin[b, TS:WINB, :])
                    P.dma("act", wins[b, WINB - TS:WINB, :], souts[b * TS:(b + 1) * TS, S_KV + 512:S_KV + 768],
                          r=["souts"])
                    P.dma("act", convs[b], souts[b * TS + TS - 3:(b + 1) * TS, S_QKV:S_QKV + 1536], r=["souts"])
                P.run()

        if "proj" in phases:
            phase_proj()


        def phase_gdn():
            with ExitStack() as s2:
                cm = sbuf(s2, "cm", [128, 3, 128], F32)
                gv = sbuf(s2, "gv2", [128, 136], F32)
                P.dma("sp", cm[:], cmask_d, w=["cm"])
                P.dma("sp", gv[:], gvec[0].partition_broadcast(128), w=["gv2"])
                TRI, NSTR, ONES = cm[:, 0, :], cm[:, 1, :], cm[:, 2, :]
                identb = sbuf(s2, "identb", [128, 128], BF16)
                P.op("act", lambda e: e.copy(out=identb[:], in_=ident[:]), r=["ident"], w=["identb"])
                qs = [sbuf(s2, f"qs2_{j}", [128, 12, 128], F32) for j in range(2)]
                qsb = [sbuf(s2, f"qsb_{j}", [128, 12, 128], BF16) for j in range(2)]
                zs = [sbuf(s2, f"zs2_{j}", [128, 512], F32) for j in range(2)]
                bg = [sbuf(s2, f"bg2_{j}", [128, 8], F32) for j in range(2)]
                gz = sbuf(s2, "gz", [128, 512], F32)
                sc = sbuf(s2, "sc2", [128, 64], F32)
                ya = sbuf(s2, "ya2", [128, 512], F32)
                yaT = sbuf(s2, "yaT2", [128, 4, 128], BF16)
                S = [sbuf(s2, f"S{h}", [128, 128], F32) for h in range(4)]
                Sb = [sbuf(s2, f"Sb{h}", [128, 128], BF16) for h in range(4)]
                W = {}
                for h in range(4):
                    for nm, dtp in (("kbg", BF16), ("kt", BF16), ("vb", BF16), ("E", F32), ("X1", F32), ("X2", F32),
                                    ("BT", F32), ("BTb", BF16), ("Bb", BF16), ("BTb2", BF16), ("Bb2", BF16),
                                    ("PT", F32), ("PTb", BF16), ("qkT", BF16), ("qdT", BF16), ("wcT", BF16),
                                    ("uc", F32), ("ub", BF16), ("tmp", F32)):
                        W[(nm, h)] = sbuf(s2, f"{nm}{h}", [128, 128], dtp)
                banks = [pA[0][:, 0:512], pA[0][:, 512:1024], pA[1][:, 0:512], pA[1][:, 512:1024],
                         pB[0][:], pB[1][:], pB[2][:], pB[3][:]]

                def slot(h, j):
                    return banks[2 * h + j // 4][:, (j % 4) * 128:(j % 4 + 1) * 128], f"gbank{2 * h + j // 4}"

                def gdn_chunk(C, tok0, first, li):
                    sl = li % 2
                    kq, kz, kb = f"qs2_{sl}", f"zs2_{sl}", f"bg2_{sl}"
                    P.dma("sp", qs[sl][:, :, :C], qs_scr[:, :, tok0:tok0 + C], r=[f"qs_scr{tok0}"], w=[kq])
                    P.dma("act", zs[sl][:C, :], zs_scr[tok0:tok0 + C, :], r=[f"zs_scr{tok0}"], w=[kz])
                    P.dma("act", bg[sl][:C, :], bg_scr[tok0:tok0 + C, :], r=[f"bg_scr{tok0}"], w=[kb])
                    P.op("act", lambda e: e.copy(out=qsb[sl][:, :, :C], in_=qs[sl][:, :, :C]), r=[kq], w=[f"qsb_{sl}"])
                    kqb = f"qsb_{sl}"
                    P.op("pool", lambda e: e.tensor_tensor(
                        out=gz[:C, :].rearrange("p (h d) -> p h d", h=4), in0=zs[sl][:C, :].rearrange("p (h d) -> p h d", h=4),
                        in1=gv[:C, 8:136].unsqueeze(1).to_broadcast([C, 4, 128]), op=ALU.mult), r=[kz, "gv2"], w=["gz"])
                    c0, k0 = slot(3, 7)
                    P.op("pe", lambda e: e.matmul(out=c0[:C, 0:4], lhsT=TRI[:C, :C], rhs=bg[sl][:C, 4:8], start=True, stop=True),
                         r=["cm", kb], w=[k0])
                    P.op("pe", lambda e: e.matmul(out=c0[:, 4:8], lhsT=ONES[:C, :], rhs=bg[sl][:C, 4:8], start=True, stop=True),
                         r=["cm", kb], w=[k0])
                    P.op("act", lambda e: e.copy(out=sc[:C, 0:4], in_=c0[:C, 0:4]), r=[k0], w=["sc"])
                    P.op("act", lambda e: e.copy(out=sc[:, 4:8], in_=c0[:, 4:8]), r=[k0], w=["sc"])
                    P.op("act", lambda e: e.activation(out=sc[:C, 8:12], in_=sc[:C, 0:4], func=AF.Exp), r=["sc"], w=["sc"])
                    P.op("dve", lambda e: e.tensor_tensor(out=sc[:C, 60:64], in0=sc[:C, 4:8], in1=sc[:C, 0:4], op=ALU.subtract),
                         r=["sc"], w=["sc"])
                    P.op("act", lambda e: e.activation(out=sc[:C, 12:16], in_=sc[:C, 60:64], func=AF.Exp), r=["sc"], w=["sc"])
                    P.op("act", lambda e: e.activation(out=sc[:, 16:20], in_=sc[:, 4:8], func=AF.Exp), r=["sc"], w=["sc"])
                    for h in range(4):
                        for (j, ch) in ((0, 4 + h), (1, 8 + h), (2, h)):
                            o_, ko = slot(h, j)
                            P.op("pe", lambda e, o_=o_, ch=ch: e.transpose(out=o_[:C, :], in_=qs[sl][:, ch, :C], identity=ident[:, :]),
                                 r=[kq, "ident"], w=[ko])
                        kt_, kkt = slot(h, 0)
                        qt_, kqt = slot(h, 2)
                        P.op("act", lambda e, h=h, qt_=qt_: e.activation(out=W[("tmp", h)][:C, :], in_=qt_[:C, :], func=AF.Square,
                                                                         accum_out=sc[:C, 20 + h:21 + h]), r=[kqt], w=["sc", f"tmp{h}"])
                        P.op("act", lambda e, h=h, kt_=kt_: e.activation(out=W[("tmp", h)][:C, :], in_=kt_[:C, :], func=AF.Square,
                                                                         accum_out=sc[:C, 24 + h:25 + h]), r=[kkt], w=["sc", f"tmp{h}"])
                    P.op("act", lambda e: e.activation(out=sc[:C, 28:36], in_=sc[:C, 20:28], func=AF.Sqrt, bias=epsc[:C, :], scale=1.0),
                         r=["sc", "epsc"], w=["sc"])
                    P.op("dve", lambda e: e.reciprocal(out=sc[:C, 28:36], in_=sc[:C, 28:36]), r=["sc"], w=["sc"])
                    P.op("dve", lambda e: e.tensor_scalar(out=sc[:C, 28:32], in0=sc[:C, 28:32], scalar1=float(128 ** -0.5), scalar2=None,
                                                          op0=ALU.mult), r=["sc"], w=["sc"])
                    P.op("dve", lambda e: e.tensor_tensor(out=sc[:C, 44:48], in0=sc[:C, 32:36], in1=bg[sl][:C, 0:4], op=ALU.mult),
                         r=["sc", kb], w=["sc"])
                    P.op("dve", lambda e: e.tensor_tensor(out=sc[:C, 36:40], in0=sc[:C, 44:48], in1=sc[:C, 8:12], op=ALU.mult),
                         r=["sc"], w=["sc"])
                    P.op("dve", lambda e: e.tensor_tensor(out=sc[:C, 40:44], in0=sc[:C, 32:36], in1=sc[:C, 12:16], op=ALU.mult),
                         r=["sc"], w=["sc"])
                    P.op("dve", lambda e: e.tensor_tensor(out=sc[:C, 48:52], in0=sc[:C, 28:32], in1=sc[:C, 8:12], op=ALU.mult),
                         r=["sc"], w=["sc"])
                    gens = [head(C, h, sl, kq, kqb, kb, first) for h in range(4)]
                    while gens:
                        for gen in list(gens):
                            try:
                                next(gen)
                            except StopIteration:
                                gens.remove(gen)
                    for h in range(4):
                        o_, ko = slot(h, 0)
                        P.op("pe", lambda e, o_=o_, h=h: e.transpose(out=o_[:, :C], in_=ya[:C, h * 128:(h + 1) * 128],
                                                                     identity=ident[:C, :C]), r=[f"ya{h}", "ident"], w=[ko])
                        P.op("act", lambda e, o_=o_, h=h: e.copy(out=yaT[:, h, :C], in_=o_[:, :C]), r=[ko], w=["yaT"])
                    P.dma("sp", yaT_scr[:, :, tok0:tok0 + C], yaT[:, :, :C], r=["yaT"], w=[f"yaT_scr{tok0}"])

                def head(C, h, sl, kq, kqb, kb, first):
                    w = lambda nm: (W[(nm, h)], f"{nm}{h}")
                    kbg, kkbg = w("kbg"); ktl, kktl = w("kt"); vb, kvb = w("vb"); E, kE = w("E"); X1, kX1 = w("X1"); X2, kX2 = w("X2")
                    BT, kBT = w("BT"); BTb, kBTb = w("BTb"); Bb, kBb = w("Bb"); BTb2, kBTb2 = w("BTb2"); Bb2, kBb2 = w("Bb2")
                    PT, kPT = w("PT"); PTb, kPTb = w("PTb"); qkT, kqkT = w("qkT"); qdT, kqdT = w("qdT"); wcT, kwcT = w("wcT")
                    uc, kuc = w("uc"); ub, kub = w("ub"); tmp, ktmp = w("tmp")
                    k_tm, kk_tm = slot(h, 0)
                    v_tm, kv_tm = slot(h, 1)
                    P.op("dve", lambda e: e.tensor_scalar(out=kbg[:C, :], in0=k_tm[:C, :], scalar1=sc[:C, 36 + h:37 + h], scalar2=None,
                                                          op0=ALU.mult), r=[kk_tm, "sc"], w=[kkbg])
                    yield
                    P.op("dve", lambda e: e.tensor_scalar(out=ktl[:C, :], in0=k_tm[:C, :], scalar1=sc[:C, 40 + h:41 + h], scalar2=None,
                                                          op0=ALU.mult), r=[kk_tm, "sc"], w=[kktl])
                    yield
                    P.op("dve", lambda e: e.tensor_scalar(out=vb[:C, :], in0=v_tm[:C, :], scalar1=bg[sl][:C, h:h + 1], scalar2=None,
                                                          op0=ALU.mult), r=[kv_tm, kb], w=[kvb])
                    yield
                    rows = []
                    for j, (col, rhs_) in enumerate(((bg[sl][:C, 4 + h:5 + h], TRI), (sc[:C, 44 + h:45 + h], ident),
                                                    (sc[:C, 28 + h:29 + h], ident), (sc[:C, 48 + h:49 + h], ident))):
                        o_, ko = slot(h, 4 + j)
                        P.op("dve", lambda e, col=col: e.tensor_scalar(out=tmp[:C, :], in0=ONES[:C, :], scalar1=col, scalar2=None,
                                                                       op0=ALU.mult), r=["cm", "sc", kb], w=[ktmp])
                        yield
                        P.op("pe", lambda e, o_=o_, rhs_=rhs_: e.matmul(out=o_[:, :C], lhsT=tmp[:C, :], rhs=rhs_[:C, :C],
                                                                       start=True, stop=True), r=[ktmp, "cm", "ident"], w=[ko])
                        yield
                        rows.append((o_, ko))
                    (GROW, kG), (BKROW, kBK), (QROW, kQ), (QDROW, kQD) = rows
                    P.op("dve", lambda e: e.tensor_scalar(out=E[:C, :C], in0=GROW[:C, :C], scalar1=sc[:C, h:h + 1], scalar2=0.0,
                                                          op0=ALU.subtract, op1=ALU.min), r=[kG, "sc"], w=[kE])
                    yield
                    P.op("act", lambda e: e.activation(out=E[:C, :C], in_=E[:C, :C], func=AF.Exp), r=[kE], w=[kE])
                    yield
                    P.op("dve", lambda e: e.tensor_tensor(out=X1[:C, :C], in0=BKROW[:C, :C], in1=NSTR[:C, :C], op=ALU.mult),
                         r=[kBK, "cm"], w=[kX1])
                    yield
                    P.op("dve", lambda e: e.tensor_tensor(out=X2[:C, :C], in0=QROW[:C, :C], in1=TRI[:C, :C], op=ALU.mult),
                         r=[kQ, "cm"], w=[kX2])
                    yield
                    P.op("dve", lambda e: e.tensor_tensor(out=qdT[:, :C], in0=qs[sl][:, h, :C], in1=QDROW[:, :C], op=ALU.mult),
                         r=[kq, kQD], w=[kqdT])
                    yield
                    KK, kKK = slot(h, 2)
                    KQ, kKQ = slot(h, 3)
                    P.op("pe", lambda e: e.matmul(out=KK[:C, :C], lhsT=qsb[sl][:, 4 + h, :C], rhs=qsb[sl][:, 4 + h, :C], start=True, stop=True),
                         r=[kqb], w=[kKK])
                    yield
                    P.op("pe", lambda e: e.matmul(out=KQ[:C, :C], lhsT=qsb[sl][:, 4 + h, :C], rhs=qsb[sl][:, h, :C], start=True, stop=True),
                         r=[kqb], w=[kKQ])
                    yield
                    P.op("dve", lambda e: e.scalar_tensor_tensor(out=BT[:C, :C], in0=KK[:C, :C], scalar=sc[:C, 32 + h:33 + h], in1=E[:C, :C],
                                                                 op0=ALU.mult, op1=ALU.mult), r=[kKK, "sc", kE], w=[kBT])
                    yield
                    P.op("dve", lambda e: e.tensor_tensor(out=BT[:C, :C], in0=BT[:C, :C], in1=X1[:C, :C], op=ALU.mult), r=[kBT, kX1], w=[kBT])
                    yield
                    P.op("dve", lambda e: e.scalar_tensor_tensor(out=tmp[:C, :C], in0=KQ[:C, :C], scalar=sc[:C, 32 + h:33 + h], in1=E[:C, :C],
                                                                 op0=ALU.mult, op1=ALU.mult), r=[kKQ, "sc", kE], w=[ktmp])
                    yield
                    P.op("dve", lambda e: e.tensor_tensor(out=qkT[:C, :C], in0=tmp[:C, :C], in1=X2[:C, :C], op=ALU.mult), r=[ktmp, kX2], w=[kqkT])
                    yield
                    P.op("act", lambda e: e.copy(out=BTb[:C, :C], in_=BT[:C, :C]), r=[kBT], w=[kBTb])
                    yield
                    P.op("dve", lambda e: e.tensor_tensor(out=PT[:C, :C], in0=BT[:C, :C], in1=ident[:C, :C], op=ALU.add), r=[kBT, "ident"], w=[kPT])
                    yield
                    P.op("act", lambda e: e.copy(out=PTb[:C, :C], in_=PT[:C, :C]), r=[kPT], w=[kPTb])
                    yield
                    t0_, kt0 = slot(h, 4)
                    P.op("pe", lambda e: e.transpose(out=t0_[:C, :C], in_=BT[:C, :C], identity=ident[:C, :C]), r=[kBT, "ident"], w=[kt0])
                    yield
                    P.op("act", lambda e: e.copy(out=Bb[:C, :C], in_=t0_[:C, :C]), r=[kt0], w=[kBb])
                    yield
                    nlev = max(1, int(np.ceil(np.log2(C))))
                    cur = (Bb, kBb, BTb, kBTb)
                    nxt = (Bb2, kBb2, BTb2, kBTb2)
                    for lv in range(1, nlev):
                        (cB, kcB, cBT, kcBT) = cur
                        (nB, knB, nBT, knBT) = nxt
                        x_, kx = slot(h, 5)
                        xt_, kxt = slot(h, 6)
                        p_, kp = slot(h, 7)
                        P.op("pe", lambda e, cB=cB, cBT=cBT, x_=x_: e.matmul(out=x_[:C, :C], lhsT=cBT[:C, :C], rhs=cB[:C, :C], start=True, stop=True),
                             r=[kcB, kcBT], w=[kx])
                        yield
                        P.op("act", lambda e, nB=nB, x_=x_: e.copy(out=nB[:C, :C], in_=x_[:C, :C]), r=[kx], w=[knB])
                        yield
                        if lv < nlev - 1:
                            P.op("pe", lambda e, cB=cB, cBT=cBT, xt_=xt_: e.matmul(out=xt_[:C, :C], lhsT=cB[:C, :C], rhs=cBT[:C, :C],
                                                                                 start=True, stop=True), r=[kcB, kcBT], w=[kxt])
                            yield
                            P.op("act", lambda e, nBT=nBT, xt_=xt_: e.copy(out=nBT[:C, :C], in_=xt_[:C, :C]), r=[kxt], w=[knBT])
                            yield
                        P.op("pe", lambda e, nB=nB, p_=p_: e.matmul(out=p_[:C, :C], lhsT=nB[:C, :C], rhs=PTb[:C, :C], start=True, stop=True),
                             r=[knB, kPTb], w=[kp])
                        yield
                        P.op("dve", lambda e, p_=p_: e.tensor_tensor(out=PT[:C, :C], in0=PT[:C, :C], in1=p_[:C, :C], op=ALU.add), r=[kPT, kp], w=[kPT])
                        yield
                        P.op("act", lambda e: e.copy(out=PTb[:C, :C], in_=PT[:C, :C]), r=[kPT], w=[kPTb])
                        yield
                        cur, nxt = nxt, cur
                    u_, ku = slot(h, 0)
                    wc_, kwc = slot(h, 3)
                    P.op("pe", lambda e: e.matmul(out=u_[:C, :], lhsT=PTb[:C, :C], rhs=vb[:C, :], start=True, stop=True), r=[kPTb, kvb], w=[ku])
                    yield
                    P.op("pe", lambda e: e.matmul(out=wc_[:, :C], lhsT=kbg[:C, :], rhs=PTb[:C, :C], start=True, stop=True), r=[kPTb, kkbg], w=[kwc])
                    yield
                    P.op("act", lambda e: e.copy(out=uc[:C, :], in_=u_[:C, :]), r=[ku], w=[kuc])
                    yield
                    P.op("act", lambda e: e.copy(out=wcT[:, :C], in_=wc_[:, :C]), r=[kwc], w=[kwcT])
                    yield
                    ws_, kws = slot(h, 1)
                    o_, ko = slot(h, 2)
                    kS, kSb = f"S{h}", f"Sb{h}"
                    if first:
                        P.op("act", lambda e: e.copy(out=ub[:C, :], in_=uc[:C, :]), r=[kuc], w=[kub])
                        yield
                        P.op("pe", lambda e: e.matmul(out=o_[:C, :], lhsT=qkT[:C, :C], rhs=ub[:C, :], start=True, stop=True), r=[kqkT, kub], w=[ko])
                        yield
                    else:
                        P.op("pe", lambda e: e.matmul(out=ws_[:C, :], lhsT=wcT[:, :C], rhs=Sb[h][:, :], start=True, stop=True), r=[kwcT, kSb], w=[kws])
                        yield
                        P.op("dve", lambda e: e.tensor_tensor(out=ub[:C, :], in0=uc[:C, :], in1=ws_[:C, :], op=ALU.subtract), r=[kuc, kws], w=[kub])
                        yield
                        P.op("pe", lambda e: e.matmul(out=o_[:C, :], lhsT=qdT[:, :C], rhs=Sb[h][:, :], start=True, stop=False), r=[kqdT, kSb], w=[ko])
                        yield
                        P.op("pe", lambda e: e.matmul(out=o_[:C, :], lhsT=qkT[:C, :C], rhs=ub[:C, :], start=False, stop=True), r=[kqkT, kub], w=[ko])
                        yield
                    su_, ksu = slot(h, 5)
                    P.op("pe", lambda e: e.matmul(out=su_[:, :], lhsT=ktl[:C, :], rhs=ub[:C, :], start=True, stop=True), r=[kktl, kub], w=[ksu])
                    yield
                    if first:
                        P.op("act", lambda e: e.copy(out=S[h][:, :], in_=su_[:, :]), r=[ksu], w=[kS])
                        yield
                    else:
                        P.op("dve", lambda e: e.scalar_tensor_tensor(out=S[h][:, :], in0=S[h][:, :], scalar=sc[:, 16 + h:17 + h], in1=su_[:, :],
                                                                     op0=ALU.mult, op1=ALU.add), r=[kS, "sc", ksu], w=[kS])
                        yield
                    P.op("act", lambda e: e.copy(out=Sb[h][:, :], in_=S[h][:, :]), r=[kS], w=[kSb])
                    yield
                    P.op("act", lambda e: e.activation(out=tmp[:C, :], in_=o_[:C, :], func=AF.Square, accum_out=sc[:C, 52 + h:53 + h]),
                         r=[ko], w=[ktmp, f"sco{h}"])
                    yield
                    P.op("act", lambda e: e.activation(out=sc[:C, 56 + h:57 + h], in_=sc[:C, 52 + h:53 + h], func=AF.Sqrt, bias=epsc[:C, :],
                                                       scale=1.0 / 128), r=[f"sco{h}", "epsc"], w=[f"sco{h}"])
                    yield
                    P.op("dve", lambda e: e.reciprocal(out=sc[:C, 56 + h:57 + h], in_=sc[:C, 56 + h:57 + h]), r=[f"sco{h}"], w=[f"sco{h}"])
                    yield
                    P.op("dve", lambda e: e.scalar_tensor_tensor(out=ya[:C, h * 128:(h + 1) * 128], in0=o_[:C, :], scalar=sc[:C, 56 + h:57 + h],
                                                                 in1=gz[:C, h * 128:(h + 1) * 128], op0=ALU.mult, op1=ALU.mult),
                         r=[ko, f"sco{h}", "gz"], w=[f"ya{h}"])
                    yield

                li = 0
                for i in range(NT):
                    gdn_chunk(128, i * 128, i == 0, li)
                    li += 1
                for h in range(4):
                    P.dma("sp", gdnp[h], S[h][:, :], r=[f"S{h}"])
                for b in range(NSEQ):
                    for h in range(4):
                        P.dma("sp", S[h][:, :], sgdn[b * 4 + h], w=[f"S{h}"])
                        P.op("act", lambda e, h=h: e.copy(out=Sb[h][:, :], in_=S[h][:, :]), r=[f"S{h}"], w=[f"Sb{h}"])
                    gdn_chunk(TS, L + b * TS, False, li)
                    li += 1
                    for h in range(4):
                        P.dma("sp", gdns[b * 4 + h], S[h][:, :], r=[f"S{h}"])
                P.run()


        def phase_nsa():
            BIG = 1.0e30
            with ExitStack() as s3:
                ovb = sbuf(s3, "ovb", [128, NCT, NSB], BF16)
                cmk = sbuf(s3, "cmk", [128, 17, 128], BF16)
                tab = sbuf(s3, "tabs", [128, 2, 2 * NSB], F32)
                tri2 = sbuf(s3, "tri2s", [128, 2, 128], BF16)
                expt = sbuf(s3, "expt_sb3", [128, NT, 128], BF16)
                pwm = sbuf(s3, "pwms", [128, 4, 8], BF16)
                wcb = sbuf(s3, "wcbs", [64, 2, 64], BF16)
                P.dma("pool", ovb[:], ov_d, w=["ovb"])
                P.dma("pool", cmk[:], cmsk_d, w=["cmk"])
                P.dma("sp", tab[:], tab_d, w=["tab"])
                P.dma("pool", tri2[:], tri2_d, w=["tri2"])
                P.dma("pool", expt[:], expt_d, w=["expt"])
                P.dma("pool", pwm[:], pw_d, w=["pwm"])
                P.dma("pool", wcb[:], wc_d, w=["wcb"])
                kcT = [sbuf(s3, f"kcT{g}", [68, NCT * 128], BF16) for g in range(2)]
                vc = [sbuf(s3, f"vc{g}", [128, NCT, 65], BF16) for g in range(2)]
                SC = [(pB[0], "pB0"), (pB[1], "pB1")]
                OC, kOC = pB[2], "pB2"
                OS, kOS = pB[3], "pB3"
                OW, kOW = pA[0][:, 0:512], "pA0lo"
                IMP, kIMP = pA[0][:, 512:1024], "pA0hi"
                MX = [(pA[1][:, 0:512], "pA1lo"), (pA[1][:, 512:1024], "pA1hi")]
                TR, kTR = IMP, kIMP

                with ExitStack() as s3a:
                    cmpin = sbuf(s3a, "cmpin", [128, NT, 256], BF16)
                    pooledb = sbuf(s3a, "pooledb", [64, 4, NCT * 128], BF16)
                    P.dma("sp", cmpin[:], kvtm_scr[:, 0:256].rearrange("(kt p) c -> p kt c", p=128), w=["cmpin"])
                    P.op("dve", lambda e: e.memset(pooledb[:], 0.0), w=["pooledb"])
                    for which in range(2):
                        for g in range(2):
                            pb, kpb = pB[which * 2 + g], f"pB{which * 2 + g}"
                            c0 = which * 128 + g * 64
                            for kt in range(NT):
                                last = (kt == NT - 1)
                                P.op("pe", lambda e, kt=kt, pb=pb, c0=c0, which=which, last=last: e.matmul(
                                    out=pb[0:64, 8 * kt:8 * kt + 8], lhsT=cmpin[:, kt, c0:c0 + 64], rhs=pwm[:, 2 * which, :],
                                    start=True, stop=last), r=["cmpin", "pwm"], w=[kpb])
                                if not last:
                                    P.op("pe", lambda e, kt=kt, pb=pb, c0=c0, which=which: e.matmul(
                                        out=pb[0:64, 8 * kt:8 * kt + 8], lhsT=cmpin[0:16, kt + 1, c0:c0 + 64],
                                        rhs=pwm[0:16, 2 * which + 1, :], start=False, stop=True), r=["cmpin", "pwm"], w=[kpb])
                            P.op("act", lambda e, pb=pb, which=which, g=g: e.copy(out=pooledb[:, which * 2 + g, 0:8 * NT],
                                                                                 in_=pb[0:64, 0:8 * NT]), r=[kpb], w=["pooledb"])
                    for g in range(2):
                        P.op("pe", lambda e, g=g: e.matmul(out=pA[0][0:64, 0:NCT * 128], lhsT=wcb[:, 0, :], rhs=pooledb[:, g, :],
                                                           start=True, stop=True), r=["wcb", "pooledb"], w=["pA0lo"])
                        P.op("act", lambda e, g=g: e.copy(out=kcT[g][0:64, :], in_=pA[0][0:64, 0:NCT * 128]), r=["pA0lo"], w=[f"kcT{g}"])
                        P.dma("pool", kcT[g][64:68, :], caug_d, w=[f"kcT{g}"])
                        for ct in range(NCT):
                            P.op("pe", lambda e, g=g, ct=ct: e.matmul(out=pA[1][:, ct * 64:(ct + 1) * 64],
                                                                      lhsT=pooledb[:, 2 + g, ct * 128:(ct + 1) * 128], rhs=wcb[:, 1, :],
                                                                      start=True, stop=True), r=["wcb", "pooledb"], w=["pA1lo"])
                        P.op("dve", lambda e, g=g: e.memset(vc[g][:, :, 64:65], 1.0), w=[f"vc{g}"])
                        P.op("act", lambda e, g=g: e.copy(out=vc[g][:, :, 0:64],
                                                          in_=pA[1][:, 0:NCT * 64].rearrange("p (c d) -> p c d", d=64)),
                             r=["pA1lo"], w=[f"vc{g}"])
                    P.run()

                kS = sbuf(s3, "kS", [68, L], BF16)
                kW = sbuf(s3, "kW", [68, L], BF16)
                vS = sbuf(s3, "vS", [128, NT, 65], BF16)
                vW = sbuf(s3, "vW", [128, NT, 65], BF16)
                R3 = 3
                qa = [sbuf(s3, f"qa{j}", [68, 4, 128], BF16) for j in range(R3)]
                gt = [sbuf(s3, f"gt{j}", [128, 24], F32) for j in range(R3)]
                ecmp = [sbuf(s3, f"ecmp{j}", [128, NCT, 512], BF16) for j in range(R3)]
                impsb = [sbuf(s3, f"impsb{j}", [128, 512], F32) for j in range(R3)]
                selT = [sbuf(s3, f"selT{j}", [128, 128], BF16) for j in range(R3)]
                otok = [[sbuf(s3, f"otok{j}_{b_}", [128, 4, 65], F32) for b_ in range(3)] for j in range(R3)]
                rs = [sbuf(s3, f"rs3_{j}", [128, 3, 4], F32) for j in range(R3)]
                coef = sbuf(s3, "coef3", [128, 3, 4], F32)
                es = [sbuf(s3, f"es{j}", [128, 512], BF16) for j in range(2)]
                oTa = sbuf(s3, "oTa", [65, 512], F32)
                oTs = sbuf(s3, "oTs", [65, 512], F32)
                oTw = sbuf(s3, "oTw", [65, 512], F32)
                ctmp = sbuf(s3, "ctmp", [128, 512], F32)
                sco = sbuf(s3, "sco", [128, NSB], F32)
                sco2 = sbuf(s3, "sco2", [128, NSB], F32)
                m8 = sbuf(s3, "m8", [128, 16], F32)
                selm = sbuf(s3, "selm", [128, NSB], F32)
                yb = sbuf(s3, "yb3", [128, 256], F32)
                ybT = sbuf(s3, "ybT3", [128, 2, 128], BF16)

                def fin_tok(src, ksrc, par, br):
                    for h in range(4):
                        P.op("pe", lambda e, h=h: e.transpose(out=TR[:, h * 65:(h + 1) * 65], in_=src[0:65, h * 128:(h + 1) * 128],
                                                              identity=ident[0:65, 0:65]), r=[ksrc, "ident"], w=[kTR])
                    P.op("dve", lambda e: e.tensor_copy(out=otok[par][br][:], in_=TR[:, 0:260].rearrange("p (h d) -> p h d", h=4)),
                         r=[kTR], w=[f"otok{par}_{br}"])
                    P.op("dve", lambda e: e.tensor_scalar(out=rs[par][:, br, :], in0=otok[par][br][:, :, 64], scalar1=1.0e-30, scalar2=None,
                                                          op0=ALU.max), r=[f"otok{par}_{br}"], w=[f"rs3_{par}"])
                    P.op("dve", lambda e: e.reciprocal(out=rs[par][:, br, :], in_=rs[par][:, br, :]), r=[f"rs3_{par}"], w=[f"rs3_{par}"])

                def genA(g, i, par):
                    t0 = i * 128
                    kqa, kgt, kec, kim = f"qa{par}", f"gt{par}", f"ecmp{par}", f"impsb{par}"
                    P.dma("sp", qa[par][0:64, :, :], qT_scr[g, :, :, t0:t0 + 128], w=[kqa])
                    P.dma("pool", qa[par][64:68, :, :], qaug_d[g, :, :, t0:t0 + 128], w=[kqa])
                    P.dma("sp", gt[par][:], gates_scr[t0:t0 + 128, :], w=[kgt])
                    yield
                    rhsq = qa[par][:].rearrange("p h t -> p (h t)")
                    nct_i = min(NCT, (8 * i + 6) // 128 + 1)
                    for ct in range(nct_i):
                        scp, kscp = SC[ct % 2]
                        m_ = i - 16 * ct
                        P.op("pe", lambda e, ct=ct, scp=scp: e.matmul(out=scp[:, :], lhsT=kcT[g][:, ct * 128:(ct + 1) * 128], rhs=rhsq,
                                                                      start=True, stop=True), r=[f"kcT{g}", kqa], w=[kscp])
                        if m_ <= 16:
                            P.op("dve", lambda e, scp=scp: e.tensor_scalar(out=ctmp[:, :], in0=scp[:, :], scalar1=60.0, scalar2=None, op0=ALU.min),
                                 r=[kscp], w=["ctmp"])
                            P.op("act", lambda e, ct=ct: e.activation(out=ecmp[par][:, ct, :], in_=ctmp[:, :], func=AF.Exp),
                                 r=["ctmp"], w=[kec])
                            P.op("dve", lambda e, ct=ct, m_=m_: e.tensor_tensor(
                                out=ecmp[par][:, ct, :].rearrange("p (h t) -> p h t", h=4),
                                in0=ecmp[par][:, ct, :].rearrange("p (h t) -> p h t", h=4),
                                in1=cmk[:, m_, :].unsqueeze(1).to_broadcast([128, 4, 128]), op=ALU.mult), r=[kec, "cmk"], w=[kec])
                        else:
                            P.op("act", lambda e, ct=ct, scp=scp: e.activation(out=ecmp[par][:, ct, :], in_=scp[:, :], func=AF.Exp),
                                 r=[kscp], w=[kec])
                        yield
                    for ct in range(nct_i):
                        P.op("pe", lambda e, ct=ct: e.matmul(out=OC[0:65, :], lhsT=vc[g][:, ct, :], rhs=ecmp[par][:, ct, :],
                                                             start=(ct == 0), stop=(ct == nct_i - 1)), r=[f"vc{g}", kec], w=[kOC])
                    yield
                    for h in range(4):
                        for ct in range(nct_i):
                            P.op("pe", lambda e, ct=ct, h=h: e.matmul(out=IMP[:, h * 128:h * 128 + NSB], lhsT=ecmp[par][:, ct, h * 128:(h + 1) * 128],
                                                                      rhs=ovb[:, ct, :], start=(ct == 0), stop=(ct == nct_i - 1)),
                                 r=[kec, "ovb"], w=[kIMP])
                    P.op("act", lambda e: e.copy(out=impsb[par][:, :], in_=IMP[:, :]), r=[kIMP], w=[kim])
                    yield
                    P.op("act", lambda e: e.copy(out=oTa[:, :], in_=OC[0:65, :]), r=[kOC], w=["oTa"])
                    fin_tok(oTa, "oTa", par, 0)
                    yield
                    krs = f"rs3_{par}"
                    P.op("dve", lambda e: e.tensor_scalar(out=sco[:, :], in0=impsb[par][:, 0:NSB], scalar1=rs[par][:, 0, 0:1], scalar2=None, op0=ALU.mult),
                         r=[kim, krs], w=["sco"])
                    yield
                    for h in range(1, 4):
                        P.op("dve", lambda e, h=h: e.scalar_tensor_tensor(out=sco[:, :], in0=impsb[par][:, h * 128:h * 128 + NSB],
                                                                         scalar=rs[par][:, 0, h:h + 1], in1=sco[:, :], op0=ALU.mult, op1=ALU.add),
                             r=[kim, krs, "sco"], w=["sco"])
                        yield
                    a0 = NSB - 2 * i
                    P.op("dve", lambda e: e.tensor_tensor(out=sco[:, :], in0=sco[:, :], in1=tab[:, 0, a0:a0 + NSB], op=ALU.mult),
                         r=["sco", "tab"], w=["sco"])
                    yield
                    P.op("dve", lambda e: e.tensor_tensor(out=sco[:, :], in0=sco[:, :], in1=tab[:, 1, a0:a0 + NSB], op=ALU.add),
                         r=["sco", "tab"], w=["sco"])
                    yield
                    P.op("dve", lambda e: e.memset(sco[:, 0:1], 1.0e4), r=["sco"], w=["sco"])
                    yield
                    P.op("dve", lambda e: e.max(out=m8[:, 0:8], in_=sco[:, :]), r=["sco"], w=["m8"])
                    yield
                    P.op("dve", lambda e: e.match_replace(out=sco2[:, :], in_to_replace=m8[:, 0:8], in_values=sco[:, :], imm_value=-BIG),
                         r=["sco", "m8"], w=["sco2"])
                    yield
                    P.op("dve", lambda e: e.max(out=m8[:, 8:16], in_=sco2[:, :]), r=["sco2"], w=["m8"])
                    yield
                    P.op("dve", lambda e: e.tensor_scalar(out=m8[:, 15:16], in0=m8[:, 15:16], scalar1=-1.0e29, scalar2=None, op0=ALU.max),
                         r=["m8"], w=["m8"])
                    yield
                    P.op("dve", lambda e: e.tensor_scalar(out=selm[:, :], in0=sco[:, :], scalar1=m8[:, 15:16], scalar2=None, op0=ALU.is_ge),
                         r=["sco", "m8"], w=["selm"])
                    yield
                    P.op("pe", lambda e: e.transpose(out=TR[0:NSB, 0:128], in_=selm[:, 0:NSB], identity=ident[:, :]),
                         r=["selm", "ident"], w=[kTR])
                    P.op("act", lambda e: e.copy(out=selT[par][0:NSB, :], in_=TR[0:NSB, 0:128]), r=[kTR], w=[f"selT{par}"])
                    yield

                def genB(g, i, par):
                    kqa = f"qa{par}"
                    rhsq = qa[par][:].rearrange("p h t -> p (h t)")
                    pairs = []
                    for kt in range(i + 1):
                        pairs.append(("s", kt, "mx" if kt < i else 0))
                    w0 = max(0, i - 4)
                    for kt in range(w0, i + 1):
                        pairs.append(("w", kt, 0 if kt == i else (1 if kt == i - 4 else None)))
                    nS = i + 1
                    nW = i + 1 - w0

                    def front(k):
                        br, kt, msk = pairs[k]
                        b2 = k % 2
                        scp, kscp = SC[b2]
                        kk, kkk = (kS, "kS") if br == "s" else (kW, "kW")
                        if msk == "mx":
                            mxp, kmx = MX[b2]
                            P.op("pe", lambda e: e.matmul(out=mxp[:, 0:128], lhsT=expt[0:NSB, kt, :], rhs=selT[par][0:NSB, :], start=True, stop=True),
                                 r=["expt", f"selT{par}"], w=[kmx])
                        P.op("pe", lambda e: e.matmul(out=scp[:, :], lhsT=kk[:, kt * 128:(kt + 1) * 128], rhs=rhsq, start=True, stop=True),
                             r=[kkk, kqa], w=[kscp])
                        P.op("act", lambda e: e.activation(out=es[b2][:, :], in_=scp[:, :], func=AF.Exp), r=[kscp], w=[f"es{b2}"])
                        if msk is not None:
                            if msk == "mx":
                                mk, kmk = MX[b2][0][:, 0:128], MX[b2][1]
                            else:
                                mk, kmk = tri2[:, msk, :], "tri2"
                            P.op("dve", lambda e: e.tensor_tensor(
                                out=es[b2][:, :].rearrange("p (h t) -> p h t", h=4), in0=es[b2][:, :].rearrange("p (h t) -> p h t", h=4),
                                in1=mk.unsqueeze(1).to_broadcast([128, 4, 128]), op=ALU.mult), r=[f"es{b2}", kmk], w=[f"es{b2}"])

                    def back(k):
                        br, kt, msk = pairs[k]
                        b2 = k % 2
                        if br == "s":
                            P.op("pe", lambda e: e.matmul(out=OS[0:65, :], lhsT=vS[:, kt, :], rhs=es[b2][:, :], start=(k == 0), stop=(k == nS - 1)),
                                 r=["vS", f"es{b2}"], w=[kOS])
                        else:
                            P.op("pe", lambda e: e.matmul(out=OW[0:65, :], lhsT=vW[:, kt, :], rhs=es[b2][:, :], start=(k == nS), stop=(k == nS + nW - 1)),
                                 r=["vW", f"es{b2}"], w=[kOW])

                    front(0)
                    yield
                    for k in range(len(pairs)):
                        if k + 1 < len(pairs):
                            front(k + 1)
                        back(k)
                        yield

                def eagerC():
                    P.op("act", lambda e: e.copy(out=oTs[:, :], in_=OS[0:65, :]), r=[kOS], w=["oTs"])
                    P.op("act", lambda e: e.copy(out=oTw[:, :], in_=OW[0:65, :]), r=[kOW], w=["oTw"])

                def genC(g, i, par):
                    t0 = i * 128
                    fin_tok(oTs, "oTs", par, 1)
                    yield
                    fin_tok(oTw, "oTw", par, 2)
                    yield
                    gv_ = gt[par][:, 12 * g:12 * g + 12].rearrange("p (h b) -> p b h", b=3)
                    P.op("dve", lambda e: e.tensor_tensor(out=coef[:], in0=rs[par][:], in1=gv_, op=ALU.mult), r=[f"rs3_{par}", f"gt{par}"], w=["coef3"])
                    yield
                    for h in range(4):
                        P.op("dve", lambda e, h=h: e.tensor_scalar(out=yb[:, h * 64:(h + 1) * 64], in0=otok[par][0][:, h, 0:64],
                                                                   scalar1=coef[:, 0, h:h + 1], scalar2=None, op0=ALU.mult),
                             r=[f"otok{par}_0", "coef3"], w=["yb3"])
                        yield
                        for br in (1, 2):
                            P.op("dve", lambda e, h=h, br=br: e.scalar_tensor_tensor(
                                out=yb[:, h * 64:(h + 1) * 64], in0=otok[par][br][:, h, 0:64], scalar=coef[:, br, h:h + 1],
                                in1=yb[:, h * 64:(h + 1) * 64], op0=ALU.mult, op1=ALU.add), r=[f"otok{par}_{br}", "coef3", "yb3"], w=["yb3"])
                            yield
                    for c2 in range(2):
                        P.op("pe", lambda e, c2=c2: e.transpose(out=TR[:, c2 * 128:(c2 + 1) * 128], in_=yb[:, c2 * 128:(c2 + 1) * 128],
                                                                identity=ident[:, :]), r=["yb3", "ident"], w=[kTR])
                    P.op("act", lambda e: e.copy(out=ybT[:], in_=TR[:, 0:256].rearrange("p (c t) -> p c t", c=2)), r=[kTR], w=["ybT3"])
                    yield
                    P.dma("sp", ybT_scr[:, 2 * g:2 * g + 2, t0:t0 + 128], ybT[:], r=["ybT3"], w=[f"ybTs{g}_{i}"])
                    yield

                def drive(gens):
                    while gens:
                        for gen in list(gens):
                            try:
                                next(gen)
                            except StopIteration:
                                gens.remove(gen)

                for g in range(2):
                    P.dma("sp", kS[0:64, :], kT_scr[2 * g], w=["kS"])
                    P.dma("sp", kW[0:64, :], kT_scr[2 * g + 1], w=["kW"])
                    P.dma("pool", kS[64:68, :], kaug_d, w=["kS"])
                    P.dma("pool", kW[64:68, :], kaug_d, w=["kW"])
                    P.dma("sp", vS[:, :, 0:64], kvtm_scr[:, 384 + g * 64:448 + g * 64].rearrange("(kt p) d -> p kt d", p=128), w=["vS"])
                    P.dma("sp", vW[:, :, 0:64], kvtm_scr[:, 640 + g * 64:704 + g * 64].rearrange("(kt p) d -> p kt d", p=128), w=["vW"])
                    P.op("dve", lambda e: e.memset(vS[:, :, 64:65], 1.0), w=["vS"])
                    P.op("dve", lambda e: e.memset(vW[:, :, 64:65], 1.0), w=["vW"])
                    drive([genA(g, 0, 0)])
                    for i in range(NT):
                        gens = []
                        if i >= 1:
                            eagerC()
                            gens.append(genC(g, i - 1, (i - 1) % R3))
                        if i + 1 < NT:
                            gens.append(genA(g, i + 1, (i + 1) % R3))
                        gens.append(genB(g, i, i % R3))
                        drive(gens)
                    eagerC()
                    drive([genC(g, NT - 1, (NT - 1) % R3)])
                P.run()


        def phase_nsas():
            BIG = 1.0e30
            NQ = TS
            NQC = 4 * NQ
            with ExitStack() as s5:
                ovs = sbuf(s5, "ovs_sb", [128, NCTS, NSBS], BF16)
                tri2 = sbuf(s5, "tri2_5", [128, 2, 128], BF16)
                expts = sbuf(s5, "expts_sb", [128, 64, 128], BF16)
                pwm = sbuf(s5, "pwm5", [128, 4, 8], F32)
                wcb = sbuf(s5, "wcb5", [64, 2, 64], BF16)
                pio = sbuf(s5, "pio", [128, 1], F32)
                P.dma("pool", ovs[:], ovs_d, w=["ovs"])
                P.dma("pool", tri2[:], tri2_d, w=["tri2_5"])
                P.dma("pool", expts[:], expts_d, w=["expts"])
                P.dma("sp", pwm[:], pw_d, w=["pwm5"])
                P.dma("pool", wcb[:], wc_d, w=["wcb5"])
                P.dma("sp", pio[:], piota, w=["pio"])
                kS = sbuf(s5, "kS5", [68, 2, NKT * 128], BF16)
                vS = sbuf(s5, "vS5", [128, NKT, 2, 65], BF16)
                kW = sbuf(s5, "kW5", [68, 2, 5 * 128], BF16)
                vW = sbuf(s5, "vW5", [128, 5, 2, 65], BF16)
                kcT = sbuf(s5, "kcT5", [68, 2, NCTS * 128], BF16)
                vc = sbuf(s5, "vc5", [128, NCTS, 2, 65], BF16)
                pooledb = sbuf(s5, "pooled5", [64, 4, NCTS * 128], BF16)
                pg = [sbuf(s5, f"pg{j}", [128, 512], F32) for j in range(3)]
                newt = sbuf(s5, "newt", [128, 768], F32)
                cwt = [sbuf(s5, f"cwt{j}", [128, 256], F32) for j in range(2)]
                ptb = sbuf(s5, "ptb", [128, NPG], I32)
                idxf = sbuf(s5, "idxf", [128, NPG], F32)
                idxi = sbuf(s5, "idxi", [128, NPG], I32)
                qa = sbuf(s5, "qa5", [68, 4, NQ], BF16)
                gt = sbuf(s5, "gt5", [NQ, 24], F32)
                ecmp = sbuf(s5, "ecmp5", [128, NCTS, NQC], BF16)
                es = [sbuf(s5, f"es5_{j}", [128, NQC], BF16) for j in range(2)]
                oT = sbuf(s5, "oT5", [65, NQC], F32)
                otok = [sbuf(s5, f"otok5_{j}", [NQ, 4, 65], F32) for j in range(3)]
                rs = sbuf(s5, "rs5", [NQ, 3, 4], F32)
                coef = sbuf(s5, "coef5", [NQ, 3, 4], F32)
                sco = sbuf(s5, "sco5", [NQ, NSBS], F32)
                sco2 = sbuf(s5, "sco25", [NQ, NSBS], F32)
                m8 = sbuf(s5, "m85", [NQ, 16], F32)
                selm = sbuf(s5, "selm5", [NQ, NSBS], F32)
                selT = sbuf(s5, "selT5", [128, 2, NQ], BF16)
                yb = sbuf(s5, "yb5", [NQ, 512], F32)
                ybT = sbuf(s5, "ybT5", [128, 4, NQ], BF16)
                SC = [(pB[0], "pB0"), (pB[1], "pB1")]
                OC, kOC = pB[2], "pB2"
                OS, kOS = pB[3], "pB3"
                OW, kOW = pA[0][:, 0:512], "pA0lo"
                TR, kTR = pA[0][:, 512:1024], "pA0hi"
                MX = [(pA[1][:, 0:512], "pA1lo"), (pA[1][:, 512:1024], "pA1hi")]
                TRk, kTRk = pA[1][:, 0:512], "pA1lo"

                for g in range(2):
                    P.dma("pool", kS[64:68, g, :], kaugs_d, w=["kS5"])
                    P.dma("pool", kW[64:68, g, :], kaugs_d[:, (NKT - 5) * 128:NKT * 128], w=["kW5"])
                    P.dma("pool", kcT[64:68, g, :], caugs_d, w=["kcT5"])
                P.op("dve", lambda e: e.memset(vS[:, :, :, 64:65], 1.0), w=["vS5"])
                P.op("dve", lambda e: e.memset(vW[:, :, :, 64:65], 1.0), w=["vW5"])
                P.op("dve", lambda e: e.memset(vc[:, :, :, 64:65], 1.0), w=["vc5"])
                P.op("dve", lambda e: e.memset(newt[:], 0.0), w=["newt"])
                P.op("dve", lambda e: e.memset(pooledb[:], 0.0), w=["pooled5"])

                def finalize(ps, kps, br):
                    P.op("act", lambda e: e.copy(out=oT[:, :], in_=ps[0:65, 0:NQC]), r=[kps], w=["oT5"])
                    for h in range(4):
                        P.op("pe", lambda e, h=h: e.transpose(out=TR[0:NQ, h * 65:(h + 1) * 65], in_=oT[0:65, h * NQ:(h + 1) * NQ],
                                                              identity=ident[0:65, 0:65]), r=["oT5", "ident"], w=[kTR])
                    P.op("dve", lambda e: e.tensor_copy(out=otok[br][:], in_=TR[0:NQ, 0:260].rearrange("p (h d) -> p h d", h=4)),
                         r=[kTR], w=[f"otok5_{br}"])
                    P.op("dve", lambda e: e.tensor_scalar(out=rs[:, br, :], in0=otok[br][:, :, 64], scalar1=1.0e-30, scalar2=None,
                                                          op0=ALU.max), r=[f"otok5_{br}"], w=["rs5"])
                    P.op("dve", lambda e: e.reciprocal(out=rs[:, br, :], in_=rs[:, br, :]), r=["rs5"], w=["rs5"])

                trk_cnt = [0]

                def kT_from(src_ap, ksrc, dst_fn):
                    alt = trk_cnt[0] % 2
                    trk_cnt[0] += 1
                    bank, kbank = MX[alt]
                    for g in range(2):
                        P.op("pe", lambda e, g=g: e.transpose(out=bank[0:64, g * 128:(g + 1) * 128], in_=src_ap(g), identity=ident[:, :]),
                             r=[ksrc, "ident"], w=[kbank])
                    return bank, kbank

                def seq(b):
                    P.dma("sp", ptb[:], ptab[b].partition_broadcast(128), w=["ptb"])
                    P.op("dve", lambda e: e.tensor_copy(out=idxf[:], in_=ptb[:]), r=["ptb"], w=["idxf"])
                    P.op("dve", lambda e: e.tensor_scalar(out=idxf[:], in0=idxf[:], scalar1=128.0, scalar2=pio[:, 0:1], op0=ALU.mult, op1=ALU.add),
                         r=["idxf", "pio"], w=["idxf"])
                    P.op("dve", lambda e: e.tensor_copy(out=idxi[:], in_=idxf[:]), r=["idxf"], w=["idxi"])

                    def gather(j):
                        P.idma(pg[j % 3][:, :], ckv, idxi[:, j:j + 1], r=["idxi"], w=[f"pg{j % 3}"])

                    gather(0)
                    if NPG > 1:
                        gather(1)
                    for j in range(NPG):
                        if j + 2 < NPG:
                            gather(j + 2)
                        t_, kt_ = pg[j % 3], f"pg{j % 3}"
                        n_, kn_ = pg[(j + 1) % 3], f"pg{(j + 1) % 3}"
                        last = (j == NPG - 1)
                        for which in range(2):
                            for g in range(2):
                                pb, kpb = pB[which * 2 + g], f"pB{which * 2 + g}"
                                c0 = which * 128 + g * 64
                                jc = 8 * (j % 64)
                                P.op("pe", lambda e, pb=pb, c0=c0, jc=jc, which=which, t_=t_, last=last: e.matmul(
                                    out=pb[0:64, jc:jc + 8], lhsT=t_[:, c0:c0 + 64], rhs=pwm[:, 2 * which, :], start=True, stop=last),
                                     r=[kt_, "pwm5"], w=[kpb])
                                if not last:
                                    P.op("pe", lambda e, pb=pb, c0=c0, jc=jc, which=which, n_=n_: e.matmul(
                                        out=pb[0:64, jc:jc + 8], lhsT=n_[0:16, c0:c0 + 64], rhs=pwm[0:16, 2 * which + 1, :],
                                        start=False, stop=True), r=[kn_, "pwm5"], w=[kpb])
                        if j % 64 == 63 or last:
                            blk0 = (j // 64) * 512
                            ncol = 8 * (j % 64 + 1)
                            for idx4 in range(4):
                                P.op("act", lambda e, idx4=idx4, blk0=blk0, ncol=ncol: e.copy(out=pooledb[:, idx4, blk0:blk0 + ncol],
                                                                                           in_=pB[idx4][0:64, 0:ncol]),
                                     r=[f"pB{idx4}"], w=["pooled5"])
                        bk_, kbk_ = kT_from(lambda g, t_=t_: t_[:, 256 + g * 64:320 + g * 64], kt_, None)
                        P.op("act", lambda e, j=j, bk_=bk_: e.copy(out=kS[0:64, :, j * 128:(j + 1) * 128],
                                                                   in_=bk_[0:64, 0:256].rearrange("p (g t) -> p g t", g=2)), r=[kbk_], w=["kS5"])
                        P.op("pool", lambda e, j=j, t_=t_: e.tensor_copy(out=vS[:, j, :, 0:64],
                                                                         in_=t_[:, 384:512].rearrange("p (g d) -> p g d", g=2)),
                             r=[kt_], w=["vS5"])
                    P.dma("sp", newt[0:NQ, :], kvs_scr[b * NQ:(b + 1) * NQ, :], w=["newt"])
                    bk1, kbk1 = kT_from(lambda g: newt[:, 256 + g * 64:320 + g * 64], "newt", None)
                    P.op("act", lambda e: e.copy(out=kS[0:64, :, NPG * 128:(NPG + 1) * 128],
                                                 in_=bk1[0:64, 0:256].rearrange("p (g t) -> p g t", g=2)), r=[kbk1], w=["kS5"])
                    P.op("pool", lambda e: e.tensor_copy(out=vS[:, NPG, :, 0:64], in_=newt[:, 384:512].rearrange("p (g d) -> p g d", g=2)),
                         r=["newt"], w=["vS5"])
                    bk2, kbk2 = kT_from(lambda g: newt[:, 512 + g * 64:576 + g * 64], "newt", None)
                    P.op("act", lambda e: e.copy(out=kW[0:64, :, 4 * 128:5 * 128],
                                                 in_=bk2[0:64, 0:256].rearrange("p (g t) -> p g t", g=2)), r=[kbk2], w=["kW5"])
                    P.op("pool", lambda e: e.tensor_copy(out=vW[:, 4, :, 0:64], in_=newt[:, 640:768].rearrange("p (g d) -> p g d", g=2)),
                         r=["newt"], w=["vW5"])
                    for a in range(4):
                        c_, kc_ = cwt[a % 2], f"cwt{a % 2}"
                        P.dma("sp", c_[:], cwin4[b, a], w=[kc_])
                        bk3, kbk3 = kT_from(lambda g, c_=c_: c_[:, g * 64:(g + 1) * 64], kc_, None)
                        P.op("act", lambda e, a=a, bk3=bk3: e.copy(out=kW[0:64, :, a * 128:(a + 1) * 128],
                                                                   in_=bk3[0:64, 0:256].rearrange("p (g t) -> p g t", g=2)), r=[kbk3], w=["kW5"])
                        P.op("pool", lambda e, a=a, c_=c_: e.tensor_copy(out=vW[:, a, :, 0:64],
                                                                         in_=c_[:, 128:256].rearrange("p (g d) -> p g d", g=2)),
                             r=[kc_], w=["vW5"])
                    for g in range(2):
                        for hf in range(NCTS * 128 // 512 if NCTS * 128 >= 512 else 1):
                            wd = min(512, NCTS * 128)
                            P.op("pe", lambda e, g=g, hf=hf, wd=wd: e.matmul(out=pB[0][0:64, 0:wd], lhsT=wcb[:, 0, :],
                                                                             rhs=pooledb[:, g, hf * wd:(hf + 1) * wd], start=True, stop=True),
                                 r=["wcb5", "pooled5"], w=["pB0"])
                            P.op("act", lambda e, g=g, hf=hf, wd=wd: e.copy(out=kcT[0:64, g, hf * wd:(hf + 1) * wd], in_=pB[0][0:64, 0:wd]),
                                 r=["pB0"], w=["kcT5"])
                        for ct in range(NCTS):
                            P.op("pe", lambda e, g=g, ct=ct: e.matmul(out=pB[1][:, ct * 64:(ct + 1) * 64],
                                                                      lhsT=pooledb[:, 2 + g, ct * 128:(ct + 1) * 128], rhs=wcb[:, 1, :],
                                                                      start=True, stop=True), r=["wcb5", "pooled5"], w=["pB1"])
                        P.op("act", lambda e, g=g: e.copy(out=vc[:, :, g, 0:64],
                                                          in_=pB[1][:, 0:NCTS * 64].rearrange("p (c d) -> p c d", d=64)), r=["pB1"], w=["vc5"])
                    P.dma("sp", gt[:], gates_s_scr[b * NQ:(b + 1) * NQ, :], w=["gt5"])
                    for g in range(2):
                        attend(b, g)
                    for c4 in range(4):
                        P.op("pe", lambda e, c4=c4: e.transpose(out=TR[:, c4 * NQ:(c4 + 1) * NQ], in_=yb[0:NQ, c4 * 128:(c4 + 1) * 128],
                                                                identity=ident[0:NQ, 0:NQ]), r=["yb5", "ident"], w=[kTR])
                    P.op("act", lambda e: e.copy(out=ybT[:], in_=TR[:, 0:4 * NQ].rearrange("p (c t) -> p c t", c=4)), r=[kTR], w=["ybT5"])
                    P.dma("sp", ybT_scr[:, :, L + b * NQ:L + (b + 1) * NQ], ybT[:], r=["ybT5"], w=[f"ybTss{b}"])

                def attend(b, g):
                    P.dma("sp", qa[0:64, :, :], qTs_scr[g, :, :, b * NQ:(b + 1) * NQ], w=["qa5"])
                    P.dma("pool", qa[64:68, :, :], qaugs_d[g], w=["qa5"])
                    rhsq = qa[:].rearrange("p h t -> p (h t)")
                    for ct in range(NCTS):
                        scp, kscp = SC[ct % 2]
                        P.op("pe", lambda e, ct=ct, scp=scp: e.matmul(out=scp[:, 0:NQC], lhsT=kcT[:, g, ct * 128:(ct + 1) * 128], rhs=rhsq,
                                                                      start=True, stop=True), r=["kcT5", "qa5"], w=[kscp])
                        P.op("act", lambda e, ct=ct, scp=scp: e.activation(out=ecmp[:, ct, :], in_=scp[:, 0:NQC], func=AF.Exp),
                             r=[kscp], w=["ecmp5"])
                    for ct in range(NCTS):
                        P.op("pe", lambda e, ct=ct: e.matmul(out=OC[0:65, 0:NQC], lhsT=vc[:, ct, g, :], rhs=ecmp[:, ct, :],
                                                             start=(ct == 0), stop=(ct == NCTS - 1)), r=["vc5", "ecmp5"], w=[kOC])
                    finalize(OC, kOC, 0)
                    for h in range(4):
                        for ct in range(NCTS):
                            P.op("pe", lambda e, ct=ct, h=h: e.matmul(out=OS[0:NQ, 0:NSBS], lhsT=ecmp[:, ct, h * NQ:(h + 1) * NQ],
                                                                      rhs=ovs[:, ct, :], start=(ct == 0), stop=(ct == NCTS - 1)),
                                 r=["ecmp5", "ovs"], w=[kOS])
                        if h == 0:
                            P.op("dve", lambda e: e.tensor_scalar(out=sco[:, :], in0=OS[0:NQ, 0:NSBS], scalar1=rs[:, 0, 0:1], scalar2=None,
                                                                  op0=ALU.mult), r=[kOS, "rs5"], w=["sco5"])
                        else:
                            P.op("dve", lambda e, h=h: e.scalar_tensor_tensor(out=sco[:, :], in0=OS[0:NQ, 0:NSBS], scalar=rs[:, 0, h:h + 1],
                                                                             in1=sco[:, :], op0=ALU.mult, op1=ALU.add),
                                 r=[kOS, "rs5", "sco5"], w=["sco5"])
                    P.op("dve", lambda e: e.memset(sco[:, 0:1], 1.0e4), r=["sco5"], w=["sco5"])
                    P.op("dve", lambda e: e.memset(sco[:, NSBS - 2:NSBS], 1.0e4), r=["sco5"], w=["sco5"])
                    P.op("dve", lambda e: e.max(out=m8[:, 0:8], in_=sco[:, :]), r=["sco5"], w=["m85"])
                    P.op("dve", lambda e: e.match_replace(out=sco2[:, :], in_to_replace=m8[:, 0:8], in_values=sco[:, :], imm_value=-BIG),
                         r=["sco5", "m85"], w=["sco25"])
                    P.op("dve", lambda e: e.max(out=m8[:, 8:16], in_=sco2[:, :]), r=["sco25"], w=["m85"])
                    P.op("dve", lambda e: e.tensor_scalar(out=m8[:, 15:16], in0=m8[:, 15:16], scalar1=-1.0e29, scalar2=None, op0=ALU.max),
                         r=["m85"], w=["m85"])
                    P.op("dve", lambda e: e.tensor_scalar(out=selm[:, :], in0=sco[:, :], scalar1=m8[:, 15:16], scalar2=None, op0=ALU.is_ge),
                         r=["sco5", "m85"], w=["selm5"])
                    for ch in range(2):
                        wch = min(128, NSBS - 1 - ch * 128)
                        if wch <= 0:
                            continue
                        P.op("pe", lambda e, ch=ch, wch=wch: e.transpose(out=TR[0:wch, ch * NQ:(ch + 1) * NQ],
                                                                         in_=selm[0:NQ, ch * 128:ch * 128 + wch], identity=ident[0:NQ, 0:NQ]),
                             r=["selm5", "ident"], w=[kTR])
                        P.op("act", lambda e, ch=ch, wch=wch: e.copy(out=selT[0:wch, ch, :], in_=TR[0:wch, ch * NQ:(ch + 1) * NQ]),
                             r=[kTR], w=["selT5"])
                    pairs = [("s", j, "mx") for j in range(NPG)] + [("s", NPG, 0)]
                    pairs += [("w", 0, 1), ("w", 1, None), ("w", 2, None), ("w", 3, None), ("w", 4, 0)]
                    nS = NPG + 1

                    def front(k):
                        br, kt, msk = pairs[k]
                        b2 = k % 2
                        scp, kscp = SC[b2]
                        if msk == "mx":
                            mxp, kmx = MX[b2]
                            wch = min(128, NSBS - 1 - (kt // 64) * 128)
                            P.op("pe", lambda e: e.matmul(out=mxp[:, 0:NQ], lhsT=expts[0:wch, kt % 64, :], rhs=selT[0:wch, kt // 64, :],
                                                          start=True, stop=True), r=["expts", "selT5"], w=[kmx])
                        kk, kkk = (kS, "kS5") if br == "s" else (kW, "kW5")
                        P.op("pe", lambda e: e.matmul(out=scp[:, 0:NQC], lhsT=kk[:, g, kt * 128:(kt + 1) * 128], rhs=rhsq, start=True, stop=True),
                             r=[kkk, "qa5"], w=[kscp])
                        P.op("act", lambda e: e.activation(out=es[b2][:, :], in_=scp[:, 0:NQC], func=AF.Exp), r=[kscp], w=[f"es5_{b2}"])
                        if msk is not None:
                            if msk == "mx":
                                mk, kmk = MX[b2][0][:, 0:NQ], MX[b2][1]
                            else:
                                mk, kmk = tri2[:, msk, 0:NQ], "tri2_5"
                            P.op("dve", lambda e: e.tensor_tensor(
                                out=es[b2][:, :].rearrange("p (h t) -> p h t", h=4), in0=es[b2][:, :].rearrange("p (h t) -> p h t", h=4),
                                in1=mk.unsqueeze(1).to_broadcast([128, 4, NQ]), op=ALU.mult), r=[f"es5_{b2}", kmk], w=[f"es5_{b2}"])

                    def back(k):
                        br, kt, msk = pairs[k]
                        b2 = k % 2
                        if br == "s":
                            P.op("pe", lambda e: e.matmul(out=OS[0:65, 0:NQC], lhsT=vS[:, kt, g, :], rhs=es[b2][:, :], start=(k == 0), stop=(k == nS - 1)),
                                 r=["vS5", f"es5_{b2}"], w=[kOS])
                        else:
                            P.op("pe", lambda e: e.matmul(out=OW[0:65, 0:NQC], lhsT=vW[:, kt, g, :], rhs=es[b2][:, :], start=(k == nS), stop=(k == len(pairs) - 1)),
                                 r=["vW5", f"es5_{b2}"], w=[kOW])

                    front(0)
                    for k in range(len(pairs)):
                        if k + 1 < len(pairs):
                            front(k + 1)
                        back(k)
                    finalize(OS, kOS, 1)
                    finalize(OW, kOW, 2)
                    gv_ = gt[:, 12 * g:12 * g + 12].rearrange("p (h b) -> p b h", b=3)
                    P.op("dve", lambda e: e.tensor_tensor(out=coef[:], in0=rs[:], in1=gv_, op=ALU.mult), r=["rs5", "gt5"], w=["coef5"])
                    for h in range(4):
                        c0 = (4 * g + h) * 64
                        P.op("dve", lambda e, h=h, c0=c0: e.tensor_scalar(out=yb[:, c0:c0 + 64], in0=otok[0][:, h, 0:64],
                                                                          scalar1=coef[:, 0, h:h + 1], scalar2=None, op0=ALU.mult),
                             r=["otok5_0", "coef5"], w=["yb5"])
                        for br in (1, 2):
                            P.op("dve", lambda e, h=h, br=br, c0=c0: e.scalar_tensor_tensor(
                                out=yb[:, c0:c0 + 64], in0=otok[br][:, h, 0:64], scalar=coef[:, br, h:h + 1],
                                in1=yb[:, c0:c0 + 64], op0=ALU.mult, op1=ALU.add), r=[f"otok5_{br}", "coef5", "yb5"], w=["yb5"])

                for b in range(NSEQ):
                    seq(b)
                P.run()

        tiles = [(xh[j * 128:(j + 1) * 128, :], 128, yp[j * 128:(j + 1) * 128, :]) for j in range(NTH)]
        tiles.append((xs, NSTOK, ysm))
        def phase_tail_a():
            with ExitStack() as s4:
                wgb = sbuf(s4, "wgb", [128, KC, 2048], BF16)
                wob = sbuf(s4, "wob", [128, KC, D], BF16)
                wr32 = sbuf(s4, "wr32", [128, KC, 36], F32)
                brb = sbuf(s4, "brb", [128, 36], F32)
                xt = [sbuf(s4, f"x4_{j}", [128, D], F32) for j in range(2)]
                junk = sbuf(s4, "junk4", [128, D], F32)
                ss = sbuf(s4, "ss4", [128, 1], F32)
                rstd = sbuf(s4, "rstd4", [128, 1], F32)
                ss2 = sbuf(s4, "ss4b", [128, 1], F32)
                rstd2 = sbuf(s4, "rstd4b", [128, 1], F32)
                xn = sbuf(s4, "xn4", [128, D], F32)
                xnT = sbuf(s4, "xnT4", [128, KC, 128], BF16)
                sga = sbuf(s4, "sga", [128, D], F32)
                sgb = sbuf(s4, "sgb", [128, D], F32)
                h = [sbuf(s4, f"h4_{j}", [128, D], F32) for j in range(2)]
                hn = sbuf(s4, "hn4", [128, D], F32)
                hnT32 = sbuf(s4, "hnT32", [128, KC, 128], F32)
                hnTb = [sbuf(s4, f"hnTb{j}", [128, KC, 128], BF16) for j in range(2)]
                lg = sbuf(s4, "lg", [128, 36], F32)
                sm = sbuf(s4, "sm4", [128, 64], F32)
                wab = sbuf(s4, "wab", [128, 4, D], BF16)
                selb = sbuf(s4, "selb", [128, 2], F32)
                y0 = sbuf(s4, "y0", [128, 4, 128], BF16)
                y1 = sbuf(s4, "y1", [128, 4, 128], BF16)
                yab = sbuf(s4, "yab", [128, 4, 128], BF16)
                m = sbuf(s4, "m4", [128, D], F32)
                mT = sbuf(s4, "mT4", [128, KC, 128], BF16)
                P.dma("pool", wab[:], wa.rearrange("(fc p) n -> p fc n", p=128), w=["wab"])
                wbb = sbuf(s4, "wbb", [128, 4, D], BF16)
                P.dma("pool", wbb[:], wb.rearrange("(fc p) n -> p fc n", p=128), w=["wbb"])
                P.dma("sp", selb[:], selv, w=["selb"])
                P.dma("pool", wgb[:], wgab.rearrange("(kc p) n -> p kc n", p=128), w=["wgb"])
                P.dma("pool", wob[:], wo.rearrange("(kc p) n -> p kc n", p=128), w=["wob"])
                P.dma("sp", wr32[:], wr.rearrange("(kc p) n -> p kc n", p=128), w=["wr32"])
                P.dma("sp", brb[:], br[0].partition_broadcast(128), w=["brb"])

                def tail_tile(j):
                    x_src, npart, _ = tiles[j]
                    sl = j % 2
                    kx = f"x4_{sl}"
                    kh = f"h4_{sl}"
                    P.dma("sp", xt[sl][:npart, :], x_src, w=[kx])
                    norm_transpose(xt[sl][:npart, :], kx, npart, junk, ss, rstd, xn, pA[0], "pA0",
                                   g1T, "g1T", [(xnT, "xnT4")], "t4")
                    for gi, (dst, kd) in enumerate(((sga, "sga"), (sgb, "sgb"))):
                        for nh in range(2):
                            pb = pB[(gi * 2 + nh) % 4]
                            kpb = f"pB{(gi * 2 + nh) % 4}"
                            c0 = gi * 1024 + nh * 512
                            for kc in range(KC):
                                P.op("pe", lambda e, kc=kc, pb=pb, c0=c0: e.matmul(
                                    out=pb[:npart, :], lhsT=xnT[:, kc, :npart], rhs=wgb[:, kc, c0:c0 + 512],
                                    start=(kc == 0), stop=(kc == KC - 1)), r=["xnT4", "wgb"], w=[kpb])
                            P.op("act", lambda e, pb=pb, dst=dst, nh=nh: e.activation(
                                out=dst[:npart, nh * 512:(nh + 1) * 512], in_=pb[:npart, :], func=AF.Sigmoid),
                                 r=[kpb], w=[kd])
                    if "gdn" in phases:
                        if j < NTH:
                            P.dma("sp", y0[:], yaT_scr[:, :, j * 128:(j + 1) * 128], w=["y0"])
                            P.dma("sp", y1[:], yaT_scr[:, :, LH + j * 128:LH + (j + 1) * 128], w=["y1"])
                            P.op("pool", lambda e: e.tensor_scalar(out=y0[:], in0=y0[:], scalar1=selb[:, 0:1], scalar2=None, op0=ALU.mult),
                                 r=["y0", "selb"], w=["y0"])
                            P.op("dve", lambda e: e.scalar_tensor_tensor(out=yab[:], in0=y1[:], scalar=selb[:, 1:2], in1=y0[:],
                                                                         op0=ALU.mult, op1=ALU.add), r=["y0", "y1", "selb"], w=["yab"])
                        else:
                            P.dma("sp", yab[:, :, :npart], yaT_scr[:, :, L:L + npart], w=["yab"])
                        for nh in range(2):
                            for fc in range(4):
                                P.op("pe", lambda e, nh=nh, fc=fc: e.matmul(
                                    out=pA[1][:npart, nh * 512:(nh + 1) * 512], lhsT=yab[:, fc, :npart],
                                    rhs=wab[:, fc, nh * 512:(nh + 1) * 512], start=(fc == 0), stop=(fc == 3)),
                                     r=["yab", "wab"], w=["pA1"])
                        P.op("dve", lambda e: e.tensor_tensor(out=m[:npart, :], in0=sga[:npart, :], in1=pA[1][:npart, :], op=ALU.mult),
                             r=["sga", "pA1"], w=["m4"])
                        if ("nsa" in phases and j < NTH) or ("nsas" in phases and j == NTH):
                            if j < NTH:
                                P.dma("sp", y0[:], ybT_scr[:, :, j * 128:(j + 1) * 128], w=["y0"])
                                P.dma("sp", y1[:], ybT_scr[:, :, LH + j * 128:LH + (j + 1) * 128], w=["y1"])
                                P.op("pool", lambda e: e.tensor_scalar(out=y0[:], in0=y0[:], scalar1=selb[:, 0:1], scalar2=None, op0=ALU.mult),
                                     r=["y0", "selb"], w=["y0"])
                                P.op("dve", lambda e: e.scalar_tensor_tensor(out=yab[:], in0=y1[:], scalar=selb[:, 1:2], in1=y0[:],
                                                                            op0=ALU.mult, op1=ALU.add), r=["y0", "y1", "selb"], w=["yab"])
                            else:
                                P.dma("sp", yab[:, :, :npart], ybT_scr[:, :, L:L + npart], w=["yab"])
                            for nh in range(2):
                                for fc in range(4):
                                    P.op("pe", lambda e, nh=nh, fc=fc: e.matmul(
                                        out=pA[1][:npart, nh * 512:(nh + 1) * 512], lhsT=yab[:, fc, :npart],
                                        rhs=wbb[:, fc, nh * 512:(nh + 1) * 512], start=(fc == 0), stop=(fc == 3)),
                                         r=["yab", "wbb"], w=["pA1"])
                            P.op("dve", lambda e: e.tensor_tensor(out=hn[:npart, :], in0=sgb[:npart, :], in1=pA[1][:npart, :], op=ALU.mult),
                                 r=["sgb", "pA1"], w=["xnt4b"])
                            P.op("dve", lambda e: e.tensor_tensor(out=m[:npart, :], in0=m[:npart, :], in1=hn[:npart, :], op=ALU.add),
                                 r=["m4", "xnt4b"], w=["m4"])
                        for kc in range(KC):
                            P.op("pe", lambda e, kc=kc: e.transpose(out=pA[0][:, kc * 128:kc * 128 + npart],
                                                                    in_=m[:npart, kc * 128:(kc + 1) * 128],
                                                                    identity=ident[:npart, :npart]), r=["m4", "ident"], w=["pA0"])
                        P.op("act", lambda e: e.copy(out=mT[:, :, :npart],
                                                     in_=pA[0][:].rearrange("p (kc t) -> p kc t", kc=KC)[:, :, :npart]),
                             r=["pA0"], w=["mT4"])
                        for nh in range(2):
                            for kc in range(KC):
                                P.op("pe", lambda e, nh=nh, kc=kc: e.matmul(
                                    out=pA[1][:npart, nh * 512:(nh + 1) * 512], lhsT=mT[:, kc, :npart],
                                    rhs=wob[:, kc, nh * 512:(nh + 1) * 512], start=(kc == 0), stop=(kc == KC - 1)),
                                     r=["mT4", "wob"], w=["pA1"])
                        P.op("dve", lambda e: e.tensor_tensor(out=h[sl][:npart, :], in0=xt[sl][:npart, :], in1=pA[1][:npart, :], op=ALU.add),
                             r=[kx, "pA1"], w=[kh])
                    else:
                        P.op("dve", lambda e: e.tensor_copy(out=h[sl][:npart, :], in_=xt[sl][:npart, :]), r=[kx], w=[kh])
                    P.dma("act", h_scr[j, :npart, :], h[sl][:npart, :], r=[kh], w=[f"h_scr{j}"])
                    norm_transpose(h[sl][:npart, :], kh, npart, junk, ss2, rstd2, hn, pA[1], "pA1",
                                   g2T, "g2T", [(hnT32, "hnT32"), (hnTb[sl], f"hnTb{sl}")], "t4b")
                    P.dma("act", hnT_scr[:, :, j * 128:j * 128 + npart], hnTb[sl][:, :, :npart],
                          r=[f"hnTb{sl}"], w=[f"hnT_scr{j}"])
                    for kc in range(KC):
                        P.op("pe", lambda e, kc=kc: e.matmul(out=pB[0][:npart, 0:36], lhsT=hnT32[:, kc, :npart],
                                                             rhs=wr32[:, kc, :], start=(kc == 0), stop=(kc == KC - 1)),
                             r=["hnT32", "wr32"], w=["pB0"])
                    n = npart
                    P.op("dve", lambda e: e.tensor_tensor(out=lg[:n, :], in0=pB[0][:n, 0:36], in1=brb[:n, :], op=ALU.add),
                         r=["pB0", "brb"], w=["lg"])
                    P.op("dve", lambda e: e.tensor_reduce(out=sm[:n, 0:1], in_=lg[:n, 0:4], axis=AX.X, op=ALU.max),
                         r=["lg"], w=["sm"])
                    P.op("dve", lambda e: e.tensor_scalar(out=sm[:n, 4:8], in0=lg[:n, 0:4], scalar1=sm[:n, 0:1], scalar2=None,
                                                          op0=ALU.is_equal), r=["lg", "sm"], w=["sm"])
                    P.op("dve", lambda e: e.tensor_scalar(out=sm[:n, 1:2], in0=sm[:n, 0:1], scalar1=-1.0, scalar2=None,
                                                          op0=ALU.mult), r=["sm"], w=["sm"])
                    P.op("act", lambda e: e.activation(out=sm[:n, 56:60], in_=lg[:n, 0:4], func=AF.Exp, bias=sm[:n, 1:2],
                                                       scale=1.0, accum_out=sm[:n, 2:3]), r=["lg", "sm"], w=["sm"])
                    P.op("dve", lambda e: e.reciprocal(out=sm[:n, 3:4], in_=sm[:n, 2:3]), r=["sm"], w=["sm"])
                    P.op("dve", lambda e: e.tensor_scalar(out=sm[:n, 8:16], in0=lg[:n, 4:12], scalar1=sm[:n, 4:5], scalar2=None,
                                                          op0=ALU.mult), r=["lg", "sm"], w=["sm"])
                    for g in range(1, 4):
                        P.op("dve", lambda e, g=g: e.scalar_tensor_tensor(
                            out=sm[:n, 8:16], in0=lg[:n, 4 + 8 * g:12 + 8 * g], scalar=sm[:n, 4 + g:5 + g],
                            in1=sm[:n, 8:16], op0=ALU.mult, op1=ALU.add), r=["lg", "sm"], w=["sm"])
                    P.op("dve", lambda e: e.max(out=sm[:n, 16:24], in_=sm[:n, 8:16]), r=["sm"], w=["sm"])
                    P.op("dve", lambda e: e.tensor_tensor(out=sm[:n, 24:25], in0=sm[:n, 16:17], in1=sm[:n, 17:18],
                                                          op=ALU.subtract), r=["sm"], w=["sm"])
                    P.op("act", lambda e: e.activation(out=sm[:n, 25:26], in_=sm[:n, 24:25], func=AF.Sigmoid),
                         r=["sm"], w=["sm"])
                    P.op("dve", lambda e: e.tensor_scalar(out=sm[:n, 26:27], in0=sm[:n, 25:26], scalar1=-1.0, scalar2=1.0,
                                                          op0=ALU.mult, op1=ALU.add), r=["sm"], w=["sm"])
                    P.op("dve", lambda e: e.tensor_scalar(out=sm[:n, 27:29], in0=sm[:n, 25:27], scalar1=sm[:n, 3:4], scalar2=None,
                                                          op0=ALU.mult), r=["sm"], w=["sm"])
                    P.op("dve", lambda e: e.tensor_scalar(out=sm[:n, 32:40], in0=sm[:n, 8:16], scalar1=sm[:n, 16:17],
                                                          scalar2=sm[:n, 27:28], op0=ALU.is_equal, op1=ALU.mult),
                         r=["sm"], w=["sm"])
                    P.op("dve", lambda e: e.tensor_scalar(out=sm[:n, 40:48], in0=sm[:n, 8:16], scalar1=sm[:n, 17:18],
                                                          scalar2=sm[:n, 28:29], op0=ALU.is_equal, op1=ALU.mult),
                         r=["sm"], w=["sm"])
                    P.op("dve", lambda e: e.tensor_tensor(out=sm[:n, 48:56], in0=sm[:n, 32:40], in1=sm[:n, 40:48], op=ALU.add),
                         r=["sm"], w=["sm"])
                    for g in range(4):
                        P.op("dve", lambda e, g=g: e.tensor_scalar(out=comb[:n, j, 8 * g:8 * g + 8], in0=sm[:n, 48:56],
                                                                   scalar1=sm[:n, 4 + g:5 + g], scalar2=None, op0=ALU.mult),
                             r=["sm"], w=[f"comb{j}"])

                for j in range(NTT):
                    tail_tile(j)
                P.run()

        def phase_tail_b():
            with ExitStack() as s5:
                gfb = sbuf(s5, "gfb", [128, D], F32)
                P.dma("sp", gfb[:], gf[0].partition_broadcast(128), w=["gfb"])
                NACC = TPP + 1
                acc = [sbuf(s5, f"acc{j}", [128, D], F32) for j in range(NACC)]
                hnTp = sbuf(s5, "hnTp", [128, KC, NACC * 128], BF16)
                wgE = [sbuf(s5, f"wgE{j}", [128, KC, DEXP], BF16) for j in range(2)]
                wuE = [sbuf(s5, f"wuE{j}", [128, KC, DEXP], BF16) for j in range(2)]
                wdE = [sbuf(s5, f"wdE{j}", [128, 2, D], BF16) for j in range(2)]
                sg = [sbuf(s5, f"sg{j}", [128, 512], F32) for j in range(2)]
                hT = sbuf(s5, "hT", [128, 2, 512], BF16)
                junk = sbuf(s5, "junk5", [128, D], F32)
                ss = sbuf(s5, "ss5", [128, 1], F32)
                rstd = sbuf(s5, "rstd5", [128, 1], F32)
                yo = [sbuf(s5, f"yo{j}", [128, D], F32) for j in range(2)]

                passes = []
                j = 0
                while j < NTH:
                    passes.append(list(range(j, min(NTH, j + TPP))))
                    j += TPP
                passes[-1].append(NTH)
                wcount = [0]

                def moe_pass(tl):
                    col0 = {}
                    c = 0
                    for a, tj in enumerate(tl):
                        col0[tj] = c
                        npart = tiles[tj][1]
                        P.dma("sp", acc[a][:npart, :], h_scr[tj, :npart, :], r=[f"h_scr{tj}"], w=[f"acc{a}"])
                        P.dma("act", hnTp[:, :, c:c + npart], hnT_scr[:, :, tj * 128:tj * 128 + npart],
                              r=[f"hnT_scr{tj}"], w=["hnTp"])
                        c += npart
                    groups = []
                    cur, curN = [], 0
                    for a, tj in enumerate(tl):
                        npart = tiles[tj][1]
                        if curN + npart > 512:
                            groups.append(cur)
                            cur, curN = [], 0
                        cur.append((a, tj))
                        curN += npart
                    groups.append(cur)
                    for ex in range(NEXP):
                        wb_ = wcount[0] % 2
                        wcount[0] += 1
                        P.dma("pool", wgE[wb_][:], weg[ex].rearrange("(kc p) f -> p kc f", p=128), w=[f"wgE{wb_}"])
                        P.dma("pool", wuE[wb_][:], weu[ex].rearrange("(kc p) f -> p kc f", p=128), w=[f"wuE{wb_}"])
                        P.dma("pool", wdE[wb_][:], wed[ex].rearrange("(fc p) d -> p fc d", p=128), w=[f"wdE{wb_}"])
                        for grp in groups:
                            g0 = col0[grp[0][1]]
                            N = sum(tiles[tj][1] for (_, tj) in grp)
                            for fc in range(2):
                                for (wt, kw, pb, kpb) in ((wgE[wb_], f"wgE{wb_}", pB[fc], f"pB{fc}"),
                                                          (wuE[wb_], f"wuE{wb_}", pB[2 + fc], f"pB{2 + fc}")):
                                    for kc in range(KC):
                                        P.op("pe", lambda e, kc=kc, wt=wt, pb=pb, fc=fc, g0=g0, N=N: e.matmul(
                                            out=pb[:, 0:N], lhsT=wt[:, kc, fc * 128:(fc + 1) * 128],
                                            rhs=hnTp[:, kc, g0:g0 + N], start=(kc == 0), stop=(kc == KC - 1)),
                                             r=[kw, "hnTp"], w=[kpb])
                                P.op("act", lambda e, fc=fc, N=N: e.activation(out=sg[fc][:, 0:N], in_=pB[fc][:, 0:N],
                                                                               func=AF.Silu), r=[f"pB{fc}"], w=[f"sg{fc}"])
                                P.op("dve", lambda e, fc=fc, N=N: e.tensor_tensor(out=hT[:, fc, 0:N], in0=sg[fc][:, 0:N],
                                                                                  in1=pB[2 + fc][:, 0:N], op=ALU.mult),
                                     r=[f"sg{fc}", f"pB{2 + fc}"], w=[f"hT{fc}"])
                            for gi_, (a, tj) in enumerate(grp):
                                npart = tiles[tj][1]
                                tc0 = col0[tj] - g0
                                pa = pA[gi_ % 2]
                                kpa = f"pA{gi_ % 2}"
                                for nh in range(2):
                                    for fc in range(2):
                                        P.op("pe", lambda e, nh=nh, fc=fc, pa=pa, tc0=tc0, npart=npart, wb_=wb_: e.matmul(
                                            out=pa[:npart, nh * 512:(nh + 1) * 512], lhsT=hT[:, fc, tc0:tc0 + npart],
                                            rhs=wdE[wb_][:, fc, nh * 512:(nh + 1) * 512], start=(fc == 0), stop=(fc == 1)),
                                             r=[f"hT{fc}", f"wdE{wb_}"], w=[kpa])
                                P.op("dve", lambda e, a=a, tj=tj, pa=pa, npart=npart, ex=ex: e.scalar_tensor_tensor(
                                    out=acc[a][:npart, :], in0=pa[:npart, :], scalar=comb[:npart, tj, ex:ex + 1],
                                    in1=acc[a][:npart, :], op0=ALU.mult, op1=ALU.add),
                                     r=[kpa, f"comb{tj}", f"acc{a}"], w=[f"acc{a}"])
                    for a, tj in enumerate(tl):
                        npart = tiles[tj][1]
                        o = a % 2
                        rms_stats(acc[a][:npart, :], npart, junk, ss, rstd, f"acc{a}", "f5")
                        P.op("dve", lambda e, a=a, npart=npart, o=o: e.scalar_tensor_tensor(
                            out=yo[o][:npart, :], in0=acc[a][:npart, :], scalar=rstd[:npart, 0:1], in1=gfb[:npart, :],
                            op0=ALU.mult, op1=ALU.mult), r=[f"acc{a}", "rstdf5", "gfb"], w=[f"yo{o}"])
                        P.dma("sp", tiles[tj][2], yo[o][:npart, :], r=[f"yo{o}"])

                for tl in passes:
                    moe_pass(tl)
                P.run()

        if "gdn" in phases:
            phase_gdn()
        if "nsa" in phases:
            phase_nsa()
        if "nsas" in phases:
            phase_nsas()
        if "tail" in phases:
            phase_tail_a()
            phase_tail_b()
        P.finish("sp")
        P.run()
    return nc


def make_in_maps(inputs, L=8192, NSEQ=4, TS=8, WINB=512, ncores=NCORES, PAST=16384):
    g = lambda k: np.asarray(inputs[k])
    w_in = g("w_in")[0]
    LH = L // 2
    tr = lambda v: np.ascontiguousarray(v.reshape(KC, 128).T)
    g1 = tr(g("norm1_g")[0])
    g2 = tr(g("norm2_g")[0])
    gf = np.ascontiguousarray(g("norm_f_g").reshape(1, D))
    ident = np.eye(128, dtype=np.float32)
    ws = np.ascontiguousarray(np.concatenate([w_in[:, O_QKV:O_QKV + 1536], w_in[:, O_B:O_B + 8], w_in[:, O_KV:O_KV + 768]], axis=1))
    wgab = np.ascontiguousarray(w_in[:, O_GA:O_GA + 2048])
    wr = np.ascontiguousarray(np.concatenate([g("w_grp")[0], g("w_rt")[0]], axis=1))
    br = np.ascontiguousarray(np.concatenate([g("b_grp")[0], g("b_rt")[0]]).reshape(1, 36))
    wz_ = np.ascontiguousarray(w_in[:, O_Z:O_Z + 512])
    cw_ = np.ascontiguousarray(g("gdn_conv_w")[0].reshape(4, 12, 128).transpose(2, 1, 0))
    gvec_ = np.ascontiguousarray(np.concatenate([g("gdn_a_log")[0], g("gdn_dt_bias")[0], g("gdn_norm_g")[0]]).reshape(1, 136))
    ii = np.arange(128)
    cmask_ = np.ascontiguousarray(np.stack([(ii[:, None] <= ii[None, :]).astype(np.float32),
                                            -(ii[:, None] < ii[None, :]).astype(np.float32),
                                            np.ones((128, 128), np.float32)], axis=1))
    NT = L // 128
    NCT = max(1, (8 * NT + 127) // 128)
    NSB = 2 * NT
    NC = 8 * NT - 1
    wn_ = np.ascontiguousarray(np.concatenate([w_in[:, O_QN:O_QN + 512], w_in[:, O_GN:O_GN + 24]], axis=1))
    tt = np.arange(L)
    qaug_ = np.zeros((2, 4, 4, L), np.float32)
    for gg in range(2):
        for hh in range(4):
            sl_ = 2.0 ** -(4 * gg + hh + 1)
            qaug_[gg, 0, hh] = -sl_ * 128 * (tt // 128)
            qaug_[gg, 1, hh] = -sl_ * (tt % 128)
            qaug_[gg, 2, hh] = sl_ * 128
            qaug_[gg, 3, hh] = sl_
    kaug_ = np.stack([np.ones(L), np.ones(L), tt // 128, tt % 128]).astype(np.float32)
    cend = 16 * np.arange(NCT * 128) + 31
    caug_ = np.stack([np.ones(NCT * 128), np.ones(NCT * 128), cend // 128, cend % 128]).astype(np.float32)
    caug_[:, NC:] = 0
    cc = np.arange(NCT * 128)[:, None]
    sj = np.arange(NSB)[None, :]
    ovf = np.maximum(np.minimum(16 * cc + 32, 64 * (sj + 1)) - np.maximum(16 * cc, 64 * sj), 0).astype(np.float32) / 32.0
    ovf[NC:] = 0
    ovt_ = np.ascontiguousarray(ovf.reshape(NCT, 128, NSB).transpose(1, 0, 2))
    cl_ = ii[:, None, None]; mm_ = np.arange(17)[None, :, None]; rr_ = ii[None, None, :]
    cmsk_ = (16 * cl_ - rr_ <= 128 * mm_ - 31).astype(np.float32)
    jrel = np.arange(2 * NSB)[None, :] - NSB
    rlo = (ii[:, None] < 64)
    BIGV = np.float32(1.0e30)
    ta_ = np.where(jrel <= -2, 1.0, np.where(jrel == -1, (~rlo).astype(np.float32), 0.0)).astype(np.float32)
    tb_ = np.where(jrel <= -2, 0.0, np.where(jrel == -1, 1.0e4 * rlo, np.where(jrel == 0, 1.0e4,
                   np.where(jrel == 1, np.where(rlo, -BIGV, 1.0e4), -BIGV)))).astype(np.float32)
    tab_ = np.ascontiguousarray(np.stack([np.broadcast_to(ta_, (128, 2 * NSB)), tb_], axis=1))
    tri2_ = np.ascontiguousarray(np.stack([(ii[:, None] <= ii[None, :]), (ii[None, :] <= ii[:, None])], axis=1).astype(np.float32))
    expt_ = (ii[:, None, None] == 2 * np.arange(NT)[None, :, None] + (ii[None, None, :] >= 64)).astype(np.float32)
    pwm_ = np.zeros((128, 4, 8), np.float32)
    for wi, key in enumerate(("cmp_pos_wk", "cmp_pos_wv")):
        pw = g(key)[0]
        for jb in range(8):
            for pp in range(32):
                if 16 * jb + pp < 128:
                    pwm_[16 * jb + pp, 2 * wi, jb] = pw[pp]
        pwm_[0:16, 2 * wi + 1, 7] = pw[16:32]
    wcmp_ = np.ascontiguousarray(np.stack([g("cmp_wk")[0], g("cmp_wv")[0]], axis=1))
    NPG = PAST // 128
    NKT = NPG + 1
    NCTS = (8 * NPG + 127) // 128
    NSBS = 2 * NPG + 1
    NCS = 8 * NPG - 1
    ckv_ = g("cache_kv")[0].reshape(-1, 512)
    piota_ = np.arange(128, dtype=np.float32).reshape(128, 1)
    qaugs_ = np.zeros((2, 4, 4, TS), np.float32)
    rr8 = np.arange(TS)
    for gg in range(2):
        for hh in range(4):
            sl_ = 2.0 ** -(4 * gg + hh + 1)
            qaugs_[gg, 0, hh] = -sl_ * 128 * NPG
            qaugs_[gg, 1, hh] = -sl_ * rr8
            qaugs_[gg, 2, hh] = sl_ * 128
            qaugs_[gg, 3, hh] = sl_
    tk = np.arange(NKT * 128)
    kaugs_ = np.stack([np.ones(NKT * 128), np.ones(NKT * 128), tk // 128, tk % 128]).astype(np.float32)
    cends = 16 * np.arange(NCTS * 128) + 31
    caugs_ = np.stack([np.ones(NCTS * 128), np.ones(NCTS * 128), cends // 128, cends % 128]).astype(np.float32)
    caugs_[2, NCS:] = -256.0
    caugs_[3, NCS:] = 0.0
    ccs = np.arange(NCTS * 128)[:, None]
    sjs = np.arange(NSBS)[None, :]
    ovfs = np.maximum(np.minimum(16 * ccs + 32, 64 * (sjs + 1)) - np.maximum(16 * ccs, 64 * sjs), 0).astype(np.float32) / 32.0
    ovfs[NCS:] = 0
    ovs_ = np.ascontiguousarray(ovfs.reshape(NCTS, 128, NSBS).transpose(1, 0, 2))
    expts_ = (ii[:, None, None] == 2 * np.arange(64)[None, :, None] + (ii[None, None, :] >= 64)).astype(np.float32)
    maps = []
    for c in range(ncores):
        b, s = c // 2, c % 2
        cols = []
        for base in (0, 512, 1024):
            cols.append(w_in[:, O_QKV + base + 256 * s:O_QKV + base + 256 * s + 256])
        cols.append(w_in[:, O_B + 2 * s:O_B + 2 * s + 2])
        cols.append(w_in[:, O_A + 2 * s:O_A + 2 * s + 2])
        for j in range(6):
            cols.append(w_in[:, O_KV + j * 128 + s * 64:O_KV + j * 128 + s * 64 + 64])
        wp = np.ascontiguousarray(np.concatenate(cols, axis=1))
        selv = np.zeros((128, 2), np.float32)
        selv[:, 0] = 1 - s
        selv[:, 1] = s
        xpb = g("x_prompt")[b, :L]
        maps.append({
            "xp": np.ascontiguousarray(xpb),
            "xh": np.ascontiguousarray(xpb[s * LH:(s + 1) * LH]),
            "xs": np.ascontiguousarray(g("x_sample")[NSEQ * c:NSEQ * (c + 1)].reshape(NSEQ * TS, D)),
            "g1": g1, "g2": g2, "gf": gf, "wp": wp, "ws": ws, "wgab": wgab,
            "wa": g("w_branch_a")[0], "wb": g("w_branch_b")[0], "wo": g("w_out")[0], "wr": wr, "br": br,
            "weg": g("w_e_gate")[0], "weu": g("w_e_up")[0], "wed": g("w_e_down")[0],
            "cwin": np.ascontiguousarray(g("cache_win")[0, NSEQ * c:NSEQ * (c + 1)].reshape(NSEQ, WINB, 256)),
            "ident": ident, "selv": selv,
            "wz": wz_, "cw": cw_, "gvec": gvec_, "cmask": cmask_,
            "wn": wn_, "qaug": qaug_, "kaug": kaug_, "caug": caug_, "ovt": ovt_, "cmsk": cmsk_, "tab": tab_, "tri2": tri2_,
            "expt": expt_, "pwm": pwm_, "wcmp": wcmp_,
            "ckv": ckv_, "ptab": np.ascontiguousarray(g("page_table")[NSEQ * c:NSEQ * (c + 1)].astype(np.int32)), "piota": piota_,
            "cwin4": np.ascontiguousarray(g("cache_win")[0, NSEQ * c:NSEQ * (c + 1)].reshape(NSEQ, 4, 128, 256)),
            "qaugs": qaugs_, "kaugs": kaugs_, "caugs": caugs_, "ovs": ovs_, "expts": expts_,
            "sgdn": np.ascontiguousarray(g("state_gdn")[0, NSEQ * c:NSEQ * (c + 1)].reshape(NSEQ * 4, 128, 128)),
            "sconv": np.ascontiguousarray(g("state_conv")[0, NSEQ * c:NSEQ * (c + 1)]),
        })
    return maps


def assemble(results, L=8192, NSEQ=4, TS=8, WINB=512):
    nb = NCORES // 2
    nsq = NCORES * NSEQ
    LH = L // 2
    y_prompt = np.zeros((nb, L, D), np.float32)
    y_sample = np.zeros((nsq, TS, D), np.float32)
    kv_prompt = np.zeros((1, nb, L, 4, 2, 64), np.float32)
    kv_sample = np.zeros((1, nsq, TS, 4, 2, 64), np.float32)
    win_prompt = np.zeros((1, nb, WINB, 2, 2, 64), np.float32)
    win_sample = np.zeros((1, nsq, WINB, 2, 2, 64), np.float32)
    gdn_prompt = np.zeros((1, nb, 4, 128, 128), np.float32)
    gdn_sample = np.zeros((1, nsq, 4, 128, 128), np.float32)
    conv_prompt = np.zeros((1, nb, 3, 1536), np.float32)
    conv_sample = np.zeros((1, nsq, 3, 1536), np.float32)
    for c, r in enumerate(results):
        b, s = c // 2, c % 2
        y_prompt[b, s * LH:(s + 1) * LH] = r["yp"]
        y_sample[NSEQ * c:NSEQ * (c + 1)] = r["ysm"].reshape(NSEQ, TS, D)
        if s == 1:
            gdn_prompt[0, b] = r["gdnp"]
        gdn_sample[0, NSEQ * c:NSEQ * (c + 1)] = r["gdns"].reshape(NSEQ, 4, 128, 128)
        kv_prompt[0, b, :, :, s, :] = r["kvp"]
        win_prompt[0, b, :, :, s, :] = r["winp"]
        cp = r["convp"]
        for j, base in enumerate((0, 512, 1024)):
            conv_prompt[0, b, :, base + 256 * s:base + 256 * s + 256] = cp[:, 256 * j:256 * j + 256]
        kv_sample[0, NSEQ * c:NSEQ * (c + 1)] = r["kvs"].reshape(NSEQ, TS, 4, 2, 64)
        win_sample[0, NSEQ * c:NSEQ * (c + 1)] = r["wins"].reshape(NSEQ, WINB, 2, 2, 64)
        conv_sample[0, NSEQ * c:NSEQ * (c + 1)] = r["convs"]
    return (y_prompt, y_sample, kv_prompt, kv_sample, win_prompt, win_sample,
            gdn_prompt, gdn_sample, conv_prompt, conv_sample)


def kernel(**inputs):
    nc = build_nc()
    in_maps = make_in_maps(inputs)
    res = run_bass_kernel_spmd(nc, in_maps, core_ids=list(range(NCORES)))
    return assemble(res.results)
```

```python
import numpy as np
from contextlib import ExitStack
import concourse.bass as bass
import concourse.mybir as mybir
from concourse.alu_op_type import AluOpType as ALU
from concourse.bass_utils import run_bass_kernel_spmd

F32 = mybir.dt.float32
BF16 = mybir.dt.bfloat16
I32 = mybir.dt.int32
AF = mybir.ActivationFunctionType
AX = mybir.AxisListType

D = 1024
KC = D // 128
EPS = 1e-6
NCORES = 8
NEXP = 32
DEXP = 256

O_QKV, O_Z, O_B, O_A, O_QN, O_KV, O_GN, O_GA, O_GB = 0, 1536, 2048, 2052, 2056, 2568, 3336, 3360, 4384

N_DMA_SEMS = 40


class Prog:
    def __init__(self, nc, stack):
        self.nc = nc
        self.stack = stack
        self.q = {e: [] for e in ("pe", "dve", "act", "pool", "sp")}
        self.sems = {}
        self.cnt = {}
        self.waited = {}
        self.lastw = {}
        self.readers = {}
        self.dma_rr = 0
        self.dma_rr_g = 0

    def sem(self, name):
        if name not in self.sems:
            self.sems[name] = self.stack.enter_context(self.nc.semaphore(name))
            self.cnt[name] = 0
        return self.sems[name]

    def _auto(self, r, w):
        deps = []
        for k in r:
            if k in self.lastw:
                deps.append(self.lastw[k])
        for k in w:
            if k in self.lastw:
                deps.append(self.lastw[k])
            deps.extend(self.readers.get(k, ()))
        return deps

    def _commit(self, ev, r, w):
        for k in r:
            self.readers.setdefault(k, []).append(ev)
        for k in w:
            self.lastw[k] = ev
            self.readers[k] = []

    def _waits(self, eng, deps):
        best = {}
        for d in deps:
            if d is None:
                continue
            name, val = d
            if eng == "pe" and name == "E_pe":
                continue
            if val > best.get(name, 0):
                best[name] = val
        out = []
        for name, val in best.items():
            if self.waited.get((eng, name), 0) >= val:
                continue
            self.waited[(eng, name)] = val
            out.append((self.sem(name), val))
        return out

    def op(self, eng, fn, r=(), w=(), deps=()):
        waits = self._waits(eng, list(deps) + self._auto(r, w))
        name = "E_" + eng
        s = self.sem(name)
        self.cnt[name] += 1
        val = self.cnt[name]

        def thunk(e):
            for (sm, v) in waits:
                e.wait_ge(sm, v)
            fn(e).then_inc(s, 1)

        self.q[eng].append(thunk)
        ev = (name, val)
        self._commit(ev, r, w)
        return ev

    def dma(self, eng, out, in_, r=(), w=(), deps=(), **kw):
        if eng == "pool":
            chan = f"QG{self.dma_rr_g % 16}"
            self.dma_rr_g += 1
        else:
            chan = f"Q{self.dma_rr % N_DMA_SEMS}"
            self.dma_rr += 1
        s = self.sem(chan)
        prev = (chan, self.cnt[chan]) if self.cnt[chan] > 0 else None
        waits = self._waits(eng, list(deps) + self._auto(r, w) + [prev])
        self.cnt[chan] += 16
        val = self.cnt[chan]

        def thunk(e):
            for (sm, v) in waits:
                e.wait_ge(sm, v)
            e.dma_start(out=out, in_=in_, **kw).then_inc(s, 16)

        self.q[eng].append(thunk)
        ev = (chan, val)
        self._commit(ev, r, w)
        return ev

    def idma(self, out, in_, idx_ap, r=(), w=(), deps=()):
        eng = "pool"
        chan = f"QG{self.dma_rr_g % 16}"
        self.dma_rr_g += 1
        s = self.sem(chan)
        prev = (chan, self.cnt[chan]) if self.cnt[chan] > 0 else None
        waits = self._waits(eng, list(deps) + self._auto(r, w) + [prev])
        self.cnt[chan] += 16
        val = self.cnt[chan]

        def thunk(e):
            for (sm, v) in waits:
                e.wait_ge(sm, v)
            e.indirect_dma_start(out=out, out_offset=None, in_=in_,
                                 in_offset=bass.IndirectOffsetOnAxis(ap=idx_ap, axis=0)).then_inc(s, 16)

        self.q[eng].append(thunk)
        ev = (chan, val)
        self._commit(ev, r, w)
        return ev

    def barrier(self):
        deps = [(n, c) for n, c in self.cnt.items() if c > 0]
        for eng in ("pe", "dve", "act", "pool", "sp"):
            waits = self._waits(eng, [d for d in deps if d[0] != "E_" + eng])

            def thunk(e, waits=waits):
                for (sm, v) in waits:
                    e.wait_ge(sm, v)

            self.q[eng].append(thunk)

    def finish(self, eng="sp"):
        deps = [(n, c) for n, c in self.cnt.items() if n.startswith("Q") and c > 0]
        waits = self._waits(eng, deps)

        def thunk(e):
            for (sm, v) in waits:
                e.wait_ge(sm, v)

        self.q[eng].append(thunk)

    def run(self):
        nc = self.nc
        self.barrier()
        qs = self.q
        self.q = {e: [] for e in ("pe", "dve", "act", "pool", "sp")}
        self._run(nc, qs)

    def _run(self, nc, q):
        self_q = q
        with nc.Block() as block:
            @block.tensor
            def _(e):
                for f in self_q["pe"]:
                    f(e)

            @block.vector
            def _(e):
                for f in self_q["dve"]:
                    f(e)

            @block.scalar
            def _(e):
                for f in self_q["act"]:
                    f(e)

            @block.gpsimd
            def _(e):
                for f in self_q["pool"]:
                    f(e)

            @block.sync
            def _(e):
                for f in self_q["sp"]:
                    f(e)


def build_nc(L=8192, NSEQ=4, TS=8, WINB=512, TPP=8, phases=("proj", "gdn", "nsa", "nsas", "tail"), PAST=16384, NPHYS=5120):
    NT = L // 128
    LH = L // 2
    NTH = LH // 128
    NSTOK = NSEQ * TS
    NP = 768 + 4 + 384
    P_QKV, P_BA, P_KV = 0, 768, 772
    NS = 1536 + 8 + 768
    S_QKV, S_BA, S_KV = 0, 1536, 1544
    NWT = min(WINB, L) // 128
    NTT = NTH + 1

    nc = bass.Bass("TRN2", target_bir_lowering=False)
    dt = nc.dram_tensor
    xp = dt("xp", [L, D], F32, kind="ExternalInput").ap()
    xh = dt("xh", [LH, D], F32, kind="ExternalInput").ap()
    xs = dt("xs", [NSTOK, D], F32, kind="ExternalInput").ap()
    g1 = dt("g1", [128, KC], F32, kind="ExternalInput").ap()
    g2 = dt("g2", [128, KC], F32, kind="ExternalInput").ap()
    gf = dt("gf", [1, D], F32, kind="ExternalInput").ap()
    wp = dt("wp", [D, NP], F32, kind="ExternalInput").ap()
    ws = dt("ws", [D, NS], F32, kind="ExternalInput").ap()
    wgab = dt("wgab", [D, 2048], F32, kind="ExternalInput").ap()
    wa = dt("wa", [512, D], F32, kind="ExternalInput").ap()
    wb = dt("wb", [512, D], F32, kind="ExternalInput").ap()
    wo = dt("wo", [D, D], F32, kind="ExternalInput").ap()
    wr = dt("wr", [D, 36], F32, kind="ExternalInput").ap()
    br = dt("br", [1, 36], F32, kind="ExternalInput").ap()
    weg = dt("weg", [NEXP, D, DEXP], F32, kind="ExternalInput").ap()
    weu = dt("weu", [NEXP, D, DEXP], F32, kind="ExternalInput").ap()
    wed = dt("wed", [NEXP, DEXP, D], F32, kind="ExternalInput").ap()
    cwin = dt("cwin", [NSEQ, WINB, 256], F32, kind="ExternalInput").ap()
    ident_d = dt("ident", [128, 128], F32, kind="ExternalInput").ap()
    selv = dt("selv", [128, 2], F32, kind="ExternalInput").ap()
    wz = dt("wz", [D, 512], F32, kind="ExternalInput").ap()
    cwd = dt("cw", [128, 12, 4], F32, kind="ExternalInput").ap()
    gvec = dt("gvec", [1, 4 + 4 + 128], F32, kind="ExternalInput").ap()
    cmask_d = dt("cmask", [128, 3, 128], F32, kind="ExternalInput").ap()
    NCT = max(1, (8 * NT + 127) // 128)
    NSB = 2 * NT
    wn = dt("wn", [D, 536], F32, kind="ExternalInput").ap()
    qaug_d = dt("qaug", [2, 4, 4, L], F32, kind="ExternalInput").ap()
    kaug_d = dt("kaug", [4, L], F32, kind="ExternalInput").ap()
    caug_d = dt("caug", [4, NCT * 128], F32, kind="ExternalInput").ap()
    ov_d = dt("ovt", [128, NCT, NSB], F32, kind="ExternalInput").ap()
    cmsk_d = dt("cmsk", [128, 17, 128], F32, kind="ExternalInput").ap()
    tab_d = dt("tab", [128, 2, 2 * NSB], F32, kind="ExternalInput").ap()
    tri2_d = dt("tri2", [128, 2, 128], F32, kind="ExternalInput").ap()
    expt_d = dt("expt", [128, NT, 128], F32, kind="ExternalInput").ap()
    pw_d = dt("pwm", [128, 4, 8], F32, kind="ExternalInput").ap()
    wc_d = dt("wcmp", [64, 2, 64], F32, kind="ExternalInput").ap()
    NPG = PAST // 128
    NKT = NPG + 1
    NCTS = (8 * NPG + 127) // 128
    NSBS = 2 * NPG + 1
    ckv = dt("ckv", [NPHYS * 128, 512], F32, kind="ExternalInput").ap()
    ptab = dt("ptab", [NSEQ, NPG], I32, kind="ExternalInput").ap()
    piota = dt("piota", [128, 1], F32, kind="ExternalInput").ap()
    cwin4 = dt("cwin4", [NSEQ, 4, 128, 256], F32, kind="ExternalInput").ap()
    qaugs_d = dt("qaugs", [2, 4, 4, TS], F32, kind="ExternalInput").ap()
    kaugs_d = dt("kaugs", [4, NKT * 128], F32, kind="ExternalInput").ap()
    caugs_d = dt("caugs", [4, NCTS * 128], F32, kind="ExternalInput").ap()
    ovs_d = dt("ovs", [128, NCTS, NSBS], F32, kind="ExternalInput").ap()
    expts_d = dt("expts", [128, 64, 128], F32, kind="ExternalInput").ap()
    sgdn = dt("sgdn", [NSEQ * 4, 128, 128], F32, kind="ExternalInput").ap()
    sconv = dt("sconv", [NSEQ, 3, 1536], F32, kind="ExternalInput").ap()

    kvp = dt("kvp", [L, 4, 64], F32, kind="ExternalOutput").ap()
    winp = dt("winp", [NWT * 128, 2, 64], F32, kind="ExternalOutput").ap()
    convp = dt("convp", [3, 768], F32, kind="ExternalOutput").ap()
    kvs = dt("kvs", [NSTOK, 512], F32, kind="ExternalOutput").ap()
    wins = dt("wins", [NSEQ, WINB, 256], F32, kind="ExternalOutput").ap()
    convs = dt("convs", [NSEQ, 3, 1536], F32, kind="ExternalOutput").ap()
    yp = dt("yp", [LH, D], F32, kind="ExternalOutput").ap()
    ysm = dt("ysm", [NSTOK, D], F32, kind="ExternalOutput").ap()
    gdnp = dt("gdnp", [4, 128, 128], F32, kind="ExternalOutput").ap()
    gdns = dt("gdns", [NSEQ * 4, 128, 128], F32, kind="ExternalOutput").ap()

    qs_scr = dt("qs_scr", [128, 12, L + NSTOK], F32, kind="Internal").ap()
    zs_scr = dt("zs_scr", [L + NSTOK, 512], F32, kind="Internal").ap()
    bg_scr = dt("bg_scr", [L + NSTOK, 8], F32, kind="Internal").ap()
    yaT_scr = dt("yaT_scr", [128, 4, L + NSTOK], BF16, kind="Internal").ap()
    kvtm_scr = dt("kvtm_scr", [L, 768], BF16, kind="Internal").ap()
    kT_scr = dt("kT_scr", [4, 64, L], BF16, kind="Internal").ap()
    qT_scr = dt("qT_scr", [2, 64, 4, L], BF16, kind="Internal").ap()
    gates_scr = dt("gates_scr", [L, 24], F32, kind="Internal").ap()
    ybT_scr = dt("ybT_scr", [128, 4, L + NSTOK], BF16, kind="Internal").ap()
    qTs_scr = dt("qTs_scr", [2, 64, 4, NSTOK], BF16, kind="Internal").ap()
    gates_s_scr = dt("gates_s_scr", [NSTOK, 24], F32, kind="Internal").ap()
    kvs_scr = dt("kvs_scr", [NSTOK, 768], F32, kind="Internal").ap()
    h_scr = dt("h_scr", [NTT, 128, D], F32, kind="Internal").ap()
    hnT_scr = dt("hnT_scr", [128, KC, NTT * 128], BF16, kind="Internal").ap()

    with ExitStack() as st:
        def sbuf(stack, name, shape, dtype):
            return stack.enter_context(nc.sbuf_tensor(name, shape, dtype))

        def psum(stack, name, shape, dtype):
            return stack.enter_context(nc.psum_tensor(name, shape, dtype))

        P = Prog(nc, st)

        ident = sbuf(st, "ident_sb", [128, 128], F32)
        g1T = sbuf(st, "g1T", [128, KC], F32)
        g2T = sbuf(st, "g2T", [128, KC], F32)
        epsc = sbuf(st, "epsc", [128, 1], F32)
        comb = sbuf(st, "comb", [128, NTT, NEXP], F32)
        onec = sbuf(st, "onec", [128, 1], F32)
        P.op("dve", lambda e: e.memset(onec[:], 1.0), w=["onec"])
        P.op("dve", lambda e: e.memset(epsc[:], EPS), w=["epsc"])
        P.dma("sp", ident[:], ident_d, w=["ident"])
        P.dma("sp", g1T[:], g1, w=["g1T"])
        P.dma("sp", g2T[:], g2, w=["g2T"])

        if "gdn" not in phases:
            zt = sbuf(st, "zt", [128, 128], F32)
            P.op("dve", lambda e: e.memset(zt[:], 0.0), w=["zt"])
            for hh in range(4):
                P.dma("sp", gdnp[hh], zt[:], r=["zt"])
            for hh in range(NSEQ * 4):
                P.dma("sp", gdns[hh], zt[:], r=["zt"])

        pA = [psum(st, f"pA{j}", [128, 1024], F32) for j in range(2)]
        pB = [psum(st, f"pB{j}", [128, 512], F32) for j in range(4)]

        def rms_stats(x_ap, npart, junk, ss, rstd, kx, tag):
            P.op("act", lambda e: e.activation(out=junk[:npart, :], in_=x_ap, func=AF.Square,
                                               accum_out=ss[:npart, :]), r=[kx], w=["junk" + tag, "ss" + tag])
            P.op("act", lambda e: e.activation(out=rstd[:npart, :], in_=ss[:npart, :], func=AF.Sqrt,
                                               bias=epsc[:npart, :], scale=1.0 / D),
                 r=["ss" + tag, "epsc"], w=["rstd" + tag])
            P.op("dve", lambda e: e.reciprocal(out=rstd[:npart, :], in_=rstd[:npart, :]),
                 r=["rstd" + tag], w=["rstd" + tag])

        def norm_transpose(x_ap, kx, npart, junk, ss, rstd, xn, pT, kpT, gT, kg, outs, tag):
            rms_stats(x_ap, npart, junk, ss, rstd, kx, tag)
            P.op("dve", lambda e: e.tensor_scalar(out=xn[:npart, :], in0=x_ap, scalar1=rstd[:npart, 0:1], scalar2=1.0,
                                                  op0=ALU.mult, op1=ALU.mult), r=[kx, "rstd" + tag], w=["xn" + tag])
            for kc in range(KC):
                P.op("pe", lambda e, kc=kc: e.transpose(out=pT[:, kc * 128:kc * 128 + npart],
                                                        in_=xn[:npart, kc * 128:(kc + 1) * 128],
                                                        identity=ident[:npart, :npart]),
                     r=["xn" + tag, "ident"], w=[kpT])
            (o0, k0) = outs[0]
            P.op("dve", lambda e: e.tensor_tensor(
                out=o0[:, :, :npart],
                in0=pT[:].rearrange("p (kc t) -> p kc t", kc=KC)[:, :, :npart],
                in1=gT[:].unsqueeze(2).to_broadcast([128, KC, npart]),
                op=ALU.mult), r=[kpT, kg], w=[k0])
            for (o1, k1) in outs[1:]:
                P.op("act", lambda e, o1=o1: e.copy(out=o1[:, :, :npart], in_=o0[:, :, :npart]), r=[k0], w=[k1])

        def phase_proj():
            with ExitStack() as s1:
                wpb = sbuf(s1, "wpb", [128, KC, NP], BF16)
                wsb = sbuf(s1, "wsb", [128, KC, NS], BF16)
                xt = [sbuf(s1, f"xt{j}", [128, D], F32) for j in range(2)]
                junk = sbuf(s1, "junk", [128, D], F32)
                ss = [sbuf(s1, f"ss{j}", [128, 1], F32) for j in range(2)]
                rstd = [sbuf(s1, f"rstd{j}", [128, 1], F32) for j in range(2)]
                xn = [sbuf(s1, f"xn{j}", [128, D], F32) for j in range(2)]
                xnT = [sbuf(s1, f"xnT{j}", [128, KC, 128], BF16) for j in range(2)]
                kvsb = [sbuf(s1, f"kvsb{j}", [128, 384], F32) for j in range(2)]
                qkvtm = sbuf(s1, "qkvtm", [128, 768], F32)
                souts = sbuf(s1, "souts", [NSTOK, NS], F32)
                P.dma("pool", wpb[:], wp.rearrange("(kc p) n -> p kc n", p=128), w=["wpb"])
                P.dma("pool", wsb[:], ws.rearrange("(kc p) n -> p kc n", p=128), w=["wsb"])
                GDN = "gdn" in phases
                if GDN:
                    wzb = sbuf(s1, "wzb", [128, KC, 512], BF16)
                    cw = sbuf(s1, "cw_sb", [128, 12, 4], F32)
                    gv = sbuf(s1, "gv_sb", [128, 136], F32)
                    nA = sbuf(s1, "nA", [128, 4], F32)
                    pre = sbuf(s1, "pre", [128, 12, 131], F32)
                    cv = sbuf(s1, "cv", [128, 12, 128], F32)
                    cv2 = sbuf(s1, "cv2", [128, 12, 128], F32)
                    qs = sbuf(s1, "qs1", [128, 12, 128], F32)
                    zs = sbuf(s1, "zs1", [128, 512], F32)
                    bg = sbuf(s1, "bg1", [128, 8], F32)
                    tsm = sbuf(s1, "tsm1", [128, 8], F32)
                    P.dma("pool", wzb[:], wz.rearrange("(kc p) n -> p kc n", p=128), w=["wzb"])
                    P.dma("sp", cw[:], cwd, w=["cw"])
                    P.dma("sp", gv[:], gvec[0].partition_broadcast(128), w=["gv"])
                    P.op("act", lambda e: e.activation(out=nA[:], in_=gv[:, 0:4], func=AF.Exp), r=["gv"], w=["nA"])
                    P.op("dve", lambda e: e.tensor_scalar(out=nA[:], in0=nA[:], scalar1=-1.0, scalar2=None, op0=ALU.mult),
                         r=["nA"], w=["nA"])

                def gdn_pre(xnT_ap, kxnT, C, tok0, halo_fn):
                    for fc in range(12):
                        dst = pA[1][:, fc * 128:fc * 128 + C] if fc < 8 else pB[2][:, (fc - 8) * 128:(fc - 8) * 128 + C]
                        kd = "pA1" if fc < 8 else "pB2"
                        for kc in range(KC):
                            P.op("pe", lambda e, kc=kc, dst=dst, fc=fc: e.matmul(
                                out=dst, lhsT=wsb[:, kc, S_QKV + fc * 128:S_QKV + (fc + 1) * 128], rhs=xnT_ap[:, kc, :C],
                                start=(kc == 0), stop=(kc == KC - 1)), r=[kxnT, "wsb"], w=[kd])
                    halo_fn()
                    P.op("act", lambda e: e.copy(out=pre[:, 0:8, 3:3 + C],
                                                 in_=pA[1][:].rearrange("p (c t) -> p c t", c=8)[:, :, :C]),
                         r=["pA1"], w=["pre_a"])
                    P.op("act", lambda e: e.copy(out=pre[:, 8:12, 3:3 + C],
                                                 in_=pB[2][:].rearrange("p (c t) -> p c t", c=4)[:, :, :C]),
                         r=["pB2"], w=["pre_b"])
                    pk = ["pre_a", "pre_b", "pre_h"]
                    P.op("pool", lambda e: e.tensor_tensor(out=cv[:, :, :C], in0=pre[:, :, 0:C],
                                                           in1=cw[:, :, 0:1].to_broadcast([128, 12, C]), op=ALU.mult),
                         r=pk + ["cw"], w=["cv"])
                    P.op("pool", lambda e: e.tensor_tensor(out=cv2[:, :, :C], in0=pre[:, :, 1:1 + C],
                                                           in1=cw[:, :, 1:2].to_broadcast([128, 12, C]), op=ALU.mult),
                         r=pk + ["cw"], w=["cv2"])
                    P.op("dve", lambda e: e.tensor_tensor(out=cv[:, :, :C], in0=cv[:, :, :C], in1=cv2[:, :, :C], op=ALU.add),
                         r=["cv", "cv2"], w=["cv"])
                    P.op("pool", lambda e: e.tensor_tensor(out=cv2[:, :, :C], in0=pre[:, :, 2:2 + C],
                                                           in1=cw[:, :, 2:3].to_broadcast([128, 12, C]), op=ALU.mult),
                         r=pk + ["cw"], w=["cv2"])
                    P.op("dve", lambda e: e.tensor_tensor(out=cv[:, :, :C], in0=cv[:, :, :C], in1=cv2[:, :, :C], op=ALU.add),
                         r=["cv", "cv2"], w=["cv"])
                    P.op("pool", lambda e: e.tensor_tensor(out=cv2[:, :, :C], in0=pre[:, :, 3:3 + C],
                                                           in1=cw[:, :, 3:4].to_broadcast([128, 12, C]), op=ALU.mult),
                         r=pk + ["cw"], w=["cv2"])
                    P.op("dve", lambda e: e.tensor_tensor(out=cv[:, :, :C], in0=cv[:, :, :C], in1=cv2[:, :, :C], op=ALU.add),
                         r=["cv", "cv2"], w=["cv"])
                    P.op("act", lambda e: e.activation(out=qs[:, :, :C], in_=cv[:, :, :C], func=AF.Silu), r=["cv"], w=["qs1"])
                    P.dma("sp", qs_scr[:, :, tok0:tok0 + C], qs[:, :, :C], r=["qs1"], w=[f"qs_scr{tok0}"])

                def gdn_zg(xnT_ap, kxnT, n, tok0):
                    for kc in range(KC):
                        P.op("pe", lambda e, kc=kc: e.matmul(out=pB[3][:n, :], lhsT=xnT_ap[:, kc, :n], rhs=wzb[:, kc, :],
                                                             start=(kc == 0), stop=(kc == KC - 1)), r=[kxnT, "wzb"], w=["pB3"])
                    P.op("act", lambda e: e.activation(out=zs[:n, :], in_=pB[3][:n, :], func=AF.Silu), r=["pB3"], w=["zs1"])
                    P.dma("sp", zs_scr[tok0:tok0 + n, :], zs[:n, :], r=["zs1"], w=[f"zs_scr{tok0}"])
                    for kc in range(KC):
                        P.op("pe", lambda e, kc=kc: e.matmul(out=pB[1][:n, 0:8], lhsT=xnT_ap[:, kc, :n],
                                                             rhs=wsb[:, kc, S_BA:S_BA + 8],
                                                             start=(kc == 0), stop=(kc == KC - 1)), r=[kxnT, "wsb"], w=["pB1"])
                    P.op("act", lambda e: e.activation(out=bg[:n, 0:4], in_=pB[1][:n, 0:4], func=AF.Sigmoid),
                         r=["pB1"], w=["bg1"])
                    P.op("dve", lambda e: e.tensor_tensor(out=tsm[:n, 0:4], in0=pB[1][:n, 4:8], in1=gv[:n, 4:8], op=ALU.add),
                         r=["pB1", "gv"], w=["tsm1"])
                    P.op("act", lambda e: e.activation(out=tsm[:n, 0:4], in_=tsm[:n, 0:4], func=AF.Exp), r=["tsm1"], w=["tsm1"])
                    P.op("act", lambda e: e.activation(out=tsm[:n, 0:4], in_=tsm[:n, 0:4], func=AF.Ln, bias=onec[:n, :]),
                         r=["tsm1", "onec"], w=["tsm1"])
                    P.op("dve", lambda e: e.tensor_tensor(out=bg[:n, 4:8], in0=tsm[:n, 0:4], in1=nA[:n, :], op=ALU.mult),
                         r=["tsm1", "nA", "bg1"], w=["bg1"])
                    P.dma("sp", bg_scr[tok0:tok0 + n, :], bg[:n, :], r=["bg1"], w=[f"bg_scr{tok0}"])

                NSA = "nsa" in phases
                if NSA or "nsas" in phases:
                    wnb = sbuf(s1, "wnb", [128, KC, 536], BF16)
                    kvb = sbuf(s1, "kvb", [128, 768], BF16)
                    kTb = sbuf(s1, "kTb", [64, 4, 128], BF16)
                    qTb = sbuf(s1, "qTb", [64, 8, 128], BF16)
                    gts = sbuf(s1, "gts", [128, 24], F32)
                    P.dma("pool", wnb[:], wn.rearrange("(kc p) n -> p kc n", p=128), w=["wnb"])

                def nsa_pre(i, xT, kxT):
                    t0 = i * 128
                    for half in range(2):
                        for kc in range(KC):
                            P.op("pe", lambda e, kc=kc, half=half: e.matmul(
                                out=pA[1][:, half * 512:half * 512 + 384], lhsT=xT[:, kc, :],
                                rhs=wsb[:, kc, S_KV + half * 384:S_KV + (half + 1) * 384],
                                start=(kc == 0), stop=(kc == KC - 1)), r=[kxT, "wsb"], w=["pA1"])
                    for half in range(2):
                        P.op("act", lambda e, half=half: e.copy(out=kvb[:, half * 384:(half + 1) * 384],
                                                                in_=pA[1][:, half * 512:half * 512 + 384]), r=["pA1"], w=["kvb"])
                    P.dma("act", kvtm_scr[t0:t0 + 128, :], kvb[:], r=["kvb"], w=[f"kvtm{i}"])
                    for idx in range(4):
                        g_, j_ = idx // 2, (2, 4)[idx % 2]
                        c0 = S_KV + j_ * 128 + g_ * 64
                        for kc in range(KC):
                            P.op("pe", lambda e, kc=kc, idx=idx, c0=c0: e.matmul(
                                out=pB[2][0:64, idx * 128:(idx + 1) * 128], lhsT=wsb[:, kc, c0:c0 + 64], rhs=xT[:, kc, :],
                                start=(kc == 0), stop=(kc == KC - 1)), r=[kxT, "wsb"], w=["pB2"])
                    P.op("act", lambda e: e.copy(out=kTb[:], in_=pB[2][0:64, :].rearrange("p (a t) -> p a t", a=4)),
                         r=["pB2"], w=["kTb"])
                    P.dma("act", kT_scr[:, :, t0:t0 + 128].rearrange("a p t -> p a t"), kTb[:], r=["kTb"], w=[f"kTs{i}"])
                    for hh in range(8):
                        for kc in range(KC):
                            P.op("pe", lambda e, kc=kc, hh=hh: e.matmul(
                                out=pA[0][0:64, hh * 128:(hh + 1) * 128], lhsT=wnb[:, kc, hh * 64:(hh + 1) * 64], rhs=xT[:, kc, :],
                                start=(kc == 0), stop=(kc == KC - 1)), r=[kxT, "wnb"], w=["pA0"])
                    P.op("act", lambda e: e.activation(out=qTb[:], in_=pA[0][0:64, :].rearrange("p (a t) -> p a t", a=8),
                                                       func=AF.Copy, scale=0.125), r=["pA0"], w=["qTb"])
                    for g_ in range(2):
                        P.dma("act", qT_scr[g_, :, :, t0:t0 + 128], qTb[:, 4 * g_:4 * g_ + 4, :], r=["qTb"], w=[f"qTs{i}"])
                    for kc in range(KC):
                        P.op("pe", lambda e, kc=kc: e.matmul(out=pB[3][:, 0:24], lhsT=xT[:, kc, :], rhs=wnb[:, kc, 512:536],
                                                             start=(kc == 0), stop=(kc == KC - 1)), r=[kxT, "wnb"], w=["pB3"])
                    P.op("act", lambda e: e.activation(out=gts[:], in_=pB[3][:, 0:24], func=AF.Sigmoid), r=["pB3"], w=["gts"])
                    P.dma("act", gates_scr[t0:t0 + 128, :], gts[:], r=["gts"], w=[f"gates{i}"])

                def prompt_tile(i):
                    sl = i % 2
                    t = str(sl)
                    P.dma("sp", xt[sl][:], xp[i * 128:(i + 1) * 128, :], w=["xt" + t])
                    norm_transpose(xt[sl][:], "xt" + t, 128, junk, ss[sl], rstd[sl], xn[sl], pA[sl], f"pA{sl}",
                                   g1T, "g1T", [(xnT[sl], "xnT" + t)], "p1" + t)
                    for kc in range(KC):
                        P.op("pe", lambda e, kc=kc: e.matmul(out=pB[sl][:, 0:384], lhsT=xnT[sl][:, kc, :],
                                                             rhs=wpb[:, kc, P_KV:P_KV + 384],
                                                             start=(kc == 0), stop=(kc == KC - 1)),
                             r=["xnT" + t, "wpb"], w=[f"pB{sl}"])
                    P.op("act", lambda e: e.copy(out=kvsb[sl][:], in_=pB[sl][:, 0:384]), r=[f"pB{sl}"], w=["kvsb" + t])
                    P.dma("act", kvp[i * 128:(i + 1) * 128, :, :],
                          kvsb[sl][:, 0:256].rearrange("p (j d) -> p j d", j=4), r=["kvsb" + t])
                    if GDN:
                        def halo():
                            if i == 0:
                                P.op("pool", lambda e: e.memset(pre[:, :, 0:3], 0.0), w=["pre_h"])
                            else:
                                P.op("pool", lambda e: e.tensor_copy(out=pre[:, :, 0:3], in_=pre[:, :, 128:131]),
                                     r=["pre_a", "pre_b"], w=["pre_h"])
                        gdn_pre(xnT[sl], "xnT" + t, 128, i * 128, halo)
                        gdn_zg(xnT[sl], "xnT" + t, 128, i * 128)
                    if NSA:
                        nsa_pre(i, xnT[sl], "xnT" + t)
                    if i >= NT - NWT:
                        r0 = (i - (NT - NWT)) * 128
                        P.dma("act", winp[r0:r0 + 128, :, :],
                              kvsb[sl][:, 256:384].rearrange("p (j d) -> p j d", j=2), r=["kvsb" + t])
                    if i == NT - 1:
                        for (c0, c1, pm) in ((0, 512, 2), (512, 768, 3)):
                            for kc in range(KC):
                                P.op("pe", lambda e, kc=kc, c0=c0, c1=c1, pm=pm: e.matmul(
                                    out=pB[pm][:, 0:c1 - c0], lhsT=xnT[sl][:, kc, :],
                                    rhs=wpb[:, kc, P_QKV + c0:P_QKV + c1],
                                    start=(kc == 0), stop=(kc == KC - 1)), r=["xnT" + t, "wpb"], w=[f"pB{pm}"])
                            P.op("act", lambda e, c0=c0, c1=c1, pm=pm: e.copy(out=qkvtm[:, c0:c1],
                                                                             in_=pB[pm][:, 0:c1 - c0]),
                                 r=[f"pB{pm}"], w=["qkvtm"])
                        P.dma("act", convp, qkvtm[125:128, :], r=["qkvtm"])

                for i in range(NT):
                    prompt_tile(i)

                sl = NT % 2
                t = str(sl)
                P.dma("sp", xt[sl][:NSTOK, :], xs, w=["xt" + t])
                norm_transpose(xt[sl][:NSTOK, :], "xt" + t, NSTOK, junk, ss[sl], rstd[sl], xn[sl], pA[sl], f"pA{sl}",
                               g1T, "g1T", [(xnT[sl], "xnT" + t)], "p1" + t)
                nchunk = (NS + 511) // 512
                for ci in range(nchunk):
                    c0, c1 = ci * 512, min(NS, ci * 512 + 512)
                    pm = ci % 4
                    for kc in range(KC):
                        P.op("pe", lambda e, kc=kc, c0=c0, c1=c1, pm=pm: e.matmul(
                            out=pB[pm][:NSTOK, 0:c1 - c0], lhsT=xnT[sl][:, kc, :NSTOK], rhs=wsb[:, kc, c0:c1],
                            start=(kc == 0), stop=(kc == KC - 1)), r=["xnT" + t, "wsb"], w=[f"pB{pm}"])
                    P.op("act", lambda e, c0=c0, c1=c1, pm=pm: e.copy(out=souts[:, c0:c1], in_=pB[pm][:NSTOK, 0:c1 - c0]),
                         r=[f"pB{pm}"], w=["souts"])
                if GDN:
                    gdn_zg(xnT[sl], "xnT" + t, NSTOK, L)
                    for b in range(NSEQ):
                        def halo(b=b):
                            for r_ in range(3):
                                P.dma("sp", pre[:, :, r_], sconv[b, r_].rearrange("(c p) -> p c", p=128), w=["pre_h"],
                                      allow_slow_non_contiguous=True)
                        xs_b = xnT[sl][:, :, b * TS:(b + 1) * TS]
                        gdn_pre(xs_b, "xnT" + t, TS, L + b * TS, halo)
                if "nsas" in phases:
                    xT = xnT[sl]
                    for hh in range(8):
                        for kc in range(KC):
                            P.op("pe", lambda e, kc=kc, hh=hh: e.matmul(
                                out=pA[0][0:64, hh * NSTOK:(hh + 1) * NSTOK], lhsT=wnb[:, kc, hh * 64:(hh + 1) * 64], rhs=xT[:, kc, :NSTOK],
                                start=(kc == 0), stop=(kc == KC - 1)), r=["xnT" + t, "wnb"], w=["pA0"])
                    P.op("act", lambda e: e.activation(out=qTb[:, :, :NSTOK], in_=pA[0][0:64, 0:8 * NSTOK].rearrange("p (a t) -> p a t", a=8),
                                                       func=AF.Copy, scale=0.125), r=["pA0"], w=["qTb"])
                    for g_ in range(2):
                        P.dma("act", qTs_scr[g_], qTb[:, 4 * g_:4 * g_ + 4, :NSTOK], r=["qTb"], w=[f"qTss{g_}"])
                    for kc in range(KC):
                        P.op("pe", lambda e, kc=kc: e.matmul(out=pB[3][:NSTOK, 0:24], lhsT=xT[:, kc, :NSTOK], rhs=wnb[:, kc, 512:536],
                                                             start=(kc == 0), stop=(kc == KC - 1)), r=["xnT" + t, "wnb"], w=["pB3"])
                    P.op("act", lambda e: e.activation(out=gts[:NSTOK, :], in_=pB[3][:NSTOK, 0:24], func=AF.Sigmoid), r=["pB3"], w=["gts"])
                    P.dma("act", gates_s_scr, gts[:NSTOK, :], r=["gts"], w=["gatess"])
                    P.dma("act", kvs_scr, souts[:, S_KV:S_KV + 768], r=["souts"], w=["kvs_scr"])
                P.dma("act", kvs, souts[:, S_KV:S_KV + 512], r=["souts"])
                for b in range(NSEQ):
                    P.dma("sp", wins[b, 0:WINB - TS, :], cwin[b, TS:WINB, :])
                    P.dma("act", wins[b, WINB - TS:WINB, :], souts[b * TS:(b + 1) * TS, S_KV + 512:S_KV + 768],
                          r=["souts"])
                    P.dma("act", convs[b], souts[b * TS + TS - 3:(b + 1) * TS, S_QKV:S_QKV + 1536], r=["souts"])
                P.run()

        if "proj" in phases:
            phase_proj()


        def phase_gdn():
            with ExitStack() as s2:
                cm = sbuf(s2, "cm", [128, 3, 128], F32)
                gv = sbuf(s2, "gv2", [128, 136], F32)
                P.dma("sp", cm[:], cmask_d, w=["cm"])
                P.dma("sp", gv[:], gvec[0].partition_broadcast(128), w=["gv2"])
                TRI, NSTR, ONES = cm[:, 0, :], cm[:, 1, :], cm[:, 2, :]
                identb = sbuf(s2, "identb", [128, 128], BF16)
                P.op("act", lambda e: e.copy(out=identb[:], in_=ident[:]), r=["ident"], w=["identb"])
                qs = [sbuf(s2, f"qs2_{j}", [128, 12, 128], F32) for j in range(2)]
                qsb = [sbuf(s2, f"qsb_{j}", [128, 12, 128], BF16) for j in range(2)]
                zs = [sbuf(s2, f"zs2_{j}", [128, 512], F32) for j in range(2)]
                bg = [sbuf(s2, f"bg2_{j}", [128, 8], F32) for j in range(2)]
                gz = sbuf(s2, "gz", [128, 512], F32)
                sc = sbuf(s2, "sc2", [128, 64], F32)
                ya = sbuf(s2, "ya2", [128, 512], F32)
                yaT = sbuf(s2, "yaT2", [128, 4, 128], BF16)
                S = [sbuf(s2, f"S{h}", [128, 128], F32) for h in range(4)]
                Sb = [sbuf(s2, f"Sb{h}", [128, 128], BF16) for h in range(4)]
                W = {}
                for h in range(4):
                    for nm, dtp in (("kbg", BF16), ("kt", BF16), ("vb", BF16), ("E", F32), ("X1", F32), ("X2", F32),
                                    ("BT", F32), ("BTb", BF16), ("Bb", BF16), ("BTb2", BF16), ("Bb2", BF16),
                                    ("PT", F32), ("PTb", BF16), ("qkT", BF16), ("qdT", BF16), ("wcT", BF16),
                                    ("uc", F32), ("ub", BF16), ("tmp", F32)):
                        W[(nm, h)] = sbuf(s2, f"{nm}{h}", [128, 128], dtp)
                banks = [pA[0][:, 0:512], pA[0][:, 512:1024], pA[1][:, 0:512], pA[1][:, 512:1024],
                         pB[0][:], pB[1][:], pB[2][:], pB[3][:]]

                def slot(h, j):
                    return banks[2 * h + j // 4][:, (j % 4) * 128:(j % 4 + 1) * 128], f"gbank{2 * h + j // 4}"

                def gdn_chunk(C, tok0, first, li):
                    sl = li % 2
                    kq, kz, kb = f"qs2_{sl}", f"zs2_{sl}", f"bg2_{sl}"
                    P.dma("sp", qs[sl][:, :, :C], qs_scr[:, :, tok0:tok0 + C], r=[f"qs_scr{tok0}"], w=[kq])
                    P.dma("act", zs[sl][:C, :], zs_scr[tok0:tok0 + C, :], r=[f"zs_scr{tok0}"], w=[kz])
                    P.dma("act", bg[sl][:C, :], bg_scr[tok0:tok0 + C, :], r=[f"bg_scr{tok0}"], w=[kb])
                    P.op("act", lambda e: e.copy(out=qsb[sl][:, :, :C], in_=qs[sl][:, :, :C]), r=[kq], w=[f"qsb_{sl}"])
                    kqb = f"qsb_{sl}"
                    P.op("pool", lambda e: e.tensor_tensor(
                        out=gz[:C, :].rearrange("p (h d) -> p h d", h=4), in0=zs[sl][:C, :].rearrange("p (h d) -> p h d", h=4),
                        in1=gv[:C, 8:136].unsqueeze(1).to_broadcast([C, 4, 128]), op=ALU.mult), r=[kz, "gv2"], w=["gz"])
                    c0, k0 = slot(3, 7)
                    P.op("pe", lambda e: e.matmul(out=c0[:C, 0:4], lhsT=TRI[:C, :C], rhs=bg[sl][:C, 4:8], start=True, stop=True),
                         r=["cm", kb], w=[k0])
                    P.op("pe", lambda e: e.matmul(out=c0[:, 4:8], lhsT=ONES[:C, :], rhs=bg[sl][:C, 4:8], start=True, stop=True),
                         r=["cm", kb], w=[k0])
                    P.op("act", lambda e: e.copy(out=sc[:C, 0:4], in_=c0[:C, 0:4]), r=[k0], w=["sc"])
                    P.op("act", lambda e: e.copy(out=sc[:, 4:8], in_=c0[:, 4:8]), r=[k0], w=["sc"])
                    P.op("act", lambda e: e.activation(out=sc[:C, 8:12], in_=sc[:C, 0:4], func=AF.Exp), r=["sc"], w=["sc"])
                    P.op("dve", lambda e: e.tensor_tensor(out=sc[:C, 60:64], in0=sc[:C, 4:8], in1=sc[:C, 0:4], op=ALU.subtract),
                         r=["sc"], w=["sc"])
                    P.op("act", lambda e: e.activation(out=sc[:C, 12:16], in_=sc[:C, 60:64], func=AF.Exp), r=["sc"], w=["sc"])
                    P.op("act", lambda e: e.activation(out=sc[:, 16:20], in_=sc[:, 4:8], func=AF.Exp), r=["sc"], w=["sc"])
                    for h in range(4):
                        for (j, ch) in ((0, 4 + h), (1, 8 + h), (2, h)):
                            o_, ko = slot(h, j)
                            P.op("pe", lambda e, o_=o_, ch=ch: e.transpose(out=o_[:C, :], in_=qs[sl][:, ch, :C], identity=ident[:, :]),
                                 r=[kq, "ident"], w=[ko])
                        kt_, kkt = slot(h, 0)
                        qt_, kqt = slot(h, 2)
                        P.op("act", lambda e, h=h, qt_=qt_: e.activation(out=W[("tmp", h)][:C, :], in_=qt_[:C, :], func=AF.Square,
                                                                         accum_out=sc[:C, 20 + h:21 + h]), r=[kqt], w=["sc", f"tmp{h}"])
                        P.op("act", lambda e, h=h, kt_=kt_: e.activation(out=W[("tmp", h)][:C, :], in_=kt_[:C, :], func=AF.Square,
                                                                         accum_out=sc[:C, 24 + h:25 + h]), r=[kkt], w=["sc", f"tmp{h}"])
                    P.op("act", lambda e: e.activation(out=sc[:C, 28:36], in_=sc[:C, 20:28], func=AF.Sqrt, bias=epsc[:C, :], scale=1.0),
                         r=["sc", "epsc"], w=["sc"])
                    P.op("dve", lambda e: e.reciprocal(out=sc[:C, 28:36], in_=sc[:C, 28:36]), r=["sc"], w=["sc"])
                    P.op("dve", lambda e: e.tensor_scalar(out=sc[:C, 28:32], in0=sc[:C, 28:32], scalar1=float(128 ** -0.5), scalar2=None,
                                                          op0=ALU.mult), r=["sc"], w=["sc"])
                    P.op("dve", lambda e: e.tensor_tensor(out=sc[:C, 44:48], in0=sc[:C, 32:36], in1=bg[sl][:C, 0:4], op=ALU.mult),
                         r=["sc", kb], w=["sc"])
                    P.op("dve", lambda e: e.tensor_tensor(out=sc[:C, 36:40], in0=sc[:C, 44:48], in1=sc[:C, 8:12], op=ALU.mult),
                         r=["sc"], w=["sc"])
                    P.op("dve", lambda e: e.tensor_tensor(out=sc[:C, 40:44], in0=sc[:C, 32:36], in1=sc[:C, 12:16], op=ALU.mult),
                         r=["sc"], w=["sc"])
                    P.op("dve", lambda e: e.tensor_tensor(out=sc[:C, 48:52], in0=sc[:C, 28:32], in1=sc[:C, 8:12], op=ALU.mult),
                         r=["sc"], w=["sc"])
                    gens = [head(C, h, sl, kq, kqb, kb, first) for h in range(4)]
                    while gens:
                        for gen in list(gens):
                            try:
                                next(gen)
                            except StopIteration:
                                gens.remove(gen)
                    for h in range(4):
                        o_, ko = slot(h, 0)
                        P.op("pe", lambda e, o_=o_, h=h: e.transpose(out=o_[:, :C], in_=ya[:C, h * 128:(h + 1) * 128],
                                                                     identity=ident[:C, :C]), r=[f"ya{h}", "ident"], w=[ko])
                        P.op("act", lambda e, o_=o_, h=h: e.copy(out=yaT[:, h, :C], in_=o_[:, :C]), r=[ko], w=["yaT"])
                    P.dma("sp", yaT_scr[:, :, tok0:tok0 + C], yaT[:, :, :C], r=["yaT"], w=[f"yaT_scr{tok0}"])

                def head(C, h, sl, kq, kqb, kb, first):
                    w = lambda nm: (W[(nm, h)], f"{nm}{h}")
                    kbg, kkbg = w("kbg"); ktl, kktl = w("kt"); vb, kvb = w("vb"); E, kE = w("E"); X1, kX1 = w("X1"); X2, kX2 = w("X2")
                    BT, kBT = w("BT"); BTb, kBTb = w("BTb"); Bb, kBb = w("Bb"); BTb2, kBTb2 = w("BTb2"); Bb2, kBb2 = w("Bb2")
                    PT, kPT = w("PT"); PTb, kPTb = w("PTb"); qkT, kqkT = w("qkT"); qdT, kqdT = w("qdT"); wcT, kwcT = w("wcT")
                    uc, kuc = w("uc"); ub, kub = w("ub"); tmp, ktmp = w("tmp")
                    k_tm, kk_tm = slot(h, 0)
                    v_tm, kv_tm = slot(h, 1)
                    P.op("dve", lambda e: e.tensor_scalar(out=kbg[:C, :], in0=k_tm[:C, :], scalar1=sc[:C, 36 + h:37 + h], scalar2=None,
                                                          op0=ALU.mult), r=[kk_tm, "sc"], w=[kkbg])
                    yield
                    P.op("dve", lambda e: e.tensor_scalar(out=ktl[:C, :], in0=k_tm[:C, :], scalar1=sc[:C, 40 + h:41 + h], scalar2=None,
                                                          op0=ALU.mult), r=[kk_tm, "sc"], w=[kktl])
                    yield
                    P.op("dve", lambda e: e.tensor_scalar(out=vb[:C, :], in0=v_tm[:C, :], scalar1=bg[sl][:C, h:h + 1], scalar2=None,
                                                          op0=ALU.mult), r=[kv_tm, kb], w=[kvb])
                    yield
                    rows = []
                    for j, (col, rhs_) in enumerate(((bg[sl][:C, 4 + h:5 + h], TRI), (sc[:C, 44 + h:45 + h], ident),
                                                    (sc[:C, 28 + h:29 + h], ident), (sc[:C, 48 + h:49 + h], ident))):
                        o_, ko = slot(h, 4 + j)
                        P.op("dve", lambda e, col=col: e.tensor_scalar(out=tmp[:C, :], in0=ONES[:C, :], scalar1=col, scalar2=None,
                                                                       op0=ALU.mult), r=["cm", "sc", kb], w=[ktmp])
                        yield
                        P.op("pe", lambda e, o_=o_, rhs_=rhs_: e.matmul(out=o_[:, :C], lhsT=tmp[:C, :], rhs=rhs_[:C, :C],
                                                                       start=True, stop=True), r=[ktmp, "cm", "ident"], w=[ko])
                        yield
                        rows.append((o_, ko))
                    (GROW, kG), (BKROW, kBK), (QROW, kQ), (QDROW, kQD) = rows
                    P.op("dve", lambda e: e.tensor_scalar(out=E[:C, :C], in0=GROW[:C, :C], scalar1=sc[:C, h:h + 1], scalar2=0.0,
                                                          op0=ALU.subtract, op1=ALU.min), r=[kG, "sc"], w=[kE])
                    yield
                    P.op("act", lambda e: e.activation(out=E[:C, :C], in_=E[:C, :C], func=AF.Exp), r=[kE], w=[kE])
                    yield
                    P.op("dve", lambda e: e.tensor_tensor(out=X1[:C, :C], in0=BKROW[:C, :C], in1=NSTR[:C, :C], op=ALU.mult),
                         r=[kBK, "cm"], w=[kX1])
                    yield
                    P.op("dve", lambda e: e.tensor_tensor(out=X2[:C, :C], in0=QROW[:C, :C], in1=TRI[:C, :C], op=ALU.mult),
                         r=[kQ, "cm"], w=[kX2])
                    yield
                    P.op("dve", lambda e: e.tensor_tensor(out=qdT[:, :C], in0=qs[sl][:, h, :C], in1=QDROW[:, :C], op=ALU.mult),
                         r=[kq, kQD], w=[kqdT])
                    yield
                    KK, kKK = slot(h, 2)
                    KQ, kKQ = slot(h, 3)
                    P.op("pe", lambda e: e.matmul(out=KK[:C, :C], lhsT=qsb[sl][:, 4 + h, :C], rhs=qsb[sl][:, 4 + h, :C], start=True, stop=True),
                         r=[kqb], w=[kKK])
                    yield
                    P.op("pe", lambda e: e.matmul(out=KQ[:C, :C], lhsT=qsb[sl][:, 4 + h, :C], rhs=qsb[sl][:, h, :C], start=True, stop=True),
                         r=[kqb], w=[kKQ])
                    yield
                    P.op("dve", lambda e: e.scalar_tensor_tensor(out=BT[:C, :C], in0=KK[:C, :C], scalar=sc[:C, 32 + h:33 + h], in1=E[:C, :C],
                                                                 op0=ALU.mult, op1=ALU.mult), r=[kKK, "sc", kE], w=[kBT])
                    yield
                    P.op("dve", lambda e: e.tensor_tensor(out=BT[:C, :C], in0=BT[:C, :C], in1=X1[:C, :C], op=ALU.mult), r=[kBT, kX1], w=[kBT])
                    yield
                    P.op("dve", lambda e: e.scalar_tensor_tensor(out=tmp[:C, :C], in0=KQ[:C, :C], scalar=sc[:C, 32 + h:33 + h], in1=E[:C, :C],
                                                                 op0=ALU.mult, op1=ALU.mult), r=[kKQ, "sc", kE], w=[ktmp])
                    yield
                    P.op("dve", lambda e: e.tensor_tensor(out=qkT[:C, :C], in0=tmp[:C, :C], in1=X2[:C, :C], op=ALU.mult), r=[ktmp, kX2], w=[kqkT])
                    yield
                    P.op("act", lambda e: e.copy(out=BTb[:C, :C], in_=BT[:C, :C]), r=[kBT], w=[kBTb])
                    yield
                    P.op("dve", lambda e: e.tensor_tensor(out=PT[:C, :C], in0=BT[:C, :C], in1=ident[:C, :C], op=ALU.add), r=[kBT, "ident"], w=[kPT])
                    yield
                    P.op("act", lambda e: e.copy(out=PTb[:C, :C], in_=PT[:C, :C]), r=[kPT], w=[kPTb])
                    yield
                    t0_, kt0 = slot(h, 4)
                    P.op("pe", lambda e: e.transpose(out=t0_[:C, :C], in_=BT[:C, :C], identity=ident[:C, :C]), r=[kBT, "ident"], w=[kt0])
                    yield
                    P.op("act", lambda e: e.copy(out=Bb[:C, :C], in_=t0_[:C, :C]), r=[kt0], w=[kBb])
                    yield
                    nlev = max(1, int(np.ceil(np.log2(C))))
                    cur = (Bb, kBb, BTb, kBTb)
                    nxt = (Bb2, kBb2, BTb2, kBTb2)
                    for lv in range(1, nlev):
                        (cB, kcB, cBT, kcBT) = cur
                        (nB, knB, nBT, knBT) = nxt
                        x_, kx = slot(h, 5)
                        xt_, kxt = slot(h, 6)
                        p_, kp = slot(h, 7)
                        P.op("pe", lambda e, cB=cB, cBT=cBT, x_=x_: e.matmul(out=x_[:C, :C], lhsT=cBT[:C, :C], rhs=cB[:C, :C], start=True, stop=True),
                             r=[kcB, kcBT], w=[kx])
                        yield
                        P.op("act", lambda e, nB=nB, x_=x_: e.copy(out=nB[:C, :C], in_=x_[:C, :C]), r=[kx], w=[knB])
                        yield
                        if lv < nlev - 1:
                            P.op("pe", lambda e, cB=cB, cBT=cBT, xt_=xt_: e.matmul(out=xt_[:C, :C], lhsT=cB[:C, :C], rhs=cBT[:C, :C],
                                                                                 start=True, stop=True), r=[kcB, kcBT], w=[kxt])
                            yield
                            P.op("act", lambda e, nBT=nBT, xt_=xt_: e.copy(out=nBT[:C, :C], in_=xt_[:C, :C]), r=[kxt], w=[knBT])
                            yield
                        P.op("pe", lambda e, nB=nB, p_=p_: e.matmul(out=p_[:C, :C], lhsT=nB[:C, :C], rhs=PTb[:C, :C], start=True, stop=True),
                             r=[knB, kPTb], w=[kp])
                        yield
                        P.op("dve", lambda e, p_=p_: e.tensor_tensor(out=PT[:C, :C], in0=PT[:C, :C], in1=p_[:C, :C], op=ALU.add), r=[kPT, kp], w=[kPT])
                        yield
                        P.op("act", lambda e: e.copy(out=PTb[:C, :C], in_=PT[:C, :C]), r=[kPT], w=[kPTb])
                        yield
                        cur, nxt = nxt, cur
                    u_, ku = slot(h, 0)
                    wc_, kwc = slot(h, 3)
                    P.op("pe", lambda e: e.matmul(out=u_[:C, :], lhsT=PTb[:C, :C], rhs=vb[:C, :], start=True, stop=True), r=[kPTb, kvb], w=[ku])
                    yield
                    P.op("pe", lambda e: e.matmul(out=wc_[:, :C], lhsT=kbg[:C, :], rhs=PTb[:C, :C], start=True, stop=True), r=[kPTb, kkbg], w=[kwc])
                    yield
                    P.op("act", lambda e: e.copy(out=uc[:C, :], in_=u_[:C, :]), r=[ku], w=[kuc])
                    yield
                    P.op("act", lambda e: e.copy(out=wcT[:, :C], in_=wc_[:, :C]), r=[kwc], w=[kwcT])
                    yield
                    ws_, kws = slot(h, 1)
                    o_, ko = slot(h, 2)
                    kS, kSb = f"S{h}", f"Sb{h}"
                    if first:
                        P.op("act", lambda e: e.copy(out=ub[:C, :], in_=uc[:C, :]), r=[kuc], w=[kub])
                        yield
                        P.op("pe", lambda e: e.matmul(out=o_[:C, :], lhsT=qkT[:C, :C], rhs=ub[:C, :], start=True, stop=True), r=[kqkT, kub], w=[ko])
                        yield
                    else:
                        P.op("pe", lambda e: e.matmul(out=ws_[:C, :], lhsT=wcT[:, :C], rhs=Sb[h][:, :], start=True, stop=True), r=[kwcT, kSb], w=[kws])
                        yield
                        P.op("dve", lambda e: e.tensor_tensor(out=ub[:C, :], in0=uc[:C, :], in1=ws_[:C, :], op=ALU.subtract), r=[kuc, kws], w=[kub])
                        yield
                        P.op("pe", lambda e: e.matmul(out=o_[:C, :], lhsT=qdT[:, :C], rhs=Sb[h][:, :], start=True, stop=False), r=[kqdT, kSb], w=[ko])
                        yield
                        P.op("pe", lambda e: e.matmul(out=o_[:C, :], lhsT=qkT[:C, :C], rhs=ub[:C, :], start=False, stop=True), r=[kqkT, kub], w=[ko])
                        yield
                    su_, ksu = slot(h, 5)
                    P.op("pe", lambda e: e.matmul(out=su_[:, :], lhsT=ktl[:C, :], rhs=ub[:C, :], start=True, stop=True), r=[kktl, kub], w=[ksu])
                    yield
                    if first:
                        P.op("act", lambda e: e.copy(out=S[h][:, :], in_=su_[:, :]), r=[ksu], w=[kS])
                        yield
                    else:
                        P.op("dve", lambda e: e.scalar_tensor_tensor(out=S[h][:, :], in0=S[h][:, :], scalar=sc[:, 16 + h:17 + h], in1=su_[:, :],
                                                                     op0=ALU.mult, op1=ALU.add), r=[kS, "sc", ksu], w=[kS])
                        yield
                    P.op("act", lambda e: e.copy(out=Sb[h][:, :], in_=S[h][:, :]), r=[kS], w=[kSb])
                    yield
                    P.op("act", lambda e: e.activation(out=tmp[:C, :], in_=o_[:C, :], func=AF.Square, accum_out=sc[:C, 52 + h:53 + h]),
                         r=[ko], w=[ktmp, f"sco{h}"])
                    yield
                    P.op("act", lambda e: e.activation(out=sc[:C, 56 + h:57 + h], in_=sc[:C, 52 + h:53 + h], func=AF.Sqrt, bias=epsc[:C, :],
                                                       scale=1.0 / 128), r=[f"sco{h}", "epsc"], w=[f"sco{h}"])
                    yield
                    P.op("dve", lambda e: e.reciprocal(out=sc[:C, 56 + h:57 + h], in_=sc[:C, 56 + h:57 + h]), r=[f"sco{h}"], w=[f"sco{h}"])
                    yield
                    P.op("dve", lambda e: e.scalar_tensor_tensor(out=ya[:C, h * 128:(h + 1) * 128], in0=o_[:C, :], scalar=sc[:C, 56 + h:57 + h],
                                                                 in1=gz[:C, h * 128:(h + 1) * 128], op0=ALU.mult, op1=ALU.mult),
                         r=[ko, f"sco{h}", "gz"], w=[f"ya{h}"])
                    yield

                li = 0
                for i in range(NT):
                    gdn_chunk(128, i * 128, i == 0, li)
                    li += 1
                for h in range(4):
                    P.dma("sp", gdnp[h], S[h][:, :], r=[f"S{h}"])
                for b in range(NSEQ):
                    for h in range(4):
                        P.dma("sp", S[h][:, :], sgdn[b * 4 + h], w=[f"S{h}"])
                        P.op("act", lambda e, h=h: e.copy(out=Sb[h][:, :], in_=S[h][:, :]), r=[f"S{h}"], w=[f"Sb{h}"])
                    gdn_chunk(TS, L + b * TS, False, li)
                    li += 1
                    for h in range(4):
                        P.dma("sp", gdns[b * 4 + h], S[h][:, :], r=[f"S{h}"])
                P.run()


        def phase_nsa():
            BIG = 1.0e30
            with ExitStack() as s3:
                ovb = sbuf(s3, "ovb", [128, NCT, NSB], BF16)
                cmk = sbuf(s3, "cmk", [128, 17, 128], BF16)
                tab = sbuf(s3, "tabs", [128, 2, 2 * NSB], F32)
                tri2 = sbuf(s3, "tri2s", [128, 2, 128], BF16)
                expt = sbuf(s3, "expt_sb3", [128, NT, 128], BF16)
                pwm = sbuf(s3, "pwms", [128, 4, 8], BF16)
                wcb = sbuf(s3, "wcbs", [64, 2, 64], BF16)
                P.dma("pool", ovb[:], ov_d, w=["ovb"])
                P.dma("pool", cmk[:], cmsk_d, w=["cmk"])
                P.dma("sp", tab[:], tab_d, w=["tab"])
                P.dma("pool", tri2[:], tri2_d, w=["tri2"])
                P.dma("pool", expt[:], expt_d, w=["expt"])
                P.dma("pool", pwm[:], pw_d, w=["pwm"])
                P.dma("pool", wcb[:], wc_d, w=["wcb"])
                kcT = [sbuf(s3, f"kcT{g}", [68, NCT * 128], BF16) for g in range(2)]
                vc = [sbuf(s3, f"vc{g}", [128, NCT, 65], BF16) for g in range(2)]
                SC = [(pB[0], "pB0"), (pB[1], "pB1")]
                SC4 = [(pB[0], "pB0"), (pB[1], "pB1"), (pA[1][:, 0:512], "pA1lo"), (pA[1][:, 512:1024], "pA1hi")]
                OC, kOC = pB[2], "pB2"
                OS, kOS = pB[3], "pB3"
                OW, kOW = pA[0][:, 0:512], "pA0lo"
                IMP, kIMP = pA[0][:, 512:1024], "pA0hi"
                MX = [(pA[1][:, 0:512], "pA1lo"), (pA[1][:, 512:1024], "pA1hi")]
                TR, kTR = IMP, kIMP

                with ExitStack() as s3a:
                    cmpin = sbuf(s3a, "cmpin", [128, NT, 256], BF16)
                    pooledb = sbuf(s3a, "pooledb", [64, 4, NCT * 128], BF16)
                    P.dma("sp", cmpin[:], kvtm_scr[:, 0:256].rearrange("(kt p) c -> p kt c", p=128), w=["cmpin"])
                    P.op("dve", lambda e: e.memset(pooledb[:], 0.0), w=["pooledb"])
                    for which in range(2):
                        for g in range(2):
                            pb, kpb = pB[which * 2 + g], f"pB{which * 2 + g}"
                            c0 = which * 128 + g * 64
                            for kt in range(NT):
                                last = (kt == NT - 1)
                                P.op("pe", lambda e, kt=kt, pb=pb, c0=c0, which=which, last=last: e.matmul(
                                    out=pb[0:64, 8 * kt:8 * kt + 8], lhsT=cmpin[:, kt, c0:c0 + 64], rhs=pwm[:, 2 * which, :],
                                    start=True, stop=last), r=["cmpin", "pwm"], w=[kpb])
                                if not last:
                                    P.op("pe", lambda e, kt=kt, pb=pb, c0=c0, which=which: e.matmul(
                                        out=pb[0:64, 8 * kt:8 * kt + 8], lhsT=cmpin[0:16, kt + 1, c0:c0 + 64],
                                        rhs=pwm[0:16, 2 * which + 1, :], start=False, stop=True), r=["cmpin", "pwm"], w=[kpb])
                            P.op("act", lambda e, pb=pb, which=which, g=g: e.copy(out=pooledb[:, which * 2 + g, 0:8 * NT],
                                                                                 in_=pb[0:64, 0:8 * NT]), r=[kpb], w=["pooledb"])
                    for g in range(2):
                        P.op("pe", lambda e, g=g: e.matmul(out=pA[0][0:64, 0:NCT * 128], lhsT=wcb[:, 0, :], rhs=pooledb[:, g, :],
                                                           start=True, stop=True), r=["wcb", "pooledb"], w=["pA0lo"])
                        P.op("act", lambda e, g=g: e.copy(out=kcT[g][0:64, :], in_=pA[0][0:64, 0:NCT * 128]), r=["pA0lo"], w=[f"kcT{g}"])
                        P.dma("pool", kcT[g][64:68, :], caug_d, w=[f"kcT{g}"])
                        for ct in range(NCT):
                            P.op("pe", lambda e, g=g, ct=ct: e.matmul(out=pA[1][:, ct * 64:(ct + 1) * 64],
                                                                      lhsT=pooledb[:, 2 + g, ct * 128:(ct + 1) * 128], rhs=wcb[:, 1, :],
                                                                      start=True, stop=True), r=["wcb", "pooledb"], w=["pA1lo"])
                        P.op("dve", lambda e, g=g: e.memset(vc[g][:, :, 64:65], 1.0), w=[f"vc{g}"])
                        P.op("act", lambda e, g=g: e.copy(out=vc[g][:, :, 0:64],
                                                          in_=pA[1][:, 0:NCT * 64].rearrange("p (c d) -> p c d", d=64)),
                             r=["pA1lo"], w=[f"vc{g}"])
                    P.run()

                kS = sbuf(s3, "kS", [68, L], BF16)
                kW = sbuf(s3, "kW", [68, L], BF16)
                vS = sbuf(s3, "vS", [128, NT, 65], BF16)
                vW = sbuf(s3, "vW", [128, NT, 65], BF16)
                R3 = 3
                neg60 = sbuf(s3, "neg60", [128, 1], F32)
                P.op("dve", lambda e: e.memset(neg60[:], -60.0), w=["neg60"])
                qa = [sbuf(s3, f"qa{j}", [68, 4, 128], BF16) for j in range(R3)]
                gt = [sbuf(s3, f"gt{j}", [128, 24], F32) for j in range(R3)]
                ecmp = [sbuf(s3, f"ecmp{j}", [128, NCT, 512], BF16) for j in range(R3)]
                impsb = [sbuf(s3, f"impsb{j}", [128, 512], F32) for j in range(R3)]
                selT = [sbuf(s3, f"selT{j}", [128, 4, 128], BF16) for j in range(R3)]
                otok = [[sbuf(s3, f"otok{j}_{b_}", [128, 4, 65], F32) for b_ in range(3)] for j in range(R3)]
                rs = [sbuf(s3, f"rs3_{j}", [128, 3, 4], F32) for j in range(R3)]
                coef = sbuf(s3, "coef3", [128, 3, 4], F32)
                es = [sbuf(s3, f"es{j}", [128, 512], BF16) for j in range(4)]
                oTa = sbuf(s3, "oTa", [65, 512], F32)
                oTs = sbuf(s3, "oTs", [65, 512], F32)
                oTw = sbuf(s3, "oTw", [65, 512], F32)
                ctmp = sbuf(s3, "ctmp", [128, 512], F32)
                sco = sbuf(s3, "sco", [128, NSB], F32)
                sco2 = sbuf(s3, "sco2", [128, NSB], F32)
                m8 = sbuf(s3, "m8", [128, 16], F32)
                selm = sbuf(s3, "selm", [128, NSB], F32)
                yb = sbuf(s3, "yb3", [128, 256], F32)
                ybT = sbuf(s3, "ybT3", [128, 2, 128], BF16)

                def fin_tok(src, ksrc, par, br):
                    for h in range(4):
                        P.op("pe", lambda e, h=h: e.transpose(out=TR[:, h * 65:(h + 1) * 65], in_=src[0:65, h * 128:(h + 1) * 128],
                                                              identity=ident[0:65, 0:65]), r=[ksrc, "ident"], w=[kTR])
                    P.op("dve", lambda e: e.tensor_copy(out=otok[par][br][:], in_=TR[:, 0:260].rearrange("p (h d) -> p h d", h=4)),
                         r=[kTR], w=[f"otok{par}_{br}"])
                    P.op("dve", lambda e: e.tensor_scalar(out=rs[par][:, br, :], in0=otok[par][br][:, :, 64], scalar1=1.0e-30, scalar2=None,
                                                          op0=ALU.max), r=[f"otok{par}_{br}"], w=[f"rs3_{par}"])
                    P.op("dve", lambda e: e.reciprocal(out=rs[par][:, br, :], in_=rs[par][:, br, :]), r=[f"rs3_{par}"], w=[f"rs3_{par}"])

                def genA(g, i, par):
                    t0 = i * 128
                    kqa, kgt, kec, kim = f"qa{par}", f"gt{par}", f"ecmp{par}", f"impsb{par}"
                    P.dma("sp", qa[par][0:64, :, :], qT_scr[g, :, :, t0:t0 + 128], w=[kqa])
                    P.dma("pool", qa[par][64:68, :, :], qaug_d[g, :, :, t0:t0 + 128], w=[kqa])
                    P.dma("sp", gt[par][:], gates_scr[t0:t0 + 128, :], w=[kgt])
                    yield
                    rhsq = qa[par][:].rearrange("p h t -> p (h t)")
                    nct_i = min(NCT, (8 * i + 6) // 128 + 1)
                    for ct in range(nct_i):
                        scp, kscp = SC[ct % 2]
                        m_ = i - 16 * ct
                        P.op("pe", lambda e, ct=ct, scp=scp: e.matmul(out=scp[:, :], lhsT=kcT[g][:, ct * 128:(ct + 1) * 128], rhs=rhsq,
                                                                      start=True, stop=True), r=[f"kcT{g}", kqa], w=[kscp])
                        if m_ <= 16:
                            P.op("dve", lambda e, scp=scp: e.tensor_scalar(out=ctmp[:, :], in0=scp[:, :], scalar1=60.0, scalar2=None, op0=ALU.min),
                                 r=[kscp], w=["ctmp"])
                            P.op("act", lambda e, ct=ct: e.activation(out=ecmp[par][:, ct, :], in_=ctmp[:, :], func=AF.Exp),
                                 r=["ctmp"], w=[kec])
                            P.op("dve", lambda e, ct=ct, m_=m_: e.tensor_tensor(
                                out=ecmp[par][:, ct, :].rearrange("p (h t) -> p h t", h=4),
                                in0=ecmp[par][:, ct, :].rearrange("p (h t) -> p h t", h=4),
                                in1=cmk[:, m_, :].unsqueeze(1).to_broadcast([128, 4, 128]), op=ALU.mult), r=[kec, "cmk"], w=[kec])
                        else:
                            P.op("act", lambda e, ct=ct, scp=scp: e.activation(out=ecmp[par][:, ct, :], in_=scp[:, :], func=AF.Exp),
                                 r=[kscp], w=[kec])
                        yield
                    for ct in range(nct_i):
                        P.op("pe", lambda e, ct=ct: e.matmul(out=OC[0:65, :], lhsT=vc[g][:, ct, :], rhs=ecmp[par][:, ct, :],
                                                             start=(ct == 0), stop=(ct == nct_i - 1)), r=[f"vc{g}", kec], w=[kOC])
                    yield
                    for h in range(4):
                        for ct in range(nct_i):
                            P.op("pe", lambda e, ct=ct, h=h: e.matmul(out=IMP[:, h * 128:h * 128 + NSB], lhsT=ecmp[par][:, ct, h * 128:(h + 1) * 128],
                                                                      rhs=ovb[:, ct, :], start=(ct == 0), stop=(ct == nct_i - 1)),
                                 r=[kec, "ovb"], w=[kIMP])
                    P.op("act", lambda e: e.copy(out=impsb[par][:, :], in_=IMP[:, :]), r=[kIMP], w=[kim])
                    yield
                    P.op("act", lambda e: e.copy(out=oTa[:, :], in_=OC[0:65, :]), r=[kOC], w=["oTa"])
                    fin_tok(oTa, "oTa", par, 0)
                    yield
                    krs = f"rs3_{par}"
                    P.op("dve", lambda e: e.tensor_scalar(out=sco[:, :], in0=impsb[par][:, 0:NSB], scalar1=rs[par][:, 0, 0:1], scalar2=None, op0=ALU.mult),
                         r=[kim, krs], w=["sco"])
                    yield
                    for h in range(1, 4):
                        P.op("dve", lambda e, h=h: e.scalar_tensor_tensor(out=sco[:, :], in0=impsb[par][:, h * 128:h * 128 + NSB],
                                                                         scalar=rs[par][:, 0, h:h + 1], in1=sco[:, :], op0=ALU.mult, op1=ALU.add),
                             r=[kim, krs, "sco"], w=["sco"])
                        yield
                    a0 = NSB - 2 * i
                    P.op("dve", lambda e: e.tensor_tensor(out=sco[:, :], in0=sco[:, :], in1=tab[:, 0, a0:a0 + NSB], op=ALU.mult),
                         r=["sco", "tab"], w=["sco"])
                    yield
                    P.op("dve", lambda e: e.tensor_tensor(out=sco[:, :], in0=sco[:, :], in1=tab[:, 1, a0:a0 + NSB], op=ALU.add),
                         r=["sco", "tab"], w=["sco"])
                    yield
                    P.op("dve", lambda e: e.memset(sco[:, 0:1], 1.0e4), r=["sco"], w=["sco"])
                    yield
                    P.op("dve", lambda e: e.max(out=m8[:, 0:8], in_=sco[:, :]), r=["sco"], w=["m8"])
                    yield
                    P.op("dve", lambda e: e.match_replace(out=sco2[:, :], in_to_replace=m8[:, 0:8], in_values=sco[:, :], imm_value=-BIG),
                         r=["sco", "m8"], w=["sco2"])
                    yield
                    P.op("dve", lambda e: e.max(out=m8[:, 8:16], in_=sco2[:, :]), r=["sco2"], w=["m8"])
                    yield
                    P.op("dve", lambda e: e.tensor_scalar(out=m8[:, 15:16], in0=m8[:, 15:16], scalar1=-1.0e29, scalar2=None, op0=ALU.max),
                         r=["m8"], w=["m8"])
                    yield
                    P.op("dve", lambda e: e.tensor_scalar(out=selm[:, :], in0=sco[:, :], scalar1=m8[:, 15:16], scalar2=None, op0=ALU.is_ge),
                         r=["sco", "m8"], w=["selm"])
                    yield
                    P.op("pe", lambda e: e.transpose(out=TR[0:NSB, 0:128], in_=selm[:, 0:NSB], identity=ident[:, :]),
                         r=["selm", "ident"], w=[kTR])
                    P.op("act", lambda e: e.activation(out=selT[par][0:NSB, :, :], in_=TR[0:NSB, 0:128].unsqueeze(1).to_broadcast([NSB, 4, 128]),
                                                       func=AF.Copy, scale=60.0, bias=-60.0), r=[kTR], w=[f"selT{par}"])
                    yield

                def genB(g, i, par):
                    kqa = f"qa{par}"
                    rhsq = qa[par][:].rearrange("p h t -> p (h t)")
                    pairs = []
                    for kt in range(i + 1):
                        pairs.append(("s", kt, "mx" if kt < i else 0))
                    w0 = max(0, i - 4)
                    for kt in range(w0, i + 1):
                        pairs.append(("w", kt, 0 if kt == i else (1 if kt == i - 4 else None)))
                    nS = i + 1
                    nW = i + 1 - w0

                    def front(k):
                        br, kt, msk = pairs[k]
                        b2 = k % 4
                        scp, kscp = SC4[b2]
                        kk, kkk = (kS, "kS") if br == "s" else (kW, "kW")
                        if msk == "mx":
                            P.op("pe", lambda e: e.matmul(out=scp[:, :], lhsT=kk[:, kt * 128:(kt + 1) * 128], rhs=rhsq, start=True, stop=False),
                                 r=[kkk, kqa], w=[kscp])
                            P.op("pe", lambda e: e.matmul(out=scp[:, :], lhsT=expt[0:NSB, kt, :],
                                                          rhs=selT[par][0:NSB, :, :].rearrange("p h t -> p (h t)"), start=False, stop=True),
                                 r=["expt", f"selT{par}"], w=[kscp])
                        else:
                            P.op("pe", lambda e: e.matmul(out=scp[:, :], lhsT=kk[:, kt * 128:(kt + 1) * 128], rhs=rhsq, start=True, stop=True),
                                 r=[kkk, kqa], w=[kscp])
                        P.op("act", lambda e: e.activation(out=es[b2][:, :], in_=scp[:, :], func=AF.Exp), r=[kscp], w=[f"es{b2}"])
                        if msk is not None and msk != "mx":
                            mk, kmk = tri2[:, msk, :], "tri2"
                            P.op("dve", lambda e: e.tensor_tensor(
                                out=es[b2][:, :].rearrange("p (h t) -> p h t", h=4), in0=es[b2][:, :].rearrange("p (h t) -> p h t", h=4),
                                in1=mk.unsqueeze(1).to_broadcast([128, 4, 128]), op=ALU.mult), r=[f"es{b2}", kmk], w=[f"es{b2}"])

                    def back(k):
                        br, kt, msk = pairs[k]
                        b2 = k % 4
                        if br == "s":
                            P.op("pe", lambda e: e.matmul(out=OS[0:65, :], lhsT=vS[:, kt, :], rhs=es[b2][:, :], start=(k == 0), stop=(k == nS - 1)),
                                 r=["vS", f"es{b2}"], w=[kOS])
                        else:
                            P.op("pe", lambda e: e.matmul(out=OW[0:65, :], lhsT=vW[:, kt, :], rhs=es[b2][:, :], start=(k == nS), stop=(k == nS + nW - 1)),
                                 r=["vW", f"es{b2}"], w=[kOW])

                    DEPTH = 3
                    for k in range(min(DEPTH, len(pairs))):
                        front(k)
                        yield
                    for k in range(len(pairs)):
                        if k + DEPTH < len(pairs):
                            front(k + DEPTH)
                        back(k)
                        yield

                def eagerC():
                    P.op("act", lambda e: e.copy(out=oTs[:, :], in_=OS[0:65, :]), r=[kOS], w=["oTs"])
                    P.op("act", lambda e: e.copy(out=oTw[:, :], in_=OW[0:65, :]), r=[kOW], w=["oTw"])

                def genC(g, i, par):
                    t0 = i * 128
                    fin_tok(oTs, "oTs", par, 1)
                    yield
                    fin_tok(oTw, "oTw", par, 2)
                    yield
                    gv_ = gt[par][:, 12 * g:12 * g + 12].rearrange("p (h b) -> p b h", b=3)
                    P.op("dve", lambda e: e.tensor_tensor(out=coef[:], in0=rs[par][:], in1=gv_, op=ALU.mult), r=[f"rs3_{par}", f"gt{par}"], w=["coef3"])
                    yield
                    for h in range(4):
                        P.op("dve", lambda e, h=h: e.tensor_scalar(out=yb[:, h * 64:(h + 1) * 64], in0=otok[par][0][:, h, 0:64],
                                                                   scalar1=coef[:, 0, h:h + 1], scalar2=None, op0=ALU.mult),
                             r=[f"otok{par}_0", "coef3"], w=["yb3"])
                        yield
                        for br in (1, 2):
                            P.op("dve", lambda e, h=h, br=br: e.scalar_tensor_tensor(
                                out=yb[:, h * 64:(h + 1) * 64], in0=otok[par][br][:, h, 0:64], scalar=coef[:, br, h:h + 1],
                                in1=yb[:, h * 64:(h + 1) * 64], op0=ALU.mult, op1=ALU.add), r=[f"otok{par}_{br}", "coef3", "yb3"], w=["yb3"])
                            yield
                    for c2 in range(2):
                        P.op("pe", lambda e, c2=c2: e.transpose(out=TR[:, c2 * 128:(c2 + 1) * 128], in_=yb[:, c2 * 128:(c2 + 1) * 128],
                                                                identity=ident[:, :]), r=["yb3", "ident"], w=[kTR])
                    P.op("act", lambda e: e.copy(out=ybT[:], in_=TR[:, 0:256].rearrange("p (c t) -> p c t", c=2)), r=[kTR], w=["ybT3"])
                    yield
                    P.dma("sp", ybT_scr[:, 2 * g:2 * g + 2, t0:t0 + 128], ybT[:], r=["ybT3"], w=[f"ybTs{g}_{i}"])
                    yield

                def drive(gens):
                    while gens:
                        for gen in list(gens):
                            try:
                                next(gen)
                            except StopIteration:
                                gens.remove(gen)

                for g in range(2):
                    P.dma("sp", kS[0:64, :], kT_scr[2 * g], w=["kS"])
                    P.dma("sp", kW[0:64, :], kT_scr[2 * g + 1], w=["kW"])
                    P.dma("pool", kS[64:68, :], kaug_d, w=["kS"])
                    P.dma("pool", kW[64:68, :], kaug_d, w=["kW"])
                    P.dma("sp", vS[:, :, 0:64], kvtm_scr[:, 384 + g * 64:448 + g * 64].rearrange("(kt p) d -> p kt d", p=128), w=["vS"])
                    P.dma("sp", vW[:, :, 0:64], kvtm_scr[:, 640 + g * 64:704 + g * 64].rearrange("(kt p) d -> p kt d", p=128), w=["vW"])
                    P.op("dve", lambda e: e.memset(vS[:, :, 64:65], 1.0), w=["vS"])
                    P.op("dve", lambda e: e.memset(vW[:, :, 64:65], 1.0), w=["vW"])
                    drive([genA(g, 0, 0)])
                    for i in range(NT):
                        gens = []
                        if i >= 1:
                            eagerC()
                            gens.append(genC(g, i - 1, (i - 1) % R3))
                        if i + 1 < NT:
                            gens.append(genA(g, i + 1, (i + 1) % R3))
                        gens.append(genB(g, i, i % R3))
                        drive(gens)
                    eagerC()
                    drive([genC(g, NT - 1, (NT - 1) % R3)])
                P.run()


        def phase_nsas():
            BIG = 1.0e30
            NQ = TS
            NQC = 4 * NQ
            with ExitStack() as s5:
                ovs = sbuf(s5, "ovs_sb", [128, NCTS, NSBS], BF16)
                tri2 = sbuf(s5, "tri2_5", [128, 2, 128], BF16)
                expts = sbuf(s5, "expts_sb", [128, 64, 128], BF16)
                pwm = sbuf(s5, "pwm5", [128, 4, 8], F32)
                wcb = sbuf(s5, "wcb5", [64, 2, 64], BF16)
                pio = sbuf(s5, "pio", [128, 1], F32)
                P.dma("pool", ovs[:], ovs_d, w=["ovs"])
                P.dma("pool", tri2[:], tri2_d, w=["tri2_5"])
                P.dma("pool", expts[:], expts_d, w=["expts"])
                P.dma("sp", pwm[:], pw_d, w=["pwm5"])
                P.dma("pool", wcb[:], wc_d, w=["wcb5"])
                P.dma("sp", pio[:], piota, w=["pio"])
                kS = sbuf(s5, "kS5", [68, 2, NKT * 128], BF16)
                vS = sbuf(s5, "vS5", [128, NKT, 2, 65], BF16)
                kW = sbuf(s5, "kW5", [68, 2, 5 * 128], BF16)
                vW = sbuf(s5, "vW5", [128, 5, 2, 65], BF16)
                kcT = sbuf(s5, "kcT5", [68, 2, NCTS * 128], BF16)
                vc = sbuf(s5, "vc5", [128, NCTS, 2, 65], BF16)
                pooledb = sbuf(s5, "pooled5", [64, 4, NCTS * 128], BF16)
                pg = [sbuf(s5, f"pg{j}", [128, 512], F32) for j in range(3)]
                newt = sbuf(s5, "newt", [128, 768], F32)
                cwt = [sbuf(s5, f"cwt{j}", [128, 256], F32) for j in range(2)]
                ptb = sbuf(s5, "ptb", [128, NPG], I32)
                idxf = sbuf(s5, "idxf", [128, NPG], F32)
                idxi = sbuf(s5, "idxi", [128, NPG], I32)
                qa = sbuf(s5, "qa5", [68, 4, NQ], BF16)
                gt = sbuf(s5, "gt5", [NQ, 24], F32)
                ecmp = sbuf(s5, "ecmp5", [128, NCTS, NQC], BF16)
                es = [sbuf(s5, f"es5_{j}", [128, NQC], BF16) for j in range(2)]
                oT = sbuf(s5, "oT5", [65, NQC], F32)
                otok = [sbuf(s5, f"otok5_{j}", [NQ, 4, 65], F32) for j in range(3)]
                rs = sbuf(s5, "rs5", [NQ, 3, 4], F32)
                coef = sbuf(s5, "coef5", [NQ, 3, 4], F32)
                sco = sbuf(s5, "sco5", [NQ, NSBS], F32)
                sco2 = sbuf(s5, "sco25", [NQ, NSBS], F32)
                m8 = sbuf(s5, "m85", [NQ, 16], F32)
                selm = sbuf(s5, "selm5", [NQ, NSBS], F32)
                selT = sbuf(s5, "selT5", [128, 2, NQ], BF16)
                yb = sbuf(s5, "yb5", [NQ, 512], F32)
                ybT = sbuf(s5, "ybT5", [128, 4, NQ], BF16)
                SC = [(pB[0], "pB0"), (pB[1], "pB1")]
                OC, kOC = pB[2], "pB2"
                OS, kOS = pB[3], "pB3"
                OW, kOW = pA[0][:, 0:512], "pA0lo"
                TR, kTR = pA[0][:, 512:1024], "pA0hi"
                MX = [(pA[1][:, 0:512], "pA1lo"), (pA[1][:, 512:1024], "pA1hi")]
                TRk, kTRk = pA[1][:, 0:512], "pA1lo"

                for g in range(2):
                    P.dma("pool", kS[64:68, g, :], kaugs_d, w=["kS5"])
                    P.dma("pool", kW[64:68, g, :], kaugs_d[:, (NKT - 5) * 128:NKT * 128], w=["kW5"])
                    P.dma("pool", kcT[64:68, g, :], caugs_d, w=["kcT5"])
                P.op("dve", lambda e: e.memset(vS[:, :, :, 64:65], 1.0), w=["vS5"])
                P.op("dve", lambda e: e.memset(vW[:, :, :, 64:65], 1.0), w=["vW5"])
                P.op("dve", lambda e: e.memset(vc[:, :, :, 64:65], 1.0), w=["vc5"])
                P.op("dve", lambda e: e.memset(newt[:], 0.0), w=["newt"])
                P.op("dve", lambda e: e.memset(pooledb[:], 0.0), w=["pooled5"])

                def finalize(ps, kps, br):
                    P.op("act", lambda e: e.copy(out=oT[:, :], in_=ps[0:65, 0:NQC]), r=[kps], w=["oT5"])
                    for h in range(4):
                        P.op("pe", lambda e, h=h: e.transpose(out=TR[0:NQ, h * 65:(h + 1) * 65], in_=oT[0:65, h * NQ:(h + 1) * NQ],
                                                              identity=ident[0:65, 0:65]), r=["oT5", "ident"], w=[kTR])
                    P.op("dve", lambda e: e.tensor_copy(out=otok[br][:], in_=TR[0:NQ, 0:260].rearrange("p (h d) -> p h d", h=4)),
                         r=[kTR], w=[f"otok5_{br}"])
                    P.op("dve", lambda e: e.tensor_scalar(out=rs[:, br, :], in0=otok[br][:, :, 64], scalar1=1.0e-30, scalar2=None,
                                                          op0=ALU.max), r=[f"otok5_{br}"], w=["rs5"])
                    P.op("dve", lambda e: e.reciprocal(out=rs[:, br, :], in_=rs[:, br, :]), r=["rs5"], w=["rs5"])

                trk_cnt = [0]

                def kT_from(src_ap, ksrc, dst_fn):
                    alt = trk_cnt[0] % 2
                    trk_cnt[0] += 1
                    bank, kbank = MX[alt]
                    for g in range(2):
                        P.op("pe", lambda e, g=g: e.transpose(out=bank[0:64, g * 128:(g + 1) * 128], in_=src_ap(g), identity=ident[:, :]),
                             r=[ksrc, "ident"], w=[kbank])
                    return bank, kbank

                def seq(b):
                    P.dma("sp", ptb[:], ptab[b].partition_broadcast(128), w=["ptb"])
                    P.op("dve", lambda e: e.tensor_copy(out=idxf[:], in_=ptb[:]), r=["ptb"], w=["idxf"])
                    P.op("dve", lambda e: e.tensor_scalar(out=idxf[:], in0=idxf[:], scalar1=128.0, scalar2=pio[:, 0:1], op0=ALU.mult, op1=ALU.add),
                         r=["idxf", "pio"], w=["idxf"])
                    P.op("dve", lambda e: e.tensor_copy(out=idxi[:], in_=idxf[:]), r=["idxf"], w=["idxi"])

                    def gather(j):
                        P.idma(pg[j % 3][:, :], ckv, idxi[:, j:j + 1], r=["idxi"], w=[f"pg{j % 3}"])

                    gather(0)
                    if NPG > 1:
                        gather(1)
                    for j in range(NPG):
                        if j + 2 < NPG:
                            gather(j + 2)
                        t_, kt_ = pg[j % 3], f"pg{j % 3}"
                        n_, kn_ = pg[(j + 1) % 3], f"pg{(j + 1) % 3}"
                        last = (j == NPG - 1)
                        for which in range(2):
                            for g in range(2):
                                pb, kpb = pB[which * 2 + g], f"pB{which * 2 + g}"
                                c0 = which * 128 + g * 64
                                jc = 8 * (j % 64)
                                P.op("pe", lambda e, pb=pb, c0=c0, jc=jc, which=which, t_=t_, last=last: e.matmul(
                                    out=pb[0:64, jc:jc + 8], lhsT=t_[:, c0:c0 + 64], rhs=pwm[:, 2 * which, :], start=True, stop=last),
                                     r=[kt_, "pwm5"], w=[kpb])
                                if not last:
                                    P.op("pe", lambda e, pb=pb, c0=c0, jc=jc, which=which, n_=n_: e.matmul(
                                        out=pb[0:64, jc:jc + 8], lhsT=n_[0:16, c0:c0 + 64], rhs=pwm[0:16, 2 * which + 1, :],
                                        start=False, stop=True), r=[kn_, "pwm5"], w=[kpb])
                        if j % 64 == 63 or last:
                            blk0 = (j // 64) * 512
                            ncol = 8 * (j % 64 + 1)
                            for idx4 in range(4):
                                P.op("act", lambda e, idx4=idx4, blk0=blk0, ncol=ncol: e.copy(out=pooledb[:, idx4, blk0:blk0 + ncol],
                                                                                           in_=pB[idx4][0:64, 0:ncol]),
                                     r=[f"pB{idx4}"], w=["pooled5"])
                        bk_, kbk_ = kT_from(lambda g, t_=t_: t_[:, 256 + g * 64:320 + g * 64], kt_, None)
                        P.op("act", lambda e, j=j, bk_=bk_: e.copy(out=kS[0:64, :, j * 128:(j + 1) * 128],
                                                                   in_=bk_[0:64, 0:256].rearrange("p (g t) -> p g t", g=2)), r=[kbk_], w=["kS5"])
                        P.op("pool", lambda e, j=j, t_=t_: e.tensor_copy(out=vS[:, j, :, 0:64],
                                                                         in_=t_[:, 384:512].rearrange("p (g d) -> p g d", g=2)),
                             r=[kt_], w=["vS5"])
                    P.dma("sp", newt[0:NQ, :], kvs_scr[b * NQ:(b + 1) * NQ, :], w=["newt"])
                    bk1, kbk1 = kT_from(lambda g: newt[:, 256 + g * 64:320 + g * 64], "newt", None)
                    P.op("act", lambda e: e.copy(out=kS[0:64, :, NPG * 128:(NPG + 1) * 128],
                                                 in_=bk1[0:64, 0:256].rearrange("p (g t) -> p g t", g=2)), r=[kbk1], w=["kS5"])
                    P.op("pool", lambda e: e.tensor_copy(out=vS[:, NPG, :, 0:64], in_=newt[:, 384:512].rearrange("p (g d) -> p g d", g=2)),
                         r=["newt"], w=["vS5"])
                    bk2, kbk2 = kT_from(lambda g: newt[:, 512 + g * 64:576 + g * 64], "newt", None)
                    P.op("act", lambda e: e.copy(out=kW[0:64, :, 4 * 128:5 * 128],
                                                 in_=bk2[0:64, 0:256].rearrange("p (g t) -> p g t", g=2)), r=[kbk2], w=["kW5"])
                    P.op("pool", lambda e: e.tensor_copy(out=vW[:, 4, :, 0:64], in_=newt[:, 640:768].rearrange("p (g d) -> p g d", g=2)),
                         r=["newt"], w=["vW5"])
                    for a in range(4):
                        c_, kc_ = cwt[a % 2], f"cwt{a % 2}"
                        P.dma("sp", c_[:], cwin4[b, a], w=[kc_])
                        bk3, kbk3 = kT_from(lambda g, c_=c_: c_[:, g * 64:(g + 1) * 64], kc_, None)
                        P.op("act", lambda e, a=a, bk3=bk3: e.copy(out=kW[0:64, :, a * 128:(a + 1) * 128],
                                                                   in_=bk3[0:64, 0:256].rearrange("p (g t) -> p g t", g=2)), r=[kbk3], w=["kW5"])
                        P.op("pool", lambda e, a=a, c_=c_: e.tensor_copy(out=vW[:, a, :, 0:64],
                                                                         in_=c_[:, 128:256].rearrange("p (g d) -> p g d", g=2)),
                             r=[kc_], w=["vW5"])
                    for g in range(2):
                        for hf in range(NCTS * 128 // 512 if NCTS * 128 >= 512 else 1):
                            wd = min(512, NCTS * 128)
                            P.op("pe", lambda e, g=g, hf=hf, wd=wd: e.matmul(out=pB[0][0:64, 0:wd], lhsT=wcb[:, 0, :],
                                                                             rhs=pooledb[:, g, hf * wd:(hf + 1) * wd], start=True, stop=True),
                                 r=["wcb5", "pooled5"], w=["pB0"])
                            P.op("act", lambda e, g=g, hf=hf, wd=wd: e.copy(out=kcT[0:64, g, hf * wd:(hf + 1) * wd], in_=pB[0][0:64, 0:wd]),
                                 r=["pB0"], w=["kcT5"])
                        for ct in range(NCTS):
                            P.op("pe", lambda e, g=g, ct=ct: e.matmul(out=pB[1][:, ct * 64:(ct + 1) * 64],
                                                                      lhsT=pooledb[:, 2 + g, ct * 128:(ct + 1) * 128], rhs=wcb[:, 1, :],
                                                                      start=True, stop=True), r=["wcb5", "pooled5"], w=["pB1"])
                        P.op("act", lambda e, g=g: e.copy(out=vc[:, :, g, 0:64],
                                                          in_=pB[1][:, 0:NCTS * 64].rearrange("p (c d) -> p c d", d=64)), r=["pB1"], w=["vc5"])
                    P.dma("sp", gt[:], gates_s_scr[b * NQ:(b + 1) * NQ, :], w=["gt5"])
                    for g in range(2):
                        attend(b, g)
                    for c4 in range(4):
                        P.op("pe", lambda e, c4=c4: e.transpose(out=TR[:, c4 * NQ:(c4 + 1) * NQ], in_=yb[0:NQ, c4 * 128:(c4 + 1) * 128],
                                                                identity=ident[0:NQ, 0:NQ]), r=["yb5", "ident"], w=[kTR])
                    P.op("act", lambda e: e.copy(out=ybT[:], in_=TR[:, 0:4 * NQ].rearrange("p (c t) -> p c t", c=4)), r=[kTR], w=["ybT5"])
                    P.dma("sp", ybT_scr[:, :, L + b * NQ:L + (b + 1) * NQ], ybT[:], r=["ybT5"], w=[f"ybTss{b}"])

                def attend(b, g):
                    P.dma("sp", qa[0:64, :, :], qTs_scr[g, :, :, b * NQ:(b + 1) * NQ], w=["qa5"])
                    P.dma("pool", qa[64:68, :, :], qaugs_d[g], w=["qa5"])
                    rhsq = qa[:].rearrange("p h t -> p (h t)")
                    for ct in range(NCTS):
                        scp, kscp = SC[ct % 2]
                        P.op("pe", lambda e, ct=ct, scp=scp: e.matmul(out=scp[:, 0:NQC], lhsT=kcT[:, g, ct * 128:(ct + 1) * 128], rhs=rhsq,
                                                                      start=True, stop=True), r=["kcT5", "qa5"], w=[kscp])
                        P.op("act", lambda e, ct=ct, scp=scp: e.activation(out=ecmp[:, ct, :], in_=scp[:, 0:NQC], func=AF.Exp),
                             r=[kscp], w=["ecmp5"])
                    for ct in range(NCTS):
                        P.op("pe", lambda e, ct=ct: e.matmul(out=OC[0:65, 0:NQC], lhsT=vc[:, ct, g, :], rhs=ecmp[:, ct, :],
                                                             start=(ct == 0), stop=(ct == NCTS - 1)), r=["vc5", "ecmp5"], w=[kOC])
                    finalize(OC, kOC, 0)
                    for h in range(4):
                        for ct in range(NCTS):
                            P.op("pe", lambda e, ct=ct, h=h: e.matmul(out=OS[0:NQ, 0:NSBS], lhsT=ecmp[:, ct, h * NQ:(h + 1) * NQ],
                                                                      rhs=ovs[:, ct, :], start=(ct == 0), stop=(ct == NCTS - 1)),
                                 r=["ecmp5", "ovs"], w=[kOS])
                        if h == 0:
                            P.op("dve", lambda e: e.tensor_scalar(out=sco[:, :], in0=OS[0:NQ, 0:NSBS], scalar1=rs[:, 0, 0:1], scalar2=None,
                                                                  op0=ALU.mult), r=[kOS, "rs5"], w=["sco5"])
                        else:
                            P.op("dve", lambda e, h=h: e.scalar_tensor_tensor(out=sco[:, :], in0=OS[0:NQ, 0:NSBS], scalar=rs[:, 0, h:h + 1],
                                                                             in1=sco[:, :], op0=ALU.mult, op1=ALU.add),
                                 r=[kOS, "rs5", "sco5"], w=["sco5"])
                    P.op("dve", lambda e: e.memset(sco[:, 0:1], 1.0e4), r=["sco5"], w=["sco5"])
                    P.op("dve", lambda e: e.memset(sco[:, NSBS - 2:NSBS], 1.0e4), r=["sco5"], w=["sco5"])
                    P.op("dve", lambda e: e.max(out=m8[:, 0:8], in_=sco[:, :]), r=["sco5"], w=["m85"])
                    P.op("dve", lambda e: e.match_replace(out=sco2[:, :], in_to_replace=m8[:, 0:8], in_values=sco[:, :], imm_value=-BIG),
                         r=["sco5", "m85"], w=["sco25"])
                    P.op("dve", lambda e: e.max(out=m8[:, 8:16], in_=sco2[:, :]), r=["sco25"], w=["m85"])
                    P.op("dve", lambda e: e.tensor_scalar(out=m8[:, 15:16], in0=m8[:, 15:16], scalar1=-1.0e29, scalar2=None, op0=ALU.max),
                         r=["m85"], w=["m85"])
                    P.op("dve", lambda e: e.tensor_scalar(out=selm[:, :], in0=sco[:, :], scalar1=m8[:, 15:16], scalar2=None, op0=ALU.is_ge),
                         r=["sco5", "m85"], w=["selm5"])
                    for ch in range(2):
                        wch = min(128, NSBS - 1 - ch * 128)
                        if wch <= 0:
                            continue
                        P.op("pe", lambda e, ch=ch, wch=wch: e.transpose(out=TR[0:wch, ch * NQ:(ch + 1) * NQ],
                                                                         in_=selm[0:NQ, ch * 128:ch * 128 + wch], identity=ident[0:NQ, 0:NQ]),
                             r=["selm5", "ident"], w=[kTR])
                        P.op("act", lambda e, ch=ch, wch=wch: e.copy(out=selT[0:wch, ch, :], in_=TR[0:wch, ch * NQ:(ch + 1) * NQ]),
                             r=[kTR], w=["selT5"])
                    pairs = [("s", j, "mx") for j in range(NPG)] + [("s", NPG, 0)]
                    pairs += [("w", 0, 1), ("w", 1, None), ("w", 2, None), ("w", 3, None), ("w", 4, 0)]
                    nS = NPG + 1

                    def front(k):
                        br, kt, msk = pairs[k]
                        b2 = k % 2
                        scp, kscp = SC[b2]
                        if msk == "mx":
                            mxp, kmx = MX[b2]
                            wch = min(128, NSBS - 1 - (kt // 64) * 128)
                            P.op("pe", lambda e: e.matmul(out=mxp[:, 0:NQ], lhsT=expts[0:wch, kt % 64, :], rhs=selT[0:wch, kt // 64, :],
                                                          start=True, stop=True), r=["expts", "selT5"], w=[kmx])
                        kk, kkk = (kS, "kS5") if br == "s" else (kW, "kW5")
                        P.op("pe", lambda e: e.matmul(out=scp[:, 0:NQC], lhsT=kk[:, g, kt * 128:(kt + 1) * 128], rhs=rhsq, start=True, stop=True),
                             r=[kkk, "qa5"], w=[kscp])
                        P.op("act", lambda e: e.activation(out=es[b2][:, :], in_=scp[:, 0:NQC], func=AF.Exp), r=[kscp], w=[f"es5_{b2}"])
                        if msk is not None:
                            if msk == "mx":
                                mk, kmk = MX[b2][0][:, 0:NQ], MX[b2][1]
                            else:
                                mk, kmk = tri2[:, msk, 0:NQ], "tri2_5"
                            P.op("dve", lambda e: e.tensor_tensor(
                                out=es[b2][:, :].rearrange("p (h t) -> p h t", h=4), in0=es[b2][:, :].rearrange("p (h t) -> p h t", h=4),
                                in1=mk.unsqueeze(1).to_broadcast([128, 4, NQ]), op=ALU.mult), r=[f"es5_{b2}", kmk], w=[f"es5_{b2}"])

                    def back(k):
                        br, kt, msk = pairs[k]
                        b2 = k % 2
                        if br == "s":
                            P.op("pe", lambda e: e.matmul(out=OS[0:65, 0:NQC], lhsT=vS[:, kt, g, :], rhs=es[b2][:, :], start=(k == 0), stop=(k == nS - 1)),
                                 r=["vS5", f"es5_{b2}"], w=[kOS])
                        else:
                            P.op("pe", lambda e: e.matmul(out=OW[0:65, 0:NQC], lhsT=vW[:, kt, g, :], rhs=es[b2][:, :], start=(k == nS), stop=(k == len(pairs) - 1)),
                                 r=["vW5", f"es5_{b2}"], w=[kOW])

                    front(0)
                    for k in range(len(pairs)):
                        if k + 1 < len(pairs):
                            front(k + 1)
                        back(k)
                    finalize(OS, kOS, 1)
                    finalize(OW, kOW, 2)
                    gv_ = gt[:, 12 * g:12 * g + 12].rearrange("p (h b) -> p b h", b=3)
                    P.op("dve", lambda e: e.tensor_tensor(out=coef[:], in0=rs[:], in1=gv_, op=ALU.mult), r=["rs5", "gt5"], w=["coef5"])
                    for h in range(4):
                        c0 = (4 * g + h) * 64
                        P.op("dve", lambda e, h=h, c0=c0: e.tensor_scalar(out=yb[:, c0:c0 + 64], in0=otok[0][:, h, 0:64],
                                                                          scalar1=coef[:, 0, h:h + 1], scalar2=None, op0=ALU.mult),
                             r=["otok5_0", "coef5"], w=["yb5"])
                        for br in (1, 2):
                            P.op("dve", lambda e, h=h, br=br, c0=c0: e.scalar_tensor_tensor(
                                out=yb[:, c0:c0 + 64], in0=otok[br][:, h, 0:64], scalar=coef[:, br, h:h + 1],
                                in1=yb[:, c0:c0 + 64], op0=ALU.mult, op1=ALU.add), r=[f"otok5_{br}", "coef5", "yb5"], w=["yb5"])

                for b in range(NSEQ):
                    seq(b)
                P.run()

        tiles = [(xh[j * 128:(j + 1) * 128, :], 128, yp[j * 128:(j + 1) * 128, :]) for j in range(NTH)]
        tiles.append((xs, NSTOK, ysm))
        def phase_tail_a():
            with ExitStack() as s4:
                wgb = sbuf(s4, "wgb", [128, KC, 2048], BF16)
                wob = sbuf(s4, "wob", [128, KC, D], BF16)
                wr32 = sbuf(s4, "wr32", [128, KC, 36], F32)
                brb = sbuf(s4, "brb", [128, 36], F32)
                xt = [sbuf(s4, f"x4_{j}", [128, D], F32) for j in range(2)]
                junk = sbuf(s4, "junk4", [128, D], F32)
                ss = sbuf(s4, "ss4", [128, 1], F32)
                rstd = sbuf(s4, "rstd4", [128, 1], F32)
                ss2 = sbuf(s4, "ss4b", [128, 1], F32)
                rstd2 = sbuf(s4, "rstd4b", [128, 1], F32)
                xn = sbuf(s4, "xn4", [128, D], F32)
                xnT = sbuf(s4, "xnT4", [128, KC, 128], BF16)
                sga = sbuf(s4, "sga", [128, D], F32)
                sgb = sbuf(s4, "sgb", [128, D], F32)
                h = [sbuf(s4, f"h4_{j}", [128, D], F32) for j in range(2)]
                hn = sbuf(s4, "hn4", [128, D], F32)
                hnT32 = sbuf(s4, "hnT32", [128, KC, 128], F32)
                hnTb = [sbuf(s4, f"hnTb{j}", [128, KC, 128], BF16) for j in range(2)]
                lg = sbuf(s4, "lg", [128, 36], F32)
                sm = sbuf(s4, "sm4", [128, 64], F32)
                wab = sbuf(s4, "wab", [128, 4, D], BF16)
                selb = sbuf(s4, "selb", [128, 2], F32)
                y0 = sbuf(s4, "y0", [128, 4, 128], BF16)
                y1 = sbuf(s4, "y1", [128, 4, 128], BF16)
                yab = sbuf(s4, "yab", [128, 4, 128], BF16)
                m = sbuf(s4, "m4", [128, D], F32)
                mT = sbuf(s4, "mT4", [128, KC, 128], BF16)
                P.dma("pool", wab[:], wa.rearrange("(fc p) n -> p fc n", p=128), w=["wab"])
                wbb = sbuf(s4, "wbb", [128, 4, D], BF16)
                P.dma("pool", wbb[:], wb.rearrange("(fc p) n -> p fc n", p=128), w=["wbb"])
                P.dma("sp", selb[:], selv, w=["selb"])
                P.dma("pool", wgb[:], wgab.rearrange("(kc p) n -> p kc n", p=128), w=["wgb"])
                P.dma("pool", wob[:], wo.rearrange("(kc p) n -> p kc n", p=128), w=["wob"])
                P.dma("sp", wr32[:], wr.rearrange("(kc p) n -> p kc n", p=128), w=["wr32"])
                P.dma("sp", brb[:], br[0].partition_broadcast(128), w=["brb"])

                def tail_tile(j):
                    x_src, npart, _ = tiles[j]
                    sl = j % 2
                    kx = f"x4_{sl}"
                    kh = f"h4_{sl}"
                    P.dma("sp", xt[sl][:npart, :], x_src, w=[kx])
                    norm_transpose(xt[sl][:npart, :], kx, npart, junk, ss, rstd, xn, pA[0], "pA0",
                                   g1T, "g1T", [(xnT, "xnT4")], "t4")
                    for gi, (dst, kd) in enumerate(((sga, "sga"), (sgb, "sgb"))):
                        for nh in range(2):
                            pb = pB[(gi * 2 + nh) % 4]
                            kpb = f"pB{(gi * 2 + nh) % 4}"
                            c0 = gi * 1024 + nh * 512
                            for kc in range(KC):
                                P.op("pe", lambda e, kc=kc, pb=pb, c0=c0: e.matmul(
                                    out=pb[:npart, :], lhsT=xnT[:, kc, :npart], rhs=wgb[:, kc, c0:c0 + 512],
                                    start=(kc == 0), stop=(kc == KC - 1)), r=["xnT4", "wgb"], w=[kpb])
                            P.op("act", lambda e, pb=pb, dst=dst, nh=nh: e.activation(
                                out=dst[:npart, nh * 512:(nh + 1) * 512], in_=pb[:npart, :], func=AF.Sigmoid),
                                 r=[kpb], w=[kd])
                    if "gdn" in phases:
                        if j < NTH:
                            P.dma("sp", y0[:], yaT_scr[:, :, j * 128:(j + 1) * 128], w=["y0"])
                            P.dma("sp", y1[:], yaT_scr[:, :, LH + j * 128:LH + (j + 1) * 128], w=["y1"])
                            P.op("pool", lambda e: e.tensor_scalar(out=y0[:], in0=y0[:], scalar1=selb[:, 0:1], scalar2=None, op0=ALU.mult),
                                 r=["y0", "selb"], w=["y0"])
                            P.op("dve", lambda e: e.scalar_tensor_tensor(out=yab[:], in0=y1[:], scalar=selb[:, 1:2], in1=y0[:],
                                                                         op0=ALU.mult, op1=ALU.add), r=["y0", "y1", "selb"], w=["yab"])
                        else:
                            P.dma("sp", yab[:, :, :npart], yaT_scr[:, :, L:L + npart], w=["yab"])
                        for nh in range(2):
                            for fc in range(4):
                                P.op("pe", lambda e, nh=nh, fc=fc: e.matmul(
                                    out=pA[1][:npart, nh * 512:(nh + 1) * 512], lhsT=yab[:, fc, :npart],
                                    rhs=wab[:, fc, nh * 512:(nh + 1) * 512], start=(fc == 0), stop=(fc == 3)),
                                     r=["yab", "wab"], w=["pA1"])
                        P.op("dve", lambda e: e.tensor_tensor(out=m[:npart, :], in0=sga[:npart, :], in1=pA[1][:npart, :], op=ALU.mult),
                             r=["sga", "pA1"], w=["m4"])
                        if ("nsa" in phases and j < NTH) or ("nsas" in phases and j == NTH):
                            if j < NTH:
                                P.dma("sp", y0[:], ybT_scr[:, :, j * 128:(j + 1) * 128], w=["y0"])
                                P.dma("sp", y1[:], ybT_scr[:, :, LH + j * 128:LH + (j + 1) * 128], w=["y1"])
                                P.op("pool", lambda e: e.tensor_scalar(out=y0[:], in0=y0[:], scalar1=selb[:, 0:1], scalar2=None, op0=ALU.mult),
                                     r=["y0", "selb"], w=["y0"])
                                P.op("dve", lambda e: e.scalar_tensor_tensor(out=yab[:], in0=y1[:], scalar=selb[:, 1:2], in1=y0[:],
                                                                            op0=ALU.mult, op1=ALU.add), r=["y0", "y1", "selb"], w=["yab"])
                            else:
                                P.dma("sp", yab[:, :, :npart], ybT_scr[:, :, L:L + npart], w=["yab"])
                            for nh in range(2):
                                for fc in range(4):
                                    P.op("pe", lambda e, nh=nh, fc=fc: e.matmul(
                                        out=pA[1][:npart, nh * 512:(nh + 1) * 512], lhsT=yab[:, fc, :npart],
                                        rhs=wbb[:, fc, nh * 512:(nh + 1) * 512], start=(fc == 0), stop=(fc == 3)),
                                         r=["yab", "wbb"], w=["pA1"])
                            P.op("dve", lambda e: e.tensor_tensor(out=hn[:npart, :], in0=sgb[:npart, :], in1=pA[1][:npart, :], op=ALU.mult),
                                 r=["sgb", "pA1"], w=["xnt4b"])
                            P.op("dve", lambda e: e.tensor_tensor(out=m[:npart, :], in0=m[:npart, :], in1=hn[:npart, :], op=ALU.add),
                                 r=["m4", "xnt4b"], w=["m4"])
                        for kc in range(KC):
                            P.op("pe", lambda e, kc=kc: e.transpose(out=pA[0][:, kc * 128:kc * 128 + npart],
                                                                    in_=m[:npart, kc * 128:(kc + 1) * 128],
                                                                    identity=ident[:npart, :npart]), r=["m4", "ident"], w=["pA0"])
                        P.op("act", lambda e: e.copy(out=mT[:, :, :npart],
                                                     in_=pA[0][:].rearrange("p (kc t) -> p kc t", kc=KC)[:, :, :npart]),
                             r=["pA0"], w=["mT4"])
                        for nh in range(2):
                            for kc in range(KC):
                                P.op("pe", lambda e, nh=nh, kc=kc: e.matmul(
                                    out=pA[1][:npart, nh * 512:(nh + 1) * 512], lhsT=mT[:, kc, :npart],
                                    rhs=wob[:, kc, nh * 512:(nh + 1) * 512], start=(kc == 0), stop=(kc == KC - 1)),
                                     r=["mT4", "wob"], w=["pA1"])
                        P.op("dve", lambda e: e.tensor_tensor(out=h[sl][:npart, :], in0=xt[sl][:npart, :], in1=pA[1][:npart, :], op=ALU.add),
                             r=[kx, "pA1"], w=[kh])
                    else:
                        P.op("dve", lambda e: e.tensor_copy(out=h[sl][:npart, :], in_=xt[sl][:npart, :]), r=[kx], w=[kh])
                    P.dma("act", h_scr[j, :npart, :], h[sl][:npart, :], r=[kh], w=[f"h_scr{j}"])
                    norm_transpose(h[sl][:npart, :], kh, npart, junk, ss2, rstd2, hn, pA[1], "pA1",
                                   g2T, "g2T", [(hnT32, "hnT32"), (hnTb[sl], f"hnTb{sl}")], "t4b")
                    P.dma("act", hnT_scr[:, :, j * 128:j * 128 + npart], hnTb[sl][:, :, :npart],
                          r=[f"hnTb{sl}"], w=[f"hnT_scr{j}"])
                    for kc in range(KC):
                        P.op("pe", lambda e, kc=kc: e.matmul(out=pB[0][:npart, 0:36], lhsT=hnT32[:, kc, :npart],
                                                             rhs=wr32[:, kc, :], start=(kc == 0), stop=(kc == KC - 1)),
                             r=["hnT32", "wr32"], w=["pB0"])
                    n = npart
                    P.op("dve", lambda e: e.tensor_tensor(out=lg[:n, :], in0=pB[0][:n, 0:36], in1=brb[:n, :], op=ALU.add),
                         r=["pB0", "brb"], w=["lg"])
                    P.op("dve", lambda e: e.tensor_reduce(out=sm[:n, 0:1], in_=lg[:n, 0:4], axis=AX.X, op=ALU.max),
                         r=["lg"], w=["sm"])
                    P.op("dve", lambda e: e.tensor_scalar(out=sm[:n, 4:8], in0=lg[:n, 0:4], scalar1=sm[:n, 0:1], scalar2=None,
                                                          op0=ALU.is_equal), r=["lg", "sm"], w=["sm"])
                    P.op("dve", lambda e: e.tensor_scalar(out=sm[:n, 1:2], in0=sm[:n, 0:1], scalar1=-1.0, scalar2=None,
                                                          op0=ALU.mult), r=["sm"], w=["sm"])
                    P.op("act", lambda e: e.activation(out=sm[:n, 56:60], in_=lg[:n, 0:4], func=AF.Exp, bias=sm[:n, 1:2],
                                                       scale=1.0, accum_out=sm[:n, 2:3]), r=["lg", "sm"], w=["sm"])
                    P.op("dve", lambda e: e.reciprocal(out=sm[:n, 3:4], in_=sm[:n, 2:3]), r=["sm"], w=["sm"])
                    P.op("dve", lambda e: e.tensor_scalar(out=sm[:n, 8:16], in0=lg[:n, 4:12], scalar1=sm[:n, 4:5], scalar2=None,
                                                          op0=ALU.mult), r=["lg", "sm"], w=["sm"])
                    for g in range(1, 4):
                        P.op("dve", lambda e, g=g: e.scalar_tensor_tensor(
                            out=sm[:n, 8:16], in0=lg[:n, 4 + 8 * g:12 + 8 * g], scalar=sm[:n, 4 + g:5 + g],
                            in1=sm[:n, 8:16], op0=ALU.mult, op1=ALU.add), r=["lg", "sm"], w=["sm"])
                    P.op("dve", lambda e: e.max(out=sm[:n, 16:24], in_=sm[:n, 8:16]), r=["sm"], w=["sm"])
                    P.op("dve", lambda e: e.tensor_tensor(out=sm[:n, 24:25], in0=sm[:n, 16:17], in1=sm[:n, 17:18],
                                                          op=ALU.subtract), r=["sm"], w=["sm"])
                    P.op("act", lambda e: e.activation(out=sm[:n, 25:26], in_=sm[:n, 24:25], func=AF.Sigmoid),
                         r=["sm"], w=["sm"])
                    P.op("dve", lambda e: e.tensor_scalar(out=sm[:n, 26:27], in0=sm[:n, 25:26], scalar1=-1.0, scalar2=1.0,
                                                          op0=ALU.mult, op1=ALU.add), r=["sm"], w=["sm"])
                    P.op("dve", lambda e: e.tensor_scalar(out=sm[:n, 27:29], in0=sm[:n, 25:27], scalar1=sm[:n, 3:4], scalar2=None,
                                                          op0=ALU.mult), r=["sm"], w=["sm"])
                    P.op("dve", lambda e: e.tensor_scalar(out=sm[:n, 32:40], in0=sm[:n, 8:16], scalar1=sm[:n, 16:17],
                                                          scalar2=sm[:n, 27:28], op0=ALU.is_equal, op1=ALU.mult),
                         r=["sm"], w=["sm"])
                    P.op("dve", lambda e: e.tensor_scalar(out=sm[:n, 40:48], in0=sm[:n, 8:16], scalar1=sm[:n, 17:18],
                                                          scalar2=sm[:n, 28:29], op0=ALU.is_equal, op1=ALU.mult),
                         r=["sm"], w=["sm"])
                    P.op("dve", lambda e: e.tensor_tensor(out=sm[:n, 48:56], in0=sm[:n, 32:40], in1=sm[:n, 40:48], op=ALU.add),
                         r=["sm"], w=["sm"])
                    for g in range(4):
                        P.op("dve", lambda e, g=g: e.tensor_scalar(out=comb[:n, j, 8 * g:8 * g + 8], in0=sm[:n, 48:56],
                                                                   scalar1=sm[:n, 4 + g:5 + g], scalar2=None, op0=ALU.mult),
                             r=["sm"], w=[f"comb{j}"])

                for j in range(NTT):
                    tail_tile(j)
                P.run()

        def phase_tail_b():
            with ExitStack() as s5:
                gfb = sbuf(s5, "gfb", [128, D], F32)
                P.dma("sp", gfb[:], gf[0].partition_broadcast(128), w=["gfb"])
                NACC = TPP + 1
                acc = [sbuf(s5, f"acc{j}", [128, D], F32) for j in range(NACC)]
                hnTp = sbuf(s5, "hnTp", [128, KC, NACC * 128], BF16)
                wgE = [sbuf(s5, f"wgE{j}", [128, KC, DEXP], BF16) for j in range(2)]
                wuE = [sbuf(s5, f"wuE{j}", [128, KC, DEXP], BF16) for j in range(2)]
                wdE = [sbuf(s5, f"wdE{j}", [128, 2, D], BF16) for j in range(2)]
                sg = [sbuf(s5, f"sg{j}", [128, 512], F32) for j in range(2)]
                hT = sbuf(s5, "hT", [128, 2, 512], BF16)
                junk = sbuf(s5, "junk5", [128, D], F32)
                ss = sbuf(s5, "ss5", [128, 1], F32)
                rstd = sbuf(s5, "rstd5", [128, 1], F32)
                yo = [sbuf(s5, f"yo{j}", [128, D], F32) for j in range(2)]

                passes = []
                j = 0
                while j < NTH:
                    passes.append(list(range(j, min(NTH, j + TPP))))
                    j += TPP
                passes[-1].append(NTH)
                wcount = [0]

                def moe_pass(tl):
                    col0 = {}
                    c = 0
                    for a, tj in enumerate(tl):
                        col0[tj] = c
                        npart = tiles[tj][1]
                        P.dma("sp", acc[a][:npart, :], h_scr[tj, :npart, :], r=[f"h_scr{tj}"], w=[f"acc{a}"])
                        P.dma("act", hnTp[:, :, c:c + npart], hnT_scr[:, :, tj * 128:tj * 128 + npart],
                              r=[f"hnT_scr{tj}"], w=["hnTp"])
                        c += npart
                    groups = []
                    cur, curN = [], 0
                    for a, tj in enumerate(tl):
                        npart = tiles[tj][1]
                        if curN + npart > 512:
                            groups.append(cur)
                            cur, curN = [], 0
                        cur.append((a, tj))
                        curN += npart
                    groups.append(cur)
                    for ex in range(NEXP):
                        wb_ = wcount[0] % 2
                        wcount[0] += 1
                        P.dma("pool", wgE[wb_][:], weg[ex].rearrange("(kc p) f -> p kc f", p=128), w=[f"wgE{wb_}"])
                        P.dma("pool", wuE[wb_][:], weu[ex].rearrange("(kc p) f -> p kc f", p=128), w=[f"wuE{wb_}"])
                        P.dma("pool", wdE[wb_][:], wed[ex].rearrange("(fc p) d -> p fc d", p=128), w=[f"wdE{wb_}"])
                        for grp in groups:
                            g0 = col0[grp[0][1]]
                            N = sum(tiles[tj][1] for (_, tj) in grp)
                            for fc in range(2):
                                for (wt, kw, pb, kpb) in ((wgE[wb_], f"wgE{wb_}", pB[fc], f"pB{fc}"),
                                                          (wuE[wb_], f"wuE{wb_}", pB[2 + fc], f"pB{2 + fc}")):
                                    for kc in range(KC):
                                        P.op("pe", lambda e, kc=kc, wt=wt, pb=pb, fc=fc, g0=g0, N=N: e.matmul(
                                            out=pb[:, 0:N], lhsT=wt[:, kc, fc * 128:(fc + 1) * 128],
                                            rhs=hnTp[:, kc, g0:g0 + N], start=(kc == 0), stop=(kc == KC - 1)),
                                             r=[kw, "hnTp"], w=[kpb])
                                P.op("act", lambda e, fc=fc, N=N: e.activation(out=sg[fc][:, 0:N], in_=pB[fc][:, 0:N],
                                                                               func=AF.Silu), r=[f"pB{fc}"], w=[f"sg{fc}"])
                                P.op("dve", lambda e, fc=fc, N=N: e.tensor_tensor(out=hT[:, fc, 0:N], in0=sg[fc][:, 0:N],
                                                                                  in1=pB[2 + fc][:, 0:N], op=ALU.mult),
                                     r=[f"sg{fc}", f"pB{2 + fc}"], w=[f"hT{fc}"])
                            for gi_, (a, tj) in enumerate(grp):
                                npart = tiles[tj][1]
                                tc0 = col0[tj] - g0
                                pa = pA[gi_ % 2]
                                kpa = f"pA{gi_ % 2}"
                                for nh in range(2):
                                    for fc in range(2):
                                        P.op("pe", lambda e, nh=nh, fc=fc, pa=pa, tc0=tc0, npart=npart, wb_=wb_: e.matmul(
                                            out=pa[:npart, nh * 512:(nh + 1) * 512], lhsT=hT[:, fc, tc0:tc0 + npart],
                                            rhs=wdE[wb_][:, fc, nh * 512:(nh + 1) * 512], start=(fc == 0), stop=(fc == 1)),
                                             r=[f"hT{fc}", f"wdE{wb_}"], w=[kpa])
                                P.op("dve", lambda e, a=a, tj=tj, pa=pa, npart=npart, ex=ex: e.scalar_tensor_tensor(
                                    out=acc[a][:npart, :], in0=pa[:npart, :], scalar=comb[:npart, tj, ex:ex + 1],
                                    in1=acc[a][:npart, :], op0=ALU.mult, op1=ALU.add),
                                     r=[kpa, f"comb{tj}", f"acc{a}"], w=[f"acc{a}"])
                    for a, tj in enumerate(tl):
                        npart = tiles[tj][1]
                        o = a % 2
                        rms_stats(acc[a][:npart, :], npart, junk, ss, rstd, f"acc{a}", "f5")
                        P.op("dve", lambda e, a=a, npart=npart, o=o: e.scalar_tensor_tensor(
                            out=yo[o][:npart, :], in0=acc[a][:npart, :], scalar=rstd[:npart, 0:1], in1=gfb[:npart, :],
                            op0=ALU.mult, op1=ALU.mult), r=[f"acc{a}", "rstdf5", "gfb"], w=[f"yo{o}"])
                        P.dma("sp", tiles[tj][2], yo[o][:npart, :], r=[f"yo{o}"])

                for tl in passes:
                    moe_pass(tl)
                P.run()

        if "gdn" in phases:
            phase_gdn()
        if "nsa" in phases:
            phase_nsa()
        if "nsas" in phases:
            phase_nsas()
        if "tail" in phases:
            phase_tail_a()
            phase_tail_b()
        P.finish("sp")
        P.run()
    return nc


def make_in_maps(inputs, L=8192, NSEQ=4, TS=8, WINB=512, ncores=NCORES, PAST=16384):
    g = lambda k: np.asarray(inputs[k])
    w_in = g("w_in")[0]
    LH = L // 2
    tr = lambda v: np.ascontiguousarray(v.reshape(KC, 128).T)
    g1 = tr(g("norm1_g")[0])
    g2 = tr(g("norm2_g")[0])
    gf = np.ascontiguousarray(g("norm_f_g").reshape(1, D))
    ident = np.eye(128, dtype=np.float32)
    ws = np.ascontiguousarray(np.concatenate([w_in[:, O_QKV:O_QKV + 1536], w_in[:, O_B:O_B + 8], w_in[:, O_KV:O_KV + 768]], axis=1))
    wgab = np.ascontiguousarray(w_in[:, O_GA:O_GA + 2048])
    wr = np.ascontiguousarray(np.concatenate([g("w_grp")[0], g("w_rt")[0]], axis=1))
    br = np.ascontiguousarray(np.concatenate([g("b_grp")[0], g("b_rt")[0]]).reshape(1, 36))
    wz_ = np.ascontiguousarray(w_in[:, O_Z:O_Z + 512])
    cw_ = np.ascontiguousarray(g("gdn_conv_w")[0].reshape(4, 12, 128).transpose(2, 1, 0))
    gvec_ = np.ascontiguousarray(np.concatenate([g("gdn_a_log")[0], g("gdn_dt_bias")[0], g("gdn_norm_g")[0]]).reshape(1, 136))
    ii = np.arange(128)
    cmask_ = np.ascontiguousarray(np.stack([(ii[:, None] <= ii[None, :]).astype(np.float32),
                                            -(ii[:, None] < ii[None, :]).astype(np.float32),
                                            np.ones((128, 128), np.float32)], axis=1))
    NT = L // 128
    NCT = max(1, (8 * NT + 127) // 128)
    NSB = 2 * NT
    NC = 8 * NT - 1
    wn_ = np.ascontiguousarray(np.concatenate([w_in[:, O_QN:O_QN + 512], w_in[:, O_GN:O_GN + 24]], axis=1))
    tt = np.arange(L)
    qaug_ = np.zeros((2, 4, 4, L), np.float32)
    for gg in range(2):
        for hh in range(4):
            sl_ = 2.0 ** -(4 * gg + hh + 1)
            qaug_[gg, 0, hh] = -sl_ * 128 * (tt // 128)
            qaug_[gg, 1, hh] = -sl_ * (tt % 128)
            qaug_[gg, 2, hh] = sl_ * 128
            qaug_[gg, 3, hh] = sl_
    kaug_ = np.stack([np.ones(L), np.ones(L), tt // 128, tt % 128]).astype(np.float32)
    cend = 16 * np.arange(NCT * 128) + 31
    caug_ = np.stack([np.ones(NCT * 128), np.ones(NCT * 128), cend // 128, cend % 128]).astype(np.float32)
    caug_[:, NC:] = 0
    cc = np.arange(NCT * 128)[:, None]
    sj = np.arange(NSB)[None, :]
    ovf = np.maximum(np.minimum(16 * cc + 32, 64 * (sj + 1)) - np.maximum(16 * cc, 64 * sj), 0).astype(np.float32) / 32.0
    ovf[NC:] = 0
    ovt_ = np.ascontiguousarray(ovf.reshape(NCT, 128, NSB).transpose(1, 0, 2))
    cl_ = ii[:, None, None]; mm_ = np.arange(17)[None, :, None]; rr_ = ii[None, None, :]
    cmsk_ = (16 * cl_ - rr_ <= 128 * mm_ - 31).astype(np.float32)
    jrel = np.arange(2 * NSB)[None, :] - NSB
    rlo = (ii[:, None] < 64)
    BIGV = np.float32(1.0e30)
    ta_ = np.where(jrel <= -2, 1.0, np.where(jrel == -1, (~rlo).astype(np.float32), 0.0)).astype(np.float32)
    tb_ = np.where(jrel <= -2, 0.0, np.where(jrel == -1, 1.0e4 * rlo, np.where(jrel == 0, 1.0e4,
                   np.where(jrel == 1, np.where(rlo, -BIGV, 1.0e4), -BIGV)))).astype(np.float32)
    tab_ = np.ascontiguousarray(np.stack([np.broadcast_to(ta_, (128, 2 * NSB)), tb_], axis=1))
    tri2_ = np.ascontiguousarray(np.stack([(ii[:, None] <= ii[None, :]), (ii[None, :] <= ii[:, None])], axis=1).astype(np.float32))
    expt_ = (ii[:, None, None] == 2 * np.arange(NT)[None, :, None] + (ii[None, None, :] >= 64)).astype(np.float32)
    pwm_ = np.zeros((128, 4, 8), np.float32)
    for wi, key in enumerate(("cmp_pos_wk", "cmp_pos_wv")):
        pw = g(key)[0]
        for jb in range(8):
            for pp in range(32):
                if 16 * jb + pp < 128:
                    pwm_[16 * jb + pp, 2 * wi, jb] = pw[pp]
        pwm_[0:16, 2 * wi + 1, 7] = pw[16:32]
    wcmp_ = np.ascontiguousarray(np.stack([g("cmp_wk")[0], g("cmp_wv")[0]], axis=1))
    NPG = PAST // 128
    NKT = NPG + 1
    NCTS = (8 * NPG + 127) // 128
    NSBS = 2 * NPG + 1
    NCS = 8 * NPG - 1
    ckv_ = g("cache_kv")[0].reshape(-1, 512)
    piota_ = np.arange(128, dtype=np.float32).reshape(128, 1)
    qaugs_ = np.zeros((2, 4, 4, TS), np.float32)
    rr8 = np.arange(TS)
    for gg in range(2):
        for hh in range(4):
            sl_ = 2.0 ** -(4 * gg + hh + 1)
            qaugs_[gg, 0, hh] = -sl_ * 128 * NPG
            qaugs_[gg, 1, hh] = -sl_ * rr8
            qaugs_[gg, 2, hh] = sl_ * 128
            qaugs_[gg, 3, hh] = sl_
    tk = np.arange(NKT * 128)
    kaugs_ = np.stack([np.ones(NKT * 128), np.ones(NKT * 128), tk // 128, tk % 128]).astype(np.float32)
    cends = 16 * np.arange(NCTS * 128) + 31
    caugs_ = np.stack([np.ones(NCTS * 128), np.ones(NCTS * 128), cends // 128, cends % 128]).astype(np.float32)
    caugs_[2, NCS:] = -256.0
    caugs_[3, NCS:] = 0.0
    ccs = np.arange(NCTS * 128)[:, None]
    sjs = np.arange(NSBS)[None, :]
    ovfs = np.maximum(np.minimum(16 * ccs + 32, 64 * (sjs + 1)) - np.maximum(16 * ccs, 64 * sjs), 0).astype(np.float32) / 32.0
    ovfs[NCS:] = 0
    ovs_ = np.ascontiguousarray(ovfs.reshape(NCTS, 128, NSBS).transpose(1, 0, 2))
    expts_ = (ii[:, None, None] == 2 * np.arange(64)[None, :, None] + (ii[None, None, :] >= 64)).astype(np.float32)
    maps = []
    for c in range(ncores):
        b, s = c // 2, c % 2
        cols = []
        for base in (0, 512, 1024):
            cols.append(w_in[:, O_QKV + base + 256 * s:O_QKV + base + 256 * s + 256])
        cols.append(w_in[:, O_B + 2 * s:O_B + 2 * s + 2])
        cols.append(w_in[:, O_A + 2 * s:O_A + 2 * s + 2])
        for j in range(6):
            cols.append(w_in[:, O_KV + j * 128 + s * 64:O_KV + j * 128 + s * 64 + 64])
        wp = np.ascontiguousarray(np.concatenate(cols, axis=1))
        selv = np.zeros((128, 2), np.float32)
        selv[:, 0] = 1 - s
        selv[:, 1] = s
        xpb = g("x_prompt")[b, :L]
        maps.append({
            "xp": np.ascontiguousarray(xpb),
            "xh": np.ascontiguousarray(xpb[s * LH:(s + 1) * LH]),
            "xs": np.ascontiguousarray(g("x_sample")[NSEQ * c:NSEQ * (c + 1)].reshape(NSEQ * TS, D)),
            "g1": g1, "g2": g2, "gf": gf, "wp": wp, "ws": ws, "wgab": wgab,
            "wa": g("w_branch_a")[0], "wb": g("w_branch_b")[0], "wo": g("w_out")[0], "wr": wr, "br": br,
            "weg": g("w_e_gate")[0], "weu": g("w_e_up")[0], "wed": g("w_e_down")[0],
            "cwin": np.ascontiguousarray(g("cache_win")[0, NSEQ * c:NSEQ * (c + 1)].reshape(NSEQ, WINB, 256)),
            "ident": ident, "selv": selv,
            "wz": wz_, "cw": cw_, "gvec": gvec_, "cmask": cmask_,
            "wn": wn_, "qaug": qaug_, "kaug": kaug_, "caug": caug_, "ovt": ovt_, "cmsk": cmsk_, "tab": tab_, "tri2": tri2_,
            "expt": expt_, "pwm": pwm_, "wcmp": wcmp_,
            "ckv": ckv_, "ptab": np.ascontiguousarray(g("page_table")[NSEQ * c:NSEQ * (c + 1)].astype(np.int32)), "piota": piota_,
            "cwin4": np.ascontiguousarray(g("cache_win")[0, NSEQ * c:NSEQ * (c + 1)].reshape(NSEQ, 4, 128, 256)),
            "qaugs": qaugs_, "kaugs": kaugs_, "caugs": caugs_, "ovs": ovs_, "expts": expts_,
            "sgdn": np.ascontiguousarray(g("state_gdn")[0, NSEQ * c:NSEQ * (c + 1)].reshape(NSEQ * 4, 128, 128)),
            "sconv": np.ascontiguousarray(g("state_conv")[0, NSEQ * c:NSEQ * (c + 1)]),
        })
    return maps


def assemble(results, L=8192, NSEQ=4, TS=8, WINB=512):
    nb = NCORES // 2
    nsq = NCORES * NSEQ
    LH = L // 2
    y_prompt = np.zeros((nb, L, D), np.float32)
    y_sample = np.zeros((nsq, TS, D), np.float32)
    kv_prompt = np.zeros((1, nb, L, 4, 2, 64), np.float32)
    kv_sample = np.zeros((1, nsq, TS, 4, 2, 64), np.float32)
    win_prompt = np.zeros((1, nb, WINB, 2, 2, 64), np.float32)
    win_sample = np.zeros((1, nsq, WINB, 2, 2, 64), np.float32)
    gdn_prompt = np.zeros((1, nb, 4, 128, 128), np.float32)
    gdn_sample = np.zeros((1, nsq, 4, 128, 128), np.float32)
    conv_prompt = np.zeros((1, nb, 3, 1536), np.float32)
    conv_sample = np.zeros((1, nsq, 3, 1536), np.float32)
    for c, r in enumerate(results):
        b, s = c // 2, c % 2
        y_prompt[b, s * LH:(s + 1) * LH] = r["yp"]
        y_sample[NSEQ * c:NSEQ * (c + 1)] = r["ysm"].reshape(NSEQ, TS, D)
        if s == 1:
            gdn_prompt[0, b] = r["gdnp"]
        gdn_sample[0, NSEQ * c:NSEQ * (c + 1)] = r["gdns"].reshape(NSEQ, 4, 128, 128)
        kv_prompt[0, b, :, :, s, :] = r["kvp"]
        win_prompt[0, b, :, :, s, :] = r["winp"]
        cp = r["convp"]
        for j, base in enumerate((0, 512, 1024)):
            conv_prompt[0, b, :, base + 256 * s:base + 256 * s + 256] = cp[:, 256 * j:256 * j + 256]
        kv_sample[0, NSEQ * c:NSEQ * (c + 1)] = r["kvs"].reshape(NSEQ, TS, 4, 2, 64)
        win_sample[0, NSEQ * c:NSEQ * (c + 1)] = r["wins"].reshape(NSEQ, WINB, 2, 2, 64)
        conv_sample[0, NSEQ * c:NSEQ * (c + 1)] = r["convs"]
    return (y_prompt, y_sample, kv_prompt, kv_sample, win_prompt, win_sample,
            gdn_prompt, gdn_sample, conv_prompt, conv_sample)


def kernel(**inputs):
    nc = build_nc()
    in_maps = make_in_maps(inputs)
    res = run_bass_kernel_spmd(nc, in_maps, core_ids=list(range(NCORES)))
    return assemble(res.results)
```
